# Optimizing a Trainium2 kernel written in Bass

```python
import jax, jax.numpy as jnp
from jax import lax
import numpy as np

D_MODEL = 1024
BATCH = 16
SEQ = 2048
DEPTH = 1

PLE_DIM = 256
CONV_WIDTH = 512
CONV_GROUPS = 8
CONV_K = 3
N_HEADS = 8
N_KV_HEADS = 2
HEAD_DIM = 64
GROUP_SIZE = N_HEADS // N_KV_HEADS
ATTN_WIDTH = N_HEADS * HEAD_DIM
KV_WIDTH = N_KV_HEADS * HEAD_DIM
MIX_WIDTH = CONV_WIDTH + ATTN_WIDTH
N_BRANCH = 3
IN_COLS = 3 * CONV_WIDTH + ATTN_WIDTH + 2 * N_BRANCH * KV_WIDTH + N_BRANCH * N_HEADS
CMP_LEN = 32
CMP_STRIDE = 16
SEL_BLOCK = 64
N_SEL = 8
WINDOW = 512
Q_BLOCK = 128
D_FF = 2816
FFN_K = 3
ROPE_THETA = 10000.0
EPS = 1e-6
NEG = -1e30
SEL_FORCE = 1e4

kernel_name = 'hybrid_shortconv_nsa_convffn_ple'


def rms_norm(x, g):
    xf = x.astype(jnp.float32)
    y = xf * lax.rsqrt(jnp.mean(xf * xf, axis=-1, keepdims=True) + EPS)
    return (y * g.astype(jnp.float32)).astype(x.dtype)


def causal_dwconv(x, w):
    k, c = w.shape
    return lax.conv_general_dilated(
        x, w[:, None, :].astype(x.dtype), window_strides=(1,), padding=[(k - 1, 0)],
        dimension_numbers=('NWC', 'WIO', 'NWC'), feature_group_count=c)


def rope_tables(pos):
    inv = ROPE_THETA ** (-jnp.arange(0, HEAD_DIM, 2, dtype=jnp.float32) / HEAD_DIM)
    ang = pos.astype(jnp.float32)[:, None] * inv[None, :]
    ang = jnp.concatenate([ang, ang], axis=-1)
    return jnp.cos(ang), jnp.sin(ang)


def apply_rope(x, cos, sin):
    x1, x2 = jnp.split(x, 2, axis=-1)
    rot = jnp.concatenate([-x2, x1], axis=-1)
    return (x * cos + rot * sin).astype(x.dtype)


def masked_softmax(s, mask):
    s = jnp.where(mask, s.astype(jnp.float32), NEG)
    m = jnp.max(s, axis=-1, keepdims=True)
    e = jnp.where(mask, jnp.exp(s - m), 0.0)
    return e / jnp.maximum(jnp.sum(e, axis=-1, keepdims=True), 1e-30)


def short_conv_mixer(x_in, b_gate, c_gate, w):
    return b_gate * causal_dwconv(c_gate * x_in, w)


def compress_kv(kv, cmp_idx, pe, w1, w2):
    blocks = kv[:, :, cmp_idx] + pe.astype(kv.dtype)
    flat = blocks.reshape(blocks.shape[:3] + (CMP_LEN * HEAD_DIM,))
    return jax.nn.silu(flat @ w1) @ w2


def native_sparse_attention(q, kc, vc, ks, vs, kw, vw, gates, cmp_end, overlap):
    b, s = q.shape[:2]
    n_blk = s // SEL_BLOCK
    n_sel = min(N_SEL, n_blk)
    n_qb = s // Q_BLOCK
    scale = HEAD_DIM ** -0.5
    qg = q.reshape(b, s, N_KV_HEADS, GROUP_SIZE, HEAD_DIM).transpose(0, 2, 3, 1, 4)
    gg = gates.reshape(b, s, N_KV_HEADS, GROUP_SIZE, N_BRANCH).transpose(0, 2, 3, 1, 4)
    ks_blk = ks.reshape(b, N_KV_HEADS, n_blk, SEL_BLOCK, HEAD_DIM)
    vs_blk = vs.reshape(b, N_KV_HEADS, n_blk, SEL_BLOCK, HEAD_DIM)
    kw_pad = jnp.pad(kw, ((0, 0), (0, 0), (WINDOW, 0), (0, 0)))
    vw_pad = jnp.pad(vw, ((0, 0), (0, 0), (WINDOW, 0), (0, 0)))
    blk_ids = jnp.arange(n_blk)
    gather = jax.vmap(jax.vmap(lambda blocks, ix: blocks[ix]))

    def query_block(i):
        q0 = i * Q_BLOCK
        t = q0 + jnp.arange(Q_BLOCK)
        qb = lax.dynamic_slice_in_dim(qg, q0, Q_BLOCK, axis=3)
        gb = lax.dynamic_slice_in_dim(gg, q0, Q_BLOCK, axis=3).astype(jnp.float32)
        s_c = jnp.einsum('bhgqd,bhcd->bhgqc', qb, kc) * scale
        p_c = masked_softmax(s_c, cmp_end[None, :] <= t[:, None])
        o_c = jnp.einsum('bhgqc,bhcd->bhgqd', p_c.astype(vc.dtype), vc)
        imp = jnp.einsum('bhqc,cn->bhqn', p_c.sum(axis=2), overlap)
        cur = t // SEL_BLOCK
        valid = blk_ids[None, :] * SEL_BLOCK <= t[:, None]
        forced = (blk_ids[None, :] == 0) | (blk_ids[None, :] == cur[:, None]) | (blk_ids[None, :] == cur[:, None] - 1)
        score = jnp.where(valid, imp + jnp.where(forced, SEL_FORCE, 0.0), NEG)
        _, idx = lax.top_k(score, n_sel)
        k_g = gather(ks_blk, idx).reshape(b, N_KV_HEADS, Q_BLOCK, n_sel * SEL_BLOCK, HEAD_DIM)
        v_g = gather(vs_blk, idx).reshape(b, N_KV_HEADS, Q_BLOCK, n_sel * SEL_BLOCK, HEAD_DIM)
        kpos = (idx[..., None] * SEL_BLOCK + jnp.arange(SEL_BLOCK)).reshape(b, N_KV_HEADS, Q_BLOCK, n_sel * SEL_BLOCK)
        s_s = jnp.einsum('bhgqd,bhqkd->bhgqk', qb, k_g) * scale
        p_s = masked_softmax(s_s, (kpos <= t[:, None])[:, :, None])
        o_s = jnp.einsum('bhgqk,bhqkd->bhgqd', p_s.astype(v_g.dtype), v_g)
        k_w = lax.dynamic_slice_in_dim(kw_pad, q0, Q_BLOCK + WINDOW, axis=2)
        v_w = lax.dynamic_slice_in_dim(vw_pad, q0, Q_BLOCK + WINDOW, axis=2)
        wpos = q0 - WINDOW + jnp.arange(Q_BLOCK + WINDOW)
        wmask = (wpos[None, :] >= 0) & (wpos[None, :] <= t[:, None]) & (wpos[None, :] > t[:, None] - WINDOW)
        s_w = jnp.einsum('bhgqd,bhkd->bhgqk', qb, k_w) * scale
        p_w = masked_softmax(s_w, wmask)
        o_w = jnp.einsum('bhgqk,bhkd->bhgqd', p_w.astype(v_w.dtype), v_w)
        o = gb[..., 0:1] * o_c + gb[..., 1:2] * o_s + gb[..., 2:3] * o_w
        return o.astype(q.dtype)

    out = lax.map(query_block, jnp.arange(n_qb))
    return out.transpose(1, 0, 4, 2, 3, 5).reshape(b, s, ATTN_WIDTH)


def setup_inputs(seed: int = 0) -> dict:
    key = jax.random.key(seed)
    ks = jax.random.split(key, 22)
    f32 = jnp.float32
    L = DEPTH

    def nrm(k, shape, fan_in):
        return jax.random.normal(k, shape, f32) * fan_in ** -0.5

    def gain(k, shape):
        return 1.0 + 0.02 * jax.random.normal(k, shape, f32)

    return {
        'x': jax.random.normal(ks[0], (BATCH, SEQ, D_MODEL), f32),
        'p': jax.random.normal(ks[1], (DEPTH, BATCH, SEQ, PLE_DIM), f32),
        'g_mix': gain(ks[2], (L, D_MODEL)),
        'w_in': nrm(ks[3], (L, D_MODEL, IN_COLS), D_MODEL),
        'w_conv_mix': nrm(ks[4], (L, CONV_K, CONV_WIDTH), CONV_K),
        'cmp_pe_k': 0.1 * jax.random.normal(ks[5], (L, CMP_LEN, HEAD_DIM), f32),
        'cmp_w1_k': nrm(ks[6], (L, CMP_LEN * HEAD_DIM, HEAD_DIM), CMP_LEN * HEAD_DIM),
        'cmp_w2_k': nrm(ks[7], (L, HEAD_DIM, HEAD_DIM), HEAD_DIM),
        'cmp_pe_v': 0.1 * jax.random.normal(ks[8], (L, CMP_LEN, HEAD_DIM), f32),
        'cmp_w1_v': nrm(ks[9], (L, CMP_LEN * HEAD_DIM, HEAD_DIM), CMP_LEN * HEAD_DIM),
        'cmp_w2_v': nrm(ks[10], (L, HEAD_DIM, HEAD_DIM), HEAD_DIM),
        'g_gn_conv': gain(ks[11], (L, CONV_WIDTH)),
        'g_gn_attn': gain(ks[12], (L, ATTN_WIDTH)),
        'w_out': nrm(ks[13], (L, MIX_WIDTH, D_MODEL), MIX_WIDTH),
        'g_ffn': gain(ks[14], (L, D_MODEL)),
        'w_up': nrm(ks[15], (L, D_MODEL, 2 * D_FF), D_MODEL),
        'w_ffn_conv': nrm(ks[16], (L, FFN_K, 2 * D_FF), FFN_K),
        'w_down': nrm(ks[17], (L, D_FF, D_MODEL), D_FF),
        'g_ple': gain(ks[18], (L, D_MODEL)),
        'w_ple_gate': nrm(ks[19], (L, D_MODEL, D_MODEL), D_MODEL),
        'w_ple_proj': nrm(ks[20], (L, PLE_DIM, D_MODEL), PLE_DIM),
        'g_final': gain(ks[21], (D_MODEL,)),
    }


def reference(x, p, g_mix, w_in, w_conv_mix, cmp_pe_k, cmp_w1_k, cmp_w2_k, cmp_pe_v, cmp_w1_v,
              cmp_w2_v, g_gn_conv, g_gn_attn, w_out, g_ffn, w_up, w_ffn_conv, w_down, g_ple,
              w_ple_gate, w_ple_proj, g_final):
    b, s, _ = x.shape
    pos = jnp.arange(s, dtype=jnp.int32)
    cos, sin = rope_tables(pos)
    cos_h, sin_h = cos[:, None, :], sin[:, None, :]
    n_cmp = (s - CMP_LEN) // CMP_STRIDE + 1
    n_blk = s // SEL_BLOCK
    cmp_start = jnp.arange(n_cmp) * CMP_STRIDE
    cmp_idx = cmp_start[:, None] + jnp.arange(CMP_LEN)[None, :]
    cmp_end = cmp_start + CMP_LEN - 1
    cos_c, sin_c = rope_tables(cmp_end)
    blk_start = jnp.arange(n_blk) * SEL_BLOCK
    overlap = (jnp.clip(jnp.minimum(cmp_start[:, None] + CMP_LEN, blk_start[None, :] + SEL_BLOCK)
                        - jnp.maximum(cmp_start[:, None], blk_start[None, :]), 0, None)
               .astype(jnp.float32) / CMP_LEN)
    sizes = [CONV_WIDTH] * 3 + [ATTN_WIDTH] + [KV_WIDTH] * (2 * N_BRANCH) + [N_BRANCH * N_HEADS]
    split_at = np.cumsum(sizes)[:-1].tolist()

    def kv_heads(t):
        return t.reshape(b, s, N_KV_HEADS, HEAD_DIM)

    h = x
    for i in range(DEPTH):
        n1 = rms_norm(h, g_mix[i])
        proj = n1 @ w_in[i]
        (x_in, b_gate, c_gate, q, k_cmp, v_cmp, k_slc, v_slc, k_win, v_win,
         gate_logits) = jnp.split(proj, split_at, axis=-1)
        y_conv = short_conv_mixer(x_in, b_gate, c_gate, w_conv_mix[i])

        q = apply_rope(q.reshape(b, s, N_HEADS, HEAD_DIM), cos_h, sin_h)
        ks_r = apply_rope(kv_heads(k_slc), cos_h, sin_h).transpose(0, 2, 1, 3)
        kw_r = apply_rope(kv_heads(k_win), cos_h, sin_h).transpose(0, 2, 1, 3)
        vs_t = kv_heads(v_slc).transpose(0, 2, 1, 3)
        vw_t = kv_heads(v_win).transpose(0, 2, 1, 3)
        kc = compress_kv(kv_heads(k_cmp).transpose(0, 2, 1, 3), cmp_idx, cmp_pe_k[i], cmp_w1_k[i], cmp_w2_k[i])
        kc = apply_rope(kc, cos_c, sin_c)
        vc = compress_kv(kv_heads(v_cmp).transpose(0, 2, 1, 3), cmp_idx, cmp_pe_v[i], cmp_w1_v[i], cmp_w2_v[i])
        gates = jax.nn.sigmoid(gate_logits).reshape(b, s, N_HEADS, N_BRANCH)
        y_attn = native_sparse_attention(q, kc, vc, ks_r, vs_t, kw_r, vw_t, gates, cmp_end, overlap)

        mixed = jnp.concatenate([rms_norm(y_conv, g_gn_conv[i]), rms_norm(y_attn, g_gn_attn[i])], axis=-1)
        h = h + mixed @ w_out[i]

        u = causal_dwconv(rms_norm(h, g_ffn[i]) @ w_up[i], w_ffn_conv[i])
        u_gate, u_val = jnp.split(u, 2, axis=-1)
        h = h + (jax.nn.silu(u_gate) * u_val) @ w_down[i]

        ple_gate = jax.nn.sigmoid(rms_norm(h, g_ple[i]) @ w_ple_gate[i])
        h = h + ple_gate * (p[i] @ w_ple_proj[i])

    return rms_norm(h, g_final)
```

```python
import numpy as np
import ml_dtypes
from contextlib import ExitStack
import concourse.bass as bass
import concourse.mybir as mybir
from concourse.bass_utils import run_bass_kernel_spmd

F32 = mybir.dt.float32
BF16 = mybir.dt.bfloat16
U8 = mybir.dt.uint8
ALU = mybir.AluOpType
AF = mybir.ActivationFunctionType

ENGS = ['pe', 'act', 'dve', 'pool', 'sp']
EPOCH = 12000
NDSEM = 16

D = 1024
S_LEN = 2048
NSEQ = 2
NT = 16
DFF = 2816
NF = 22
NEGB = -30000.0
EPS = 1e-6
CFG = dict(tilebar=False, ntile=4, tm=True, a_stage=9, nseq=2, nga=4, do_b=True, nqt=16, do_p2=True, ng2=4)


class Op:
    __slots__ = ('eng', 'emit', 'deps', 'id', 'sig', 'dma', 'comp', 'dk')


class Sched:
    def __init__(self, nc):
        self.nc = nc
        self.ops = []
        self.last_w = {}
        self.readers = {}
        self.eng_list = {e: [] for e in ENGS}
        self.ndma = {e: 0 for e in ENGS}

    def add(self, eng, emit, r=(), w=(), dma=False):
        op = Op()
        op.eng = eng; op.emit = emit; op.id = len(self.ops); op.dma = dma
        op.sig = False; op.comp = None; op.dk = None
        deps = set()
        rw = set()
        for res in r:
            if res in self.last_w:
                deps.add(self.last_w[res]); rw.add(self.last_w[res])
            if res == 'PB' or (isinstance(res, tuple) and res[0] == 'P'):
                for x in self.readers.get(res, ()):
                    if self.ops[x].eng != eng:
                        deps.add(x)
        for res in w:
            if res in self.last_w:
                deps.add(self.last_w[res])
            deps.update(self.readers.get(res, ()))
        fd = set()
        for d in deps:
            dop = self.ops[d]
            if (not dma) and (not dop.dma) and dop.eng == eng:
                if eng == 'pe':
                    continue
            fd.add(d)
        op.deps = fd
        for res in r:
            lst = self.readers.setdefault(res, [])
            if not dma:
                lst[:] = [x for x in lst if self.ops[x].dma or self.ops[x].eng != eng]
            lst.append(op.id)
        for res in w:
            self.last_w[res] = op.id
            self.readers[res] = []
        if dma:
            op.dk = self.ndma[eng]
            self.ndma[eng] += 1
        self.ops.append(op)
        self.eng_list[eng].append(op)
        return op

    def barrier(self):
        n = len(self.ops)
        for e in ENGS:
            self.add(e, None, r=(), w=[('bar', n, e)])
        lastd = {}
        for op in self.ops:
            if op.dma:
                lastd[(op.eng, op.dk % NDSEM)] = op.id
        for e in ENGS:
            w = self.add(e, None, r=[('bar', n, e2) for e2 in ENGS], w=())
            w.deps.update(lastd.values())
        self.last_w.clear()
        self.readers.clear()

    def finalize(self):
        for op in self.ops:
            for d in op.deps:
                self.ops[d].sig = True
        self.nsem_eng = {}
        for e in ENGS:
            c = 0
            for op in self.eng_list[e]:
                if op.dma:
                    continue
                if op.sig:
                    c += 1
                    op.comp = ('c', e, (c - 1) // EPOCH, (c - 1) % EPOCH + 1)
            self.nsem_eng[e] = (c + EPOCH - 1) // EPOCH if c else 0
        for op in self.ops:
            if op.dma:
                op.comp = ('d', op.eng, op.dk % NDSEM, 16 * (op.dk // NDSEM + 1))

    def run(self):
        nc = self.nc
        self.finalize()
        es = ExitStack()
        sems = {}
        for e in ENGS:
            for i in range(self.nsem_eng[e]):
                sems[('c', e, i)] = es.enter_context(nc.semaphore(f"s_{e}_{i}"))
            for i in range(min(NDSEM, self.ndma[e])):
                sems[('d', e, i)] = es.enter_context(nc.semaphore(f"d_{e}_{i}"))
        block = es.enter_context(nc.Block())
        sched = self

        def body(ename):
            def f(eng):
                waited = {}
                cnt = {}
                for op in sched.eng_list[ename]:
                    need = {}
                    for d in op.deps:
                        k = sched.ops[d].comp
                        need[k[:3]] = max(need.get(k[:3], 0), k[3])
                    if op.dma and op.dk >= NDSEM:
                        key = ('d', ename, op.dk % NDSEM)
                        need[key] = max(need.get(key, 0), 16 * (op.dk // NDSEM))
                    for key, val in need.items():
                        if waited.get(key, 0) >= val:
                            continue
                        waited[key] = val
                        eng.wait_ge(sems[key], val)
                    if op.emit is None:
                        if op.sig:
                            eng.drain().then_inc(sems[op.comp[:3]], 1)
                        continue
                    ins = op.emit(eng)
                    if op.dma:
                        ins.then_inc(sems[op.comp[:3]], 16)
                        cnt[op.dk % NDSEM] = cnt.get(op.dk % NDSEM, 0) + 1
                    elif op.sig:
                        ins.then_inc(sems[op.comp[:3]], 1)
                for i, c in cnt.items():
                    key = ('d', ename, i)
                    if waited.get(key, 0) < 16 * c:
                        eng.wait_ge(sems[key], 16 * c)
            return f

        block.tensor(body('pe'))
        block.scalar(body('act'))
        block.vector(body('dve'))
        block.gpsimd(body('pool'))
        block.sync(body('sp'))
        es.close()


def host_consts():
    bf = ml_dtypes.bfloat16
    c = {}
    c['ident'] = np.eye(128, dtype=np.float32).astype(bf)
    k = np.arange(128)[:, None]; m = np.arange(128)[None, :]
    c['rblk'] = ((k // 64 == m // 64) & (k % 64 == (m % 64 + 32) % 64)).astype(np.float32).astype(bf)
    inv = (10000.0 ** (-np.arange(0, 64, 2, dtype=np.float32) / 64)).astype(np.float32)
    pr = np.arange(128) % 64
    fr = inv[pr % 32][:, None]
    sgn = np.where(pr < 32, -1.0, 1.0)[:, None].astype(np.float32)
    pos = np.arange(S_LEN, dtype=np.float32)[None, :]
    ang = (pos * fr).astype(np.float32)
    c['cosT'] = np.cos(ang).astype(np.float32).astype(bf)
    c['sinT'] = (np.sin(ang) * sgn).astype(np.float32).astype(bf)
    posc = (np.arange(127, dtype=np.float32) * 16 + 31)[None, :]
    angc = (posc * fr).astype(np.float32)
    c['coscT'] = np.cos(angc).astype(np.float32).astype(bf)
    c['sincT'] = (np.sin(angc) * sgn).astype(np.float32).astype(bf)
    cc = np.arange(128)[:, None, None]; ii = np.arange(16)[None, :, None]; qq = np.arange(128)[None, None, :]
    c['maskc'] = np.where(16 * cc + 31 <= 128 * ii + qq, 0.0, NEGB).astype(np.float32).astype(bf)
    qq2 = np.arange(128)[:, None, None]; ii2 = np.arange(16)[None, :, None]; nn = np.arange(32)[None, None, :]
    t = 128 * ii2 + qq2
    cur = t // 64
    valid = nn * 64 <= t
    forced = (nn == 0) | (nn == cur) | (nn == cur - 1)
    c['selbias'] = np.where(valid, np.where(forced, 1e4, 0.0), -1e30).astype(np.float32)
    b = np.arange(32)[:, None, None]; jj = np.arange(16)[None, :, None]; p = np.arange(128)[None, None, :]
    c['eall'] = (b == 2 * jj + p // 64).astype(np.float32).astype(bf)
    kk = np.arange(128)[:, None]; q = np.arange(128)[None, :]
    c['causal'] = np.where(kk > q, NEGB, 0.0).astype(np.float32).astype(bf)
    c['lower'] = np.where(kk <= q, NEGB, 0.0).astype(np.float32).astype(bf)
    cs = np.arange(127)[:, None] * 16; bs = np.arange(32)[None, :] * 64
    ov = np.clip(np.minimum(cs + 32, bs + 64) - np.maximum(cs, bs), 0, None).astype(np.float32) / 32
    c['ov'] = np.concatenate([ov, np.zeros((1, 32), np.float32)], 0).astype(bf)
    c['ones'] = np.ones((128, 128), np.float32).astype(bf)
    return c


CONST_SHAPES = {
    'ident': ([128, 128], BF16), 'rblk': ([128, 128], BF16), 'cosT': ([128, S_LEN], BF16),
    'sinT': ([128, S_LEN], BF16), 'coscT': ([128, 127], BF16), 'sincT': ([128, 127], BF16),
    'maskc': ([128, 16, 128], BF16), 'selbias': ([128, 16, 32], F32), 'eall': ([32, 16, 128], BF16),
    'causal': ([128, 128], BF16), 'lower': ([128, 128], BF16), 'ov': ([128, 32], BF16),
    'ones': ([128, 128], BF16),
}

WSHAPES = {
    'g_mix': [D], 'w_in': [D, 2840], 'w_conv_mix': [3, 512], 'cmp_pe_k': [32, 64], 'cmp_w1_k': [2048, 64],
    'cmp_w2_k': [64, 64], 'cmp_pe_v': [32, 64], 'cmp_w1_v': [2048, 64], 'cmp_w2_v': [64, 64],
    'g_gn_conv': [512], 'g_gn_attn': [512], 'w_out': [D, D], 'g_ffn': [D], 'w_up': [D, 2 * DFF],
    'w_ffn_conv': [3, 2 * DFF], 'w_down': [DFF, D], 'g_ple': [D], 'w_ple_gate': [D, D],
    'w_ple_proj': [256, D], 'g_final': [D],
}


def build_program(debug=None):
    nc = bass.Bass("TRN2", target_bir_lowering=False)
    dr = {}
    dr['x'] = nc.dram_tensor("x", [NSEQ, S_LEN, D], F32, kind="ExternalInput").ap()
    dr['p'] = nc.dram_tensor("p", [NSEQ, S_LEN, 256], F32, kind="ExternalInput").ap()
    for k, shp in WSHAPES.items():
        dr[k] = nc.dram_tensor(k, shp, F32, kind="ExternalInput").ap()
    for k, (shp, dt) in CONST_SHAPES.items():
        dr[k] = nc.dram_tensor("c_" + k, shp, dt, kind="ExternalInput").ap()
    out = nc.dram_tensor("out", [NSEQ, S_LEN, D], F32, kind="ExternalOutput").ap()
    wbf = {}
    for k in ('w_in', 'w_up', 'w_down', 'w_out', 'w_ple_gate', 'w_ple_proj'):
        wbf[k] = nc.dram_tensor(k + "_bf", WSHAPES[k], BF16, kind="Internal").ap()
    dbg_out = {}
    if debug:
        for name, shp in debug.items():
            dbg_out[name] = nc.dram_tensor("dbg_" + name, shp, F32, kind="ExternalOutput").ap()

    es = ExitStack()
    ARENA = 206 * 1024
    arena = es.enter_context(nc.sbuf_tensor("arena", [128, ARENA], U8))
    pbank = [es.enter_context(nc.psum_tensor(f"pb{i}", [128, 512], F32)) for i in range(7)]
    PB = es.enter_context(nc.psum_tensor("pbt", [128, 1024], BF16))
    P = [b[:] for b in pbank]
    PBa = PB[:]

    state = {'off': 0}

    def carve(shape, dt):
        esz = 4 if dt == F32 else 2
        n = int(np.prod(shape[1:]))
        off = state['off']
        nb = (n * esz + 63) // 64 * 64
        assert off + nb <= ARENA, ("SBUF arena overflow", off, nb)
        state['off'] = off + nb
        ap = arena[0:shape[0], off:off + n * esz].bitcast(dt)
        if len(shape) == 3:
            ap = ap.rearrange("p (a b) -> p a b", a=shape[1])
        elif len(shape) == 4:
            ap = ap.rearrange("p (a b c) -> p a b c", a=shape[1], b=shape[2])
        return ap

    S = Sched(nc)

    def mm(o, lhsT, rhs, start, stop, r, w, **kw):
        S.add('pe', lambda e: e.matmul(o, lhsT=lhsT, rhs=rhs, start=start, stop=stop, **kw), r=r, w=w)

    def tr(o, in_, r, w):
        S.add('pe', lambda e: e.transpose(out=o, in_=in_, identity=ident), r=list(r) + ['ident'], w=w)

    def act(o, in_, func, r, w, **kw):
        S.add('act', lambda e: e.activation(out=o, in_=in_, func=func, **kw), r=r, w=w)

    def tt(eng, o, a, b, op, r, w):
        S.add(eng, lambda e: e.tensor_tensor(out=o, in0=a, in1=b, op=op), r=r, w=w)

    def stt(eng, o, a, sc, b, op0, op1, r, w):
        S.add(eng, lambda e: e.scalar_tensor_tensor(out=o, in0=a, scalar=sc, in1=b, op0=op0, op1=op1), r=r, w=w)

    def ts(eng, o, a, s1, s2, op0, op1, r, w):
        if op1 is None:
            S.add(eng, lambda e: e.tensor_scalar(out=o, in0=a, scalar1=s1, scalar2=None, op0=op0), r=r, w=w)
        else:
            S.add(eng, lambda e: e.tensor_scalar(out=o, in0=a, scalar1=s1, scalar2=s2, op0=op0, op1=op1), r=r, w=w)

    def cp(eng, o, a, r, w):
        S.add(eng, lambda e: e.tensor_copy(out=o, in_=a), r=r, w=w)

    def recip(o, a, r, w):
        S.add('dve', lambda e: e.reciprocal(out=o, in_=a), r=r, w=w)

    def memset(eng, o, v, w):
        S.add(eng, lambda e: e.memset(o, v), r=(), w=w)

    def dma(eng, o, in_, r, w, **kw):
        S.add(eng, lambda e: e.dma_start(out=o, in_=in_, **kw), r=r, w=w, dma=True)

    def dump(name, ap, r):
        if name in dbg_out:
            dma('pool', dbg_out[name], ap, r=r, w=[])

    ident = carve([128, 128], BF16)
    ones = carve([128, 128], BF16)
    mixedT = carve([128, 8, S_LEN], BF16)
    g_mix = carve([128, 8], F32); g_ffn = carve([128, 8], F32); g_ple = carve([128, 8], F32)
    g_gc = carve([128, 4], F32); g_ga = carve([128, 4], F32)
    xs = [carve([128, D], BF16) for _ in range(2)]
    nTb = [carve([128, 8, 512], BF16) for _ in range(2)]
    nT = nTb[0]
    ss = [carve([128, 1], F32) for _ in range(2)]
    rr = [carve([128, 1], F32) for _ in range(2)]
    junk = carve([128, D], BF16)
    dma('sp', ident, dr['ident'], r=[], w=['ident'])
    dma('sp', ones, dr['ones'], r=[], w=['ones'])
    for nm, tl, n in (('g_mix', g_mix, 8), ('g_ffn', g_ffn, 8), ('g_ple', g_ple, 8), ('g_gn_conv', g_gc, 4), ('g_gn_attn', g_ga, 4)):
        dma('sp', tl, dr[nm].rearrange("(c p) -> p c", p=128), r=[], w=[nm], allow_slow_non_contiguous=True)
    base_off = state['off']
    cnt = {'x': 0, 'n': 0}
    def cast_w(k, inner, rsplit=1, gate=()):
        R = WSHAPES[k][0]
        step = R // rsplit
        for i in range(rsplit):
            rs = slice(i * step, (i + 1) * step)
            dma('pool', wbf[k][rs, :].rearrange("r (a b) -> r a b", b=inner), dr[k][rs, :].rearrange("r (a b) -> r a b", b=inner),
                r=list(gate), w=[k + '_bf'])
    for blk in (4, 0, 1, 2, 3):
        dma('pool', wbf['w_in'][:, blk * 568:(blk + 1) * 568], dr['w_in'][:, blk * 568:(blk + 1) * 568], r=[], w=[('w_in_bf', blk)])

    def win_res(c0, n):
        return [('w_in_bf', b) for b in range(c0 // 568, (c0 + n - 1) // 568 + 1)]

    def rmsnorm_T(src, src_res, gt, g_res, tsl, nb=0, nres=None):
        k = cnt['n'] % 2; cnt['n'] += 1
        act(junk, src, AF.Square, r=[src_res], w=['junk', ('ss', k)], scale=1.0 / 32, accum_out=ss[k])
        act(rr[k], ss[k], AF.Sqrt, r=[('ss', k)], w=[('rr', k)], bias=EPS, scale=1.0)
        recip(rr[k], rr[k], r=[('rr', k)], w=[('rr', k)])
        act(xs[k], src, AF.Copy, r=[src_res, ('rr', k)], w=[('xs', k)], scale=rr[k])
        for c in range(8):
            tr(PBa[:, c * 128:(c + 1) * 128], xs[k][:, c * 128:(c + 1) * 128], r=[('xs', k)], w=['PB'])
        tt('dve', nTb[nb][:, :, tsl], PBa.rearrange("p (c t) -> p c t", c=8), gt.unsqueeze(2).to_broadcast([128, 8, 128]),
           ALU.mult, r=['PB', g_res], w=[nres if nres is not None else ('nT', nb)])

    def phase1(s):
        state['off'] = base_off
        qT = carve([128, 4, S_LEN], BF16)
        ksT = carve([128, S_LEN], BF16); kwT = carve([128, S_LEN], BF16)
        kcmpT = carve([128, S_LEN], BF16); vcmpT = carve([128, S_LEN], BF16)
        vs_ext = carve([128, NT, 2, 65], BF16); vw_ext = carve([128, NT, 2, 65], BF16)
        gates = carve([128, NT, 24], F32)
        w1blk = carve([128, 32, 128], BF16)
        w1d = carve([128, 16, 128], BF16)
        pecol = carve([128, 16], BF16)
        w2blk = carve([128, 128], BF16)
        c1 = carve([128, 1], F32)
        hidT = carve([128, 128], BF16)
        kcraw = carve([128, 128], BF16)
        kcT = carve([128, 128], BF16)
        vc_ext = carve([128, 2, 97], BF16)
        cosT = carve([128, S_LEN], BF16); sinT = carve([128, S_LEN], BF16)
        coscT = carve([128, 127], BF16); sincT = carve([128, 127], BF16)
        maskc = carve([128, 16, 128], BF16)
        selbias = carve([128, 16, 32], F32)
        eall = carve([32, 16, 128], BF16)
        rblk = carve([128, 128], BF16); causal = carve([128, 128], BF16); lower = carve([128, 128], BF16)
        ovt = carve([128, 32], BF16)
        cw = carve([128, 3, 4], F32)
        xt = [carve([128, D], F32) for _ in range(2)]
        wfm = [carve([128, 8, 128], BF16) for _ in range(4)]
        wtm = carve([128, 8, 280], BF16)
        xin_sb = [carve([128, 512], F32) for _ in range(2)]
        zb = [carve([128, 514], F32) for _ in range(2)]
        zh = carve([128, 4, 2], F32)
        accb = [carve([128, 512], F32) for _ in range(2)]
        ycv = carve([128, 4, 512], F32)
        ysq = carve([128, 4, 512], BF16)
        rc = carve([128, 512], F32)
        qraw = [carve([128, 512], BF16) for _ in range(2)]
        t1 = [carve([128, 512], F32) for _ in range(2)]
        t2 = [carve([128, 512], F32) for _ in range(2)]
        pT = [carve([128, 512], BF16) for _ in range(4)]
        yat = carve([128, 8, 64], F32)
        yab = carve([128, 512], BF16)
        tmpa = carve([128, 4, 64], F32)
        tmpw = carve([128, 4, 64], F32)
        sm = carve([128, 96], F32)
        score = carve([128, 32], F32)
        negsel = carve([128, 32], BF16)
        negselT = carve([32, 128], BF16)
        top8 = carve([128, 8], F32)

        for k3 in range(3):
            dma('sp', cw[:, k3, :], dr['w_conv_mix'][k3].rearrange("(j p) -> p j", p=128), r=[], w=['cw'], allow_slow_non_contiguous=True)
        memset('pool', vs_ext[:, :, :, 64:65], 1.0, w=['vs_ext'])
        memset('pool', vw_ext[:, :, :, 64:65], 1.0, w=['vw_ext'])
        memset('pool', vc_ext[:, :, 64:65], 1.0, w=['vc_ext'])
        memset('pool', zh, 0.0, w=['zh'])

        w_in_v = wbf['w_in'].rearrange("(c p) n -> p c n", p=128)
        xsrc = dr['x']
        fmk = {'k': 0, 'pk': 0, 'nb': 0, 'gi': 0}
        FMB = [0, 1, 2, 5, 6]

        def fm_chunk(col_pieces):
            k = fmk['k'] % 4; fmk['k'] += 1
            pk = FMB[fmk['pk'] % 5]; fmk['pk'] += 1
            o = 0
            for (c0, n) in col_pieces:
                dma('sp', wfm[k][:, :, o:o + n], w_in_v[:, :, c0:c0 + n], r=win_res(c0, n), w=[('wfm', k)])
                o += n
            for kc in range(8):
                mm(P[pk], lhsT=wfm[k][:, kc, :], rhs=nTb[fmk['nb']][:, kc, :], start=(kc == 0), stop=(kc == 7),
                   r=[('wfm', k), ('nT', fmk['nb'])], w=[('P', pk)])
            if pend:
                fn_, g_, t_ = pend.pop(0)
                fn_(g_, t_)
            return pk

        def rope(pk, dst, dst_res, tsl, cT, sT, cres, n=512):
            kq = cnt['x'] % 2; cnt['x'] += 1
            act(qraw[kq][:, 0:n], P[pk][:, 0:n], AF.Copy, r=[('P', pk)], w=[('qraw', kq)])
            mm(P[3][:, 0:n], lhsT=rblk, rhs=qraw[kq][:, 0:n], start=True, stop=True, r=['rblk', ('qraw', kq)], w=[('P', 3)])
            tt('dve', t1[kq][:, 0:n], P[pk][:, 0:n], cT, ALU.mult, r=[('P', pk), cres[0]], w=[('t1', kq)])
            tt('dve', t2[kq][:, 0:n], P[3][:, 0:n], sT, ALU.mult, r=[('P', 3), cres[1]], w=[('t2', kq)])
            tt('dve', dst, t1[kq][:, 0:n], t2[kq][:, 0:n], ALU.add, r=[('t1', kq), ('t2', kq)], w=[dst_res])

        dma('sp', wtm[:, :, 0:128], w_in_v[:, :, 2432:2560], r=win_res(2432, 128), w=['wtm'])
        dma('sp', wtm[:, :, 128:256], w_in_v[:, :, 2688:2816], r=win_res(2688, 128), w=['wtm'])
        dma('sp', wtm[:, :, 256:280], w_in_v[:, :, 2816:2840], r=win_res(2816, 24), w=['wtm'])

        def tf_a(gi, t4):
            ti = gi * 4 + t4
            kx = ti % 2
            k = ti % 2
            dma('sp', xt[kx], xsrc[s, ti * 128:(ti + 1) * 128, :], r=[], w=[('xt', kx)])
            act(junk, xt[kx], AF.Square, r=[('xt', kx)], w=['junk', ('ss', k)], scale=1.0 / 32, accum_out=ss[k])
            act(rr[k], ss[k], AF.Sqrt, r=[('ss', k)], w=[('rr', k)], bias=EPS, scale=1.0)
            recip(rr[k], rr[k], r=[('rr', k)], w=[('rr', k)])
            act(xs[k], xt[kx], AF.Copy, r=[('xt', kx), ('rr', k)], w=[('xs', k)], scale=rr[k])

        def tf_b(gi, t4):
            nb = gi % 2
            k = (gi * 4 + t4) % 2
            for c in range(8):
                tr(PBa[:, c * 128:(c + 1) * 128], xs[k][:, c * 128:(c + 1) * 128], r=[('xs', k)], w=['PB'])
            tt('dve', nTb[nb][:, :, t4 * 128:(t4 + 1) * 128], PBa.rearrange("p (c t) -> p c t", c=8),
               g_mix.unsqueeze(2).to_broadcast([128, 8, 128]), ALU.mult, r=['PB', 'g_mix'], w=[('nT', nb)])

        def tf_c(gi, t4):
            nb = gi % 2
            ti = gi * 4 + t4
            for kc in range(8):
                mm(P[4][:, 0:280], lhsT=nTb[nb][:, kc, t4 * 128:(t4 + 1) * 128], rhs=wtm[:, kc, :], start=(kc == 0),
                   stop=(kc == 7), r=[('nT', nb), 'wtm'], w=[('P', 4)])
            act(vs_ext[:, ti, :, 0:64], P[4][:, 0:128].rearrange("p (h d) -> p h d", h=2), AF.Copy, r=[('P', 4)], w=['vs_ext'])
            act(vw_ext[:, ti, :, 0:64], P[4][:, 128:256].rearrange("p (h d) -> p h d", h=2), AF.Copy, r=[('P', 4)], w=['vw_ext'])
            act(gates[:, ti, :], P[4][:, 256:280], AF.Sigmoid, r=[('P', 4)], w=['gates'])

        def tile_front(gi, t4):
            tf_a(gi, t4); tf_b(gi, t4); tf_c(gi, t4)

        pend = []

        def front_stages(gi):
            st = []
            order = [('a', 0), ('a', 1), ('b', 0), ('a', 2), ('c', 0), ('b', 1), ('a', 3), ('c', 1), ('b', 2), ('c', 2), ('b', 3), ('c', 3)]
            fn = {'a': tf_a, 'b': tf_b, 'c': tf_c}
            for (kind, t4) in order:
                st.append((fn[kind], gi, t4))
            return st

        for t4 in range(4):
            tile_front(0, t4)
        for nm, tl in (('cosT', cosT), ('sinT', sinT), ('coscT', coscT), ('sincT', sincT), ('maskc', maskc),
                       ('selbias', selbias), ('eall', eall), ('rblk', rblk), ('causal', causal), ('lower', lower),
                       ('ov', ovt)):
            dma('sp', tl, dr[nm], r=[], w=[nm])
        for kvh in range(2):
            cp('pool', vc_ext[0:127, kvh, 65:97], ovt[0:127, :], r=['ov'], w=['vc_ext'])
        for gi in range(CFG['nga']):
            g0 = gi * 512
            fmk['nb'] = gi % 2
            fmk['gi'] = gi
            while pend:
                fn_, g_, t_ = pend.pop(0)
                fn_(g_, t_)
            if gi + 1 < CFG['nga']:
                pend.extend(front_stages(gi + 1))
            tsl = slice(g0, g0 + 512)
            if CFG['a_stage'] < 1:
                continue
            for j in range(4):
                kb = j % 2
                pk = fm_chunk([(128 * j, 128)])
                act(xin_sb[kb], P[pk], AF.Copy, r=[('P', pk)], w=[('xin', kb)])
                pk = fm_chunk([(1024 + 128 * j, 128)])
                tt('dve', zb[kb][:, 2:514], P[pk], xin_sb[kb], ALU.mult, r=[('P', pk), ('xin', kb)], w=[('zb', kb)])
                cp('dve', zb[kb][:, 0:2], zh[:, j, :], r=['zh'], w=[('zb', kb)])
                pk = fm_chunk([(512 + 128 * j, 128)])
                act(accb[kb], zb[kb][:, 0:512], AF.Copy, r=[('zb', kb), 'cw'], w=[('acc', kb)], scale=cw[:, 0, j:j + 1])
                stt('dve', accb[kb], zb[kb][:, 1:513], cw[:, 1, j:j + 1], accb[kb], ALU.mult, ALU.add, r=[('zb', kb), 'cw', ('acc', kb)], w=[('acc', kb)])
                stt('dve', accb[kb], zb[kb][:, 2:514], cw[:, 2, j:j + 1], accb[kb], ALU.mult, ALU.add, r=[('zb', kb), 'cw', ('acc', kb)], w=[('acc', kb)])
                cp('dve', zh[:, j, :], zb[kb][:, 512:514], r=[('zb', kb)], w=['zh'])
                tt('dve', ycv[:, j, :], P[pk], accb[kb], ALU.mult, r=[('P', pk), ('acc', kb)], w=[('ycv', j)])
            for j in range(4):
                act(ysq[:, j, :], ycv[:, j, :], AF.Square, r=[('ycv', j)], w=[('ysq', j)])
            for j in range(4):
                mm(P[3], lhsT=ones, rhs=ysq[:, j, :], start=(j == 0), stop=(j == 3), r=['ones', ('ysq', j)], w=[('P', 3)])
            act(rc, P[3], AF.Sqrt, r=[('P', 3)], w=['rc'], bias=EPS, scale=1.0 / 512)
            recip(rc, rc, r=['rc'], w=['rc'])
            for j in range(4):
                stt('dve', mixedT[:, j, tsl], ycv[:, j, :], g_gc[:, j:j + 1], rc, ALU.mult, ALU.mult,
                    r=[('ycv', j), 'g_gn_conv', 'rc'], w=['mixedT'])
            if CFG['a_stage'] < 2:
                continue
            for c in range(4):
                pk = fm_chunk([(1536 + 64 * c, 64), (1536 + 64 * (4 + c), 64)])
                rope(pk, qT[:, c, tsl], 'qT', tsl, cosT[:, tsl], sinT[:, tsl], ('cosT', 'sinT'))
            if CFG['a_stage'] < 3:
                continue
            pk = fm_chunk([(2304, 128)])
            rope(pk, ksT[:, tsl], 'ksT', tsl, cosT[:, tsl], sinT[:, tsl], ('cosT', 'sinT'))
            pk = fm_chunk([(2560, 128)])
            rope(pk, kwT[:, tsl], 'kwT', tsl, cosT[:, tsl], sinT[:, tsl], ('cosT', 'sinT'))
            pk = fm_chunk([(2048, 128)])
            act(kcmpT[:, tsl], P[pk], AF.Copy, r=[('P', pk)], w=['kcmpT'])
            pk = fm_chunk([(2176, 128)])
            act(vcmpT[:, tsl], P[pk], AF.Copy, r=[('P', pk)], w=['vcmpT'])

        for which, srcT, sres in ((('k', kcmpT, 'kcmpT'), ('v', vcmpT, 'vcmpT')) if CFG['do_b'] else ()):
            w1 = dr['cmp_w1_' + which]; w2 = dr['cmp_w2_' + which]; pe = dr['cmp_pe_' + which]
            memset('pool', w1blk, 0.0, w=['w1blk'])
            memset('pool', w2blk, 0.0, w=['w2blk'])
            w1v = w1.rearrange("(j d) h -> d j h", d=64)
            dma('pool', w1blk[0:64, :, 0:64], w1v, r=[], w=['w1blk'])
            dma('pool', w1blk[64:128, :, 64:128], w1v, r=[], w=['w1blk'])
            dma('pool', w2blk[0:64, 0:64], w2, r=[], w=['w2blk'])
            dma('pool', w2blk[64:128, 64:128], w2, r=[], w=['w2blk'])
            w1c = w1.rearrange("(i p) h -> p i h", p=128)
            dma('pool', w1d[:, :, 0:64], w1c, r=[], w=['w1d'])
            dma('pool', w1d[:, :, 64:128], w1c, r=[], w=['w1d'])
            dma('pool', pecol, pe.rearrange("(i a) d -> (a d) i", a=2), r=[], w=['pecol'],
                allow_slow_non_contiguous=True)
            for i in range(16):
                mm(P[6][:, 0:1], lhsT=w1d[:, i, :], rhs=pecol[:, i:i + 1], start=(i == 0), stop=(i == 15),
                   r=['w1d', 'pecol'], w=[('P', 6)])
            act(c1, P[6][:, 0:1], AF.Copy, r=[('P', 6)], w=['c1'])
            for j in range(32):
                mm(P[5][:, 0:127], lhsT=w1blk[:, j, :], rhs=srcT[:, j:j + 2017:16], start=(j == 0), stop=(j == 31),
                   r=['w1blk', sres], w=[('P', 5)])
            act(hidT[:, 0:127], P[5][:, 0:127], AF.Silu, r=[('P', 5), 'c1'], w=['hidT'], bias=c1)
            if which == 'k':
                mm(P[6][:, 0:127], lhsT=w2blk, rhs=hidT[:, 0:127], start=True, stop=True, r=['w2blk', 'hidT'], w=[('P', 6)])
                act(kcraw[:, 0:127], P[6][:, 0:127], AF.Copy, r=[('P', 6)], w=['kcraw'])
                mm(P[3][:, 0:127], lhsT=rblk, rhs=kcraw[:, 0:127], start=True, stop=True, r=['rblk', 'kcraw'], w=[('P', 3)])
                tt('dve', t1[0][:, 0:127], P[6][:, 0:127], coscT, ALU.mult, r=[('P', 6), 'coscT'], w=[('t1', 0)])
                tt('dve', t2[0][:, 0:127], P[3][:, 0:127], sincT, ALU.mult, r=[('P', 3), 'sincT'], w=[('t2', 0)])
                tt('dve', kcT[:, 0:127], t1[0][:, 0:127], t2[0][:, 0:127], ALU.add, r=[('t1', 0), ('t2', 0)], w=['kcT'])
            else:
                mm(P[6][0:127, 0:128], lhsT=hidT[:, 0:127], rhs=w2blk, start=True, stop=True, r=['w2blk', 'hidT'], w=[('P', 6)])
                act(vc_ext[0:127, :, 0:64], P[6][0:127, 0:128].rearrange("p (h d) -> p h d", h=2), AF.Copy,
                    r=[('P', 6)], w=['vc_ext'])
        if s == 0:
            cast_w('w_out', 1024, gate=['kcT'])
            cast_w('w_up', 512, rsplit=2, gate=['kcT'])
            cast_w('w_down', 1024, gate=['kcT'])
            cast_w('w_ple_gate', 1024, gate=['kcT'])
            cast_w('w_ple_proj', 1024, gate=['kcT'])
        dump('qT', qT[:, :, 0:512], r=['qT'])
        dump('kcT', kcT[:, 0:127], r=['kcT'])
        dump('mixc', mixedT[:, 0:4, 0:512], r=['mixedT'])

        sk = {'s': 0, 'p': 0, 'u': 0}
        for i in range(CFG['nqt']):
            qsl = slice(i * 128, (i + 1) * 128)
            for kvh in range(2):
                hp = slice(0, 64) if kvh == 0 else slice(64, 128)
                q_rhs = qT[hp, :, qsl]

                def score_tile(kT_l, extra, kparts=128):
                    b = sk['s'] % 3; sk['s'] += 1
                    kp = sk['p'] % 4; sk['p'] += 1
                    n_ex = len(extra)
                    o3 = P[b][0:kparts, :].rearrange("p (h q) -> p h q", h=4)
                    mm(o3, lhsT=kT_l[0], rhs=q_rhs, start=True, stop=(n_ex == 0), r=[kT_l[1], 'qT'], w=[('P', b)])
                    for xi, (l, rhs_, rres) in enumerate(extra):
                        mm(o3, lhsT=l, rhs=rhs_, start=False, stop=(xi == n_ex - 1), r=rres, w=[('P', b)])
                    act(pT[kp][0:kparts, :], P[b][0:kparts, :], AF.Exp, r=[('P', b)], w=[('pT', kp)], scale=0.125)
                    return kp

                gv = gates[:, i, :].rearrange("p (h b) -> p h b", b=3)
                yv = yat[:, kvh * 4:(kvh + 1) * 4, :]
                ccc = sm[:, 8:12]; ccs = sm[:, 12:16]; ccw = sm[:, 16:20]
                owv = P[5][:, 0:260].rearrange("p (h c) -> p h c", h=4)
                ocb = 3 if (sk['u'] % 2 == 0) else 6
                sk['u'] += 1
                oc = P[ocb][:, 0:388].rearrange("p (h c) -> p h c", h=4)
                rdc = sm[:, 0:4]
                nsb = negselT.unsqueeze(1).to_broadcast([32, 4, 128])
                cb = causal.unsqueeze(1).to_broadcast([128, 4, 128])
                lb = lower.unsqueeze(1).to_broadcast([128, 4, 128])
                mk = maskc[0:127, i, :].unsqueeze(1).to_broadcast([127, 4, 128])

                def pv_c(kp):
                    for hb in range(4):
                        mm(P[ocb][:, hb * 97:(hb + 1) * 97], lhsT=pT[kp][0:127, hb * 128:(hb + 1) * 128], rhs=vc_ext[0:127, kvh, :],
                           start=True, stop=True, r=[('pT', kp), 'vc_ext'], w=[('P', ocb)])
                    ts('dve', rdc, oc[:, :, 64], 1e-30, None, ALU.max, None, r=[('P', ocb)], w=['rdc'])
                    recip(rdc, rdc, r=['rdc'], w=['rdc'])
                    ts('dve', score, oc[:, 0, 65:97], rdc[:, 0:1], None, ALU.mult, None, r=[('P', ocb), 'rdc'], w=['score'])
                    for hb in range(1, 4):
                        stt('dve', score, oc[:, hb, 65:97], rdc[:, hb:hb + 1], score, ALU.mult, ALU.add,
                            r=[('P', ocb), 'rdc', 'score'], w=['score'])
                    tt('dve', score, score, selbias[:, i, :], ALU.add, r=['score', 'selbias'], w=['score'])
                    S.add('dve', lambda e: e.max(out=top8, in_=score), r=['score'], w=['top8'])
                    ts('dve', score, score, top8[:, 7:8], None, ALU.is_ge, None, r=['score', 'top8'], w=['score'])
                    ts('dve', negsel, score, 1.0, -NEGB, ALU.subtract, ALU.mult, r=['score'], w=['negsel'])
                    tr(PBa[0:32, 0:128], negsel, r=['negsel'], w=['PB'])
                    cp('dve', negselT, PBa[0:32, 0:128], r=['PB'], w=['negselT'])
                    tt('dve', ccc, rdc, gv[:, kvh * 4:(kvh + 1) * 4, 0], ALU.mult, r=['rdc', 'gates'], w=['ccc'])
                    tt('dve', yv, oc[:, :, 0:64], ccc.unsqueeze(2).to_broadcast([128, 4, 64]), ALU.mult, r=[('P', ocb), 'ccc'], w=['yat'])

                def win_done():
                    ts('dve', ccw, owv[:, :, 64], 1e-30, None, ALU.max, None, r=[('P', 5)], w=['ccw'])
                    recip(ccw, ccw, r=['ccw'], w=['ccw'])
                    tt('dve', ccw, ccw, gv[:, kvh * 4:(kvh + 1) * 4, 2], ALU.mult, r=['ccw', 'gates'], w=['ccw'])
                    tt('dve', tmpw, owv[:, :, 0:64], ccw.unsqueeze(2).to_broadcast([128, 4, 64]), ALU.mult, r=[('P', 5), 'ccw'], w=['tmpw'])
                    tt('dve', yv, yv, tmpw, ALU.add, r=['yat', 'tmpw'], w=['yat'])

                def mk_pv(bank, vext, vres, j, first, last):
                    def pv(kp):
                        for hb in range(4):
                            mm(P[bank][:, hb * 65:(hb + 1) * 65], lhsT=pT[kp][:, hb * 128:(hb + 1) * 128], rhs=vext[:, j, kvh, :],
                               start=(first and hb == 0), stop=(hb == 3), r=[('pT', kp), vres], w=[('P', bank)],
                               skip_group_check=True)
                        if last and bank == 5:
                            win_done()
                    return pv

                tasks = [(((kcT[hp, 0:127], 'kcT'), [(ident[0:127, 0:127], mk, ['ident', 'maskc'])], 127), pv_c)]
                j0 = max(0, i - 4)
                for j in range(j0, i + 1):
                    ex = []
                    if j == i:
                        ex.append((ident, cb, ['ident', 'causal']))
                    if j == i - 4:
                        ex.append((ident, lb, ['ident', 'lower']))
                    tasks.append((((kwT[hp, j * 128:(j + 1) * 128], 'kwT'), ex, 128), mk_pv(5, vw_ext, 'vw_ext', j, j == j0, j == i)))
                for j in range(i + 1):
                    ex = [(eall[:, j, :], nsb, ['eall', 'negselT'])]
                    if j == i:
                        ex.append((ident, cb, ['ident', 'causal']))
                    tasks.append((((ksT[hp, j * 128:(j + 1) * 128], 'ksT'), ex, 128), mk_pv(4, vs_ext, 'vs_ext', j, j == 0, j == i)))
                prev = None
                for (sargs, pvf) in tasks:
                    kp = score_tile(sargs[0], sargs[1], kparts=sargs[2])
                    if prev is not None:
                        prev[0](prev[1])
                    prev = (pvf, kp)
                prev[0](prev[1])
                osv = P[4][:, 0:260].rearrange("p (h c) -> p h c", h=4)
                ts('dve', ccs, osv[:, :, 64], 1e-30, None, ALU.max, None, r=[('P', 4)], w=['ccs'])
                recip(ccs, ccs, r=['ccs'], w=['ccs'])
                tt('dve', ccs, ccs, gv[:, kvh * 4:(kvh + 1) * 4, 1], ALU.mult, r=['ccs', 'gates'], w=['ccs'])
                tt('dve', tmpa, osv[:, :, 0:64], ccs.unsqueeze(2).to_broadcast([128, 4, 64]), ALU.mult, r=[('P', 4), 'ccs'], w=['tmpa'])
                tt('dve', yv, yv, tmpa, ALU.add, r=['yat', 'tmpa'], w=['yat'])
            k = cnt['n'] % 2; cnt['n'] += 1
            yflat = yat.rearrange("p h d -> p (h d)")
            if i == 0:
                dump('yat0', yflat, r=['yat'])
            act(junk[:, 0:512], yflat, AF.Square, r=['yat'], w=['junk', ('ss', k)], scale=float(512 ** -0.5), accum_out=ss[k])
            act(rr[k], ss[k], AF.Sqrt, r=[('ss', k)], w=[('rr', k)], bias=EPS, scale=1.0)
            recip(rr[k], rr[k], r=[('rr', k)], w=[('rr', k)])
            act(yab, yflat, AF.Copy, r=['yat', ('rr', k)], w=['yab'], scale=rr[k])
            for c in range(4):
                tr(PBa[:, c * 128:(c + 1) * 128], yab[:, c * 128:(c + 1) * 128], r=['yab'], w=['PB'])
            tt('dve', mixedT[:, 4:8, qsl], PBa[:, 0:512].rearrange("p (c t) -> p c t", c=4),
               g_ga.unsqueeze(2).to_broadcast([128, 4, 128]), ALU.mult, r=['PB', 'g_gn_attn'], w=['mixedT'])
        dump('mixa', mixedT[:, 4:8, 0:512], r=['mixedT'])

    def phase2(s):
        state['off'] = base_off
        hsets = [[carve([128, D], F32) for _ in range(4)] for _ in range(2)]
        actT = carve([128, NF, 512], BF16)
        abuf = [carve([128, 514], F32) for _ in range(4)]
        ub = [carve([128, 512], F32) for _ in range(4)]
        sg = [carve([128, 512], F32) for _ in range(2)]
        wub = [carve([128, 8, 256], BF16) for _ in range(4)]
        wdb = [carve([128, 512], BF16) for _ in range(4)]
        w_out = carve([128, 8, D], BF16)
        wpg = carve([128, 8, D], BF16)
        wpp = carve([128, 2, D], BF16)
        gfin = carve([128, D], F32)
        pt = [carve([128, 256], F32) for _ in range(2)]
        pbf = [carve([128, 256], BF16) for _ in range(2)]
        ppT = carve([128, 2, 512], BF16)
        sgm = [carve([128, 512], F32) for _ in range(2)]
        tmp = [carve([128, 512], F32) for _ in range(2)]
        hal = carve([128, 2 * NF, 2], F32)
        cwf = carve([128, 3, 2 * NF], F32)

        dma('sp', w_out, wbf['w_out'].rearrange("(c p) n -> p c n", p=128), r=['w_out_bf'], w=['w_out'])
        dma('sp', wpg, wbf['w_ple_gate'].rearrange("(c p) n -> p c n", p=128), r=['w_ple_gate_bf'], w=['wpg'])
        dma('sp', wpp, wbf['w_ple_proj'].rearrange("(c p) n -> p c n", p=128), r=['w_ple_proj_bf'], w=['wpp'])
        dma('sp', gfin, dr['g_final'].partition_broadcast(128), r=[], w=['gfin'])
        for k3 in range(3):
            dma('sp', cwf[:, k3, :], dr['w_ffn_conv'][k3].rearrange("(f p) -> p f", p=128), r=[], w=['cwf'], allow_slow_non_contiguous=True)
        memset('pool', hal, 0.0, w=['hal'])
        w_up_v = wbf['w_up'].rearrange("(c p) n -> p c n", p=128)
        w_dn_v = wbf['w_down'].rearrange("(f p) n -> p f n", p=128)
        ck = {'u': 0, 'd': 0, 'pp': 0, 'ab': 0, 'o': 0, 'sg': 0}

        def up_head(f):
            k = ck['u'] % 4; ck['u'] += 1
            dma('sp', wub[k][:, :, 0:128], w_up_v[:, :, f * 128:(f + 1) * 128], r=['w_up_bf'], w=[('wub', k)])
            dma('sp', wub[k][:, :, 128:256], w_up_v[:, :, DFF + f * 128:DFF + (f + 1) * 128], r=['w_up_bf'], w=[('wub', k)])
            pg = (ck['pp'] % 3) * 2; ck['pp'] += 1
            us = []
            for hv in range(2):
                pk = pg + hv
                for kc in range(8):
                    mm(P[pk], lhsT=wub[k][:, kc, hv * 128:(hv + 1) * 128], rhs=nT[:, kc, :], start=(kc == 0), stop=(kc == 7),
                       r=[('wub', k), ('nT', 0)], w=[('P', pk)])
                a = ck['ab'] % 4; ck['ab'] += 1
                us.append((a, hv * NF + f, pk))
            for (a, fi, pk) in us:
                act(abuf[a][:, 2:514], P[pk], AF.Copy, r=[('P', pk)], w=[('abuf', a)])
                cp('dve', abuf[a][:, 0:2], hal[:, fi, :], r=['hal'], w=[('abuf', a)])
            return us

        def up_mid(us):
            for (a, fi, pk) in us:
                act(ub[a], abuf[a][:, 0:512], AF.Copy, r=[('abuf', a), 'cwf'], w=[('ub', a)], scale=cwf[:, 0, fi:fi + 1])
                cp('dve', hal[:, fi, :], abuf[a][:, 512:514], r=[('abuf', a)], w=['hal'])
            for tap in (1, 2):
                for (a, fi, pk) in us:
                    stt('dve', ub[a], abuf[a][:, tap:tap + 512], cwf[:, tap, fi:fi + 1], ub[a], ALU.mult, ALU.add,
                        r=[('abuf', a), 'cwf', ('ub', a)], w=[('ub', a)])

        def up_tail(f, us):
            q = ck['sg'] % 2; ck['sg'] += 1
            act(sg[q], ub[us[0][0]], AF.Silu, r=[('ub', us[0][0])], w=[('sg', q)])
            tt('dve', actT[:, f, :], sg[q], ub[us[1][0]], ALU.mult, r=[('sg', q), ('ub', us[1][0])], w=['actT'])

        pend2 = []

        def pop2():
            if pend2:
                pend2.pop(0)()

        def tail_stages(gi, h):
            g0 = gi * 512

            def s1(t4):
                def f():
                    tok = slice(g0 + t4 * 128, g0 + (t4 + 1) * 128)
                    rmsnorm_T(h[t4], ('h', gi % 2, t4), g_ple, 'g_ple', slice(t4 * 128, (t4 + 1) * 128), nb=1, nres=('nT1', t4))
                    kp = t4 % 2
                    dma('sp', pt[kp], dr['p'][s, tok, :], r=[], w=[('pt', kp)])
                    act(pbf[kp], pt[kp], AF.Copy, r=[('pt', kp)], w=[('pbf', kp)])
                    for c in range(2):
                        tr(PBa[:, c * 128:(c + 1) * 128], pbf[kp][:, c * 128:(c + 1) * 128], r=[('pbf', kp)], w=['PB'])
                    cp('dve', ppT[:, :, t4 * 128:(t4 + 1) * 128], PBa[:, 0:256].rearrange("p (c t) -> p c t", c=2), r=['PB'], w=[('ppT', t4)])
                return f

            def s2(t4, half):
                def f():
                    pk = (ck['pp'] % 3) * 2; ck['pp'] += 1
                    cs = slice(half * 512, (half + 1) * 512)
                    for kc in range(8):
                        mm(P[pk], lhsT=nTb[1][:, kc, t4 * 128:(t4 + 1) * 128], rhs=wpg[:, kc, cs], start=(kc == 0), stop=(kc == 7),
                           r=[('nT1', t4), 'wpg'], w=[('P', pk)])
                    for kc in range(2):
                        mm(P[pk + 1], lhsT=ppT[:, kc, t4 * 128:(t4 + 1) * 128], rhs=wpp[:, kc, cs], start=(kc == 0), stop=(kc == 1),
                           r=[('ppT', t4), 'wpp'], w=[('P', pk + 1)])
                    q = ck['sg'] % 2; ck['sg'] += 1
                    act(sgm[q], P[pk], AF.Sigmoid, r=[('P', pk)], w=[('sgm', q)])
                    tt('dve', tmp[q], P[pk + 1], sgm[q], ALU.mult, r=[('P', pk + 1), ('sgm', q)], w=[('tmp', q)])
                    tt('dve', h[t4][:, cs], h[t4][:, cs], tmp[q], ALU.add, r=[('h', gi % 2, t4), ('tmp', q)], w=[('h', gi % 2, t4)])
                return f

            def s3(t4):
                def f():
                    tok = slice(g0 + t4 * 128, g0 + (t4 + 1) * 128)
                    k = cnt['n'] % 2; cnt['n'] += 1
                    hr = ('h', gi % 2, t4)
                    act(junk, h[t4], AF.Square, r=[hr], w=['junk', ('ss', k)], scale=1.0 / 32, accum_out=ss[k])
                    act(rr[k], ss[k], AF.Sqrt, r=[('ss', k)], w=[('rr', k)], bias=EPS, scale=1.0)
                    recip(rr[k], rr[k], r=[('rr', k)], w=[('rr', k)])
                    stt('dve', h[t4], h[t4], rr[k], gfin, ALU.mult, ALU.mult, r=[hr, ('rr', k), 'gfin'], w=[hr])
                    dma('sp', out[s, tok, :], h[t4], r=[hr], w=[])
                return f

            return [s1(0), s1(1), s2(0, 0), s2(0, 1), s1(2), s3(0), s2(1, 0), s2(1, 1), s1(3), s3(1),
                    s2(2, 0), s2(2, 1), s3(2), s2(3, 0), s2(3, 1), s3(3)]

        for gi in range(CFG['ng2']):
            g0 = gi * 512
            h = hsets[gi % 2]
            for t4 in range(4):
                tok = slice(g0 + t4 * 128, g0 + (t4 + 1) * 128)
                hr = ('h', gi % 2, t4)
                dma('sp', h[t4], dr['x'][s, tok, :], r=[], w=[hr])
                for half in range(2):
                    pk = ck['pp'] % 4; ck['pp'] += 1
                    for kc in range(8):
                        mm(P[pk], lhsT=mixedT[:, kc, tok], rhs=w_out[:, kc, half * 512:(half + 1) * 512], start=(kc == 0),
                           stop=(kc == 7), r=['mixedT', 'w_out'], w=[('P', pk)])
                    hs = h[t4][:, half * 512:(half + 1) * 512]
                    tt('dve', hs, P[pk], hs, ALU.add, r=[('P', pk), hr], w=[hr])
                if t4 >= 1:
                    rmsnorm_T(h[t4 - 1], ('h', gi % 2, t4 - 1), g_ffn, 'g_ffn', slice((t4 - 1) * 128, t4 * 128))
            rmsnorm_T(h[3], ('h', gi % 2, 3), g_ffn, 'g_ffn', slice(3 * 128, 4 * 128))
            hist = {}
            for f in range(NF + 2):
                if f < NF:
                    hist[f] = up_head(f)
                if 0 <= f - 1 < NF:
                    up_mid(hist[f - 1])
                if 0 <= f - 2 < NF:
                    up_tail(f - 2, hist[f - 2])
                pop2()
            while pend2:
                pop2()
            for half in range(2):
                banks = [0, 1, 2, 3] if half == 0 else [4, 5, 6, 0]
                for f in range(NF):
                    k = ck['d'] % 4; ck['d'] += 1
                    dma('sp', wdb[k], w_dn_v[:, f, half * 512:(half + 1) * 512], r=['w_down_bf'], w=[('wdb', k)])
                    for t4 in range(4):
                        mm(P[banks[t4]], lhsT=actT[:, f, t4 * 128:(t4 + 1) * 128], rhs=wdb[k], start=(f == 0), stop=True,
                           r=['actT', ('wdb', k)], w=[('P', banks[t4])], skip_group_check=True)
                for t4 in range(4):
                    hs = h[t4][:, half * 512:(half + 1) * 512]
                    tt('dve', hs, P[banks[t4]], hs, ALU.add, r=[('P', banks[t4]), ('h', gi % 2, t4)], w=[('h', gi % 2, t4)])
            pend2.extend(tail_stages(gi, h))
        while pend2:
            pop2()

    for s in range(CFG['nseq']):
        phase1(s)
        S.barrier()
        if CFG['do_p2']:
            phase2(s)
            S.barrier()
    S.run()
    es.close()
    return nc


_CACHE = {}


def kernel(**inputs):
    ncores = 8
    consts = host_consts()
    if 'nc' not in _CACHE:
        _CACHE['nc'] = build_program()
    nc = _CACHE['nc']
    x = np.ascontiguousarray(np.asarray(inputs['x'], dtype=np.float32))
    p = np.ascontiguousarray(np.asarray(inputs['p'], dtype=np.float32))
    in_maps = []
    for c in range(ncores):
        m = {'x': x[NSEQ * c:NSEQ * (c + 1)], 'p': p[0, NSEQ * c:NSEQ * (c + 1)]}
        for k in WSHAPES:
            a = np.asarray(inputs[k], dtype=np.float32)
            if k != 'g_final':
                a = a[0]
            m[k] = np.ascontiguousarray(a)
        for k, v in consts.items():
            m['c_' + k] = v
        in_maps.append(m)
    res = run_bass_kernel_spmd(nc, in_maps, core_ids=list(range(ncores)))
    outs = [np.asarray(r['out'], dtype=np.float32) for r in res.results]
    return np.concatenate(outs, axis=0)
```

```python
import numpy as np
import ml_dtypes
from contextlib import ExitStack
import concourse.bass as bass
import concourse.mybir as mybir
from concourse.bass_utils import run_bass_kernel_spmd

F32 = mybir.dt.float32
BF16 = mybir.dt.bfloat16
U8 = mybir.dt.uint8
ALU = mybir.AluOpType
AF = mybir.ActivationFunctionType

ENGS = ['pe', 'act', 'dve', 'pool', 'sp']
EPOCH = 12000
NDSEM = 16

D = 1024
S_LEN = 2048
NSEQ = 2
NT = 16
DFF = 2816
NF = 22
NEGB = -30000.0
EPS = 1e-6
CFG = dict(tilebar=False, ntile=4, tm=True, a_stage=9, nseq=2, nga=4, do_b=True, nqt=16, do_p2=True, ng2=4)


class Op:
    __slots__ = ('eng', 'emit', 'deps', 'id', 'sig', 'dma', 'comp', 'dk')


class Sched:
    def __init__(self, nc):
        self.nc = nc
        self.ops = []
        self.last_w = {}
        self.readers = {}
        self.eng_list = {e: [] for e in ENGS}
        self.ndma = {e: 0 for e in ENGS}

    def add(self, eng, emit, r=(), w=(), dma=False):
        op = Op()
        op.eng = eng; op.emit = emit; op.id = len(self.ops); op.dma = dma
        op.sig = False; op.comp = None; op.dk = None
        deps = set()
        rw = set()
        for res in r:
            if res in self.last_w:
                deps.add(self.last_w[res]); rw.add(self.last_w[res])
            if res == 'PB' or (isinstance(res, tuple) and res[0] == 'P'):
                for x in self.readers.get(res, ()):
                    if self.ops[x].eng != eng:
                        deps.add(x)
        for res in w:
            if res in self.last_w:
                deps.add(self.last_w[res])
            deps.update(self.readers.get(res, ()))
        fd = set()
        for d in deps:
            dop = self.ops[d]
            if (not dma) and (not dop.dma) and dop.eng == eng:
                if eng == 'pe':
                    continue
            fd.add(d)
        op.deps = fd
        for res in r:
            lst = self.readers.setdefault(res, [])
            if not dma:
                lst[:] = [x for x in lst if self.ops[x].dma or self.ops[x].eng != eng]
            lst.append(op.id)
        for res in w:
            self.last_w[res] = op.id
            self.readers[res] = []
        if dma:
            op.dk = self.ndma[eng]
            self.ndma[eng] += 1
        self.ops.append(op)
        self.eng_list[eng].append(op)
        return op

    def barrier(self):
        n = len(self.ops)
        for e in ENGS:
            self.add(e, None, r=(), w=[('bar', n, e)])
        lastd = {}
        for op in self.ops:
            if op.dma:
                lastd[(op.eng, op.dk % NDSEM)] = op.id
        for e in ENGS:
            w = self.add(e, None, r=[('bar', n, e2) for e2 in ENGS], w=())
            w.deps.update(lastd.values())
        self.last_w.clear()
        self.readers.clear()

    def finalize(self):
        for op in self.ops:
            for d in op.deps:
                self.ops[d].sig = True
        self.nsem_eng = {}
        for e in ENGS:
            c = 0
            for op in self.eng_list[e]:
                if op.dma:
                    continue
                if op.sig:
                    c += 1
                    op.comp = ('c', e, (c - 1) // EPOCH, (c - 1) % EPOCH + 1)
            self.nsem_eng[e] = (c + EPOCH - 1) // EPOCH if c else 0
        for op in self.ops:
            if op.dma:
                op.comp = ('d', op.eng, op.dk % NDSEM, 16 * (op.dk // NDSEM + 1))

    def run(self):
        nc = self.nc
        self.finalize()
        es = ExitStack()
        sems = {}
        for e in ENGS:
            for i in range(self.nsem_eng[e]):
                sems[('c', e, i)] = es.enter_context(nc.semaphore(f"s_{e}_{i}"))
            for i in range(min(NDSEM, self.ndma[e])):
                sems[('d', e, i)] = es.enter_context(nc.semaphore(f"d_{e}_{i}"))
        block = es.enter_context(nc.Block())
        sched = self

        def body(ename):
            def f(eng):
                waited = {}
                cnt = {}
                for op in sched.eng_list[ename]:
                    need = {}
                    for d in op.deps:
                        k = sched.ops[d].comp
                        need[k[:3]] = max(need.get(k[:3], 0), k[3])
                    if op.dma and op.dk >= NDSEM:
                        key = ('d', ename, op.dk % NDSEM)
                        need[key] = max(need.get(key, 0), 16 * (op.dk // NDSEM))
                    for key, val in need.items():
                        if waited.get(key, 0) >= val:
                            continue
                        waited[key] = val
                        eng.wait_ge(sems[key], val)
                    if op.emit is None:
                        if op.sig:
                            eng.drain().then_inc(sems[op.comp[:3]], 1)
                        continue
                    ins = op.emit(eng)
                    if op.dma:
                        ins.then_inc(sems[op.comp[:3]], 16)
                        cnt[op.dk % NDSEM] = cnt.get(op.dk % NDSEM, 0) + 1
                    elif op.sig:
                        ins.then_inc(sems[op.comp[:3]], 1)
                for i, c in cnt.items():
                    key = ('d', ename, i)
                    if waited.get(key, 0) < 16 * c:
                        eng.wait_ge(sems[key], 16 * c)
            return f

        block.tensor(body('pe'))
        block.scalar(body('act'))
        block.vector(body('dve'))
        block.gpsimd(body('pool'))
        block.sync(body('sp'))
        es.close()


def host_consts():
    bf = ml_dtypes.bfloat16
    c = {}
    c['ident'] = np.eye(128, dtype=np.float32).astype(bf)
    k = np.arange(128)[:, None]; m = np.arange(128)[None, :]
    c['rblk'] = ((k // 64 == m // 64) & (k % 64 == (m % 64 + 32) % 64)).astype(np.float32).astype(bf)
    inv = (10000.0 ** (-np.arange(0, 64, 2, dtype=np.float32) / 64)).astype(np.float32)
    pr = np.arange(128) % 64
    fr = inv[pr % 32][:, None]
    sgn = np.where(pr < 32, -1.0, 1.0)[:, None].astype(np.float32)
    pos = np.arange(S_LEN, dtype=np.float32)[None, :]
    ang = (pos * fr).astype(np.float32)
    c['cosT'] = np.cos(ang).astype(np.float32).astype(bf)
    c['sinT'] = (np.sin(ang) * sgn).astype(np.float32).astype(bf)
    posc = (np.arange(127, dtype=np.float32) * 16 + 31)[None, :]
    angc = (posc * fr).astype(np.float32)
    c['coscT'] = np.cos(angc).astype(np.float32).astype(bf)
    c['sincT'] = (np.sin(angc) * sgn).astype(np.float32).astype(bf)
    cc = np.arange(128)[:, None, None]; ii = np.arange(16)[None, :, None]; qq = np.arange(128)[None, None, :]
    c['maskc'] = np.where(16 * cc + 31 <= 128 * ii + qq, 0.0, NEGB).astype(np.float32).astype(bf)
    qq2 = np.arange(128)[:, None, None]; ii2 = np.arange(16)[None, :, None]; nn = np.arange(32)[None, None, :]
    t = 128 * ii2 + qq2
    cur = t // 64
    valid = nn * 64 <= t
    forced = (nn == 0) | (nn == cur) | (nn == cur - 1)
    c['selbias'] = np.where(valid, np.where(forced, 1e4, 0.0), -1e30).astype(np.float32)
    b = np.arange(128)[:, None, None]; jj = np.arange(16)[None, :, None]; p = np.arange(128)[None, None, :]
    c['eall'] = (b == 2 * jj + p // 64).astype(np.float32).astype(bf)
    kk = np.arange(128)[:, None]; q = np.arange(128)[None, :]
    c['causal'] = np.where(kk > q, NEGB, 0.0).astype(np.float32).astype(bf)
    c['lower'] = np.where(kk <= q, NEGB, 0.0).astype(np.float32).astype(bf)
    cs = np.arange(127)[:, None] * 16; bs = np.arange(32)[None, :] * 64
    ov = np.clip(np.minimum(cs + 32, bs + 64) - np.maximum(cs, bs), 0, None).astype(np.float32) / 32
    c['ov'] = np.concatenate([ov, np.zeros((1, 32), np.float32)], 0).astype(bf)
    c['ones'] = np.ones((128, 128), np.float32).astype(bf)
    return c


CONST_SHAPES = {
    'ident': ([128, 128], BF16), 'rblk': ([128, 128], BF16), 'cosT': ([128, S_LEN], BF16),
    'sinT': ([128, S_LEN], BF16), 'coscT': ([128, 127], BF16), 'sincT': ([128, 127], BF16),
    'maskc': ([128, 16, 128], BF16), 'selbias': ([128, 16, 32], F32), 'eall': ([128, 16, 128], BF16),
    'causal': ([128, 128], BF16), 'lower': ([128, 128], BF16), 'ov': ([128, 32], BF16),
    'ones': ([128, 128], BF16),
}

WSHAPES = {
    'g_mix': [D], 'w_in': [D, 2840], 'w_conv_mix': [3, 512], 'cmp_pe_k': [32, 64], 'cmp_w1_k': [2048, 64],
    'cmp_w2_k': [64, 64], 'cmp_pe_v': [32, 64], 'cmp_w1_v': [2048, 64], 'cmp_w2_v': [64, 64],
    'g_gn_conv': [512], 'g_gn_attn': [512], 'w_out': [D, D], 'g_ffn': [D], 'w_up': [D, 2 * DFF],
    'w_ffn_conv': [3, 2 * DFF], 'w_down': [DFF, D], 'g_ple': [D], 'w_ple_gate': [D, D],
    'w_ple_proj': [256, D], 'g_final': [D],
}


def build_program(debug=None):
    nc = bass.Bass("TRN2", target_bir_lowering=False)
    dr = {}
    dr['x'] = nc.dram_tensor("x", [NSEQ, S_LEN, D], F32, kind="ExternalInput").ap()
    dr['p'] = nc.dram_tensor("p", [NSEQ, S_LEN, 256], F32, kind="ExternalInput").ap()
    for k, shp in WSHAPES.items():
        dr[k] = nc.dram_tensor(k, shp, F32, kind="ExternalInput").ap()
    for k, (shp, dt) in CONST_SHAPES.items():
        dr[k] = nc.dram_tensor("c_" + k, shp, dt, kind="ExternalInput").ap()
    out = nc.dram_tensor("out", [NSEQ, S_LEN, D], F32, kind="ExternalOutput").ap()
    wbf = {}
    for k in ('w_in', 'w_up', 'w_down', 'w_out', 'w_ple_gate', 'w_ple_proj'):
        wbf[k] = nc.dram_tensor(k + "_bf", WSHAPES[k], BF16, kind="Internal").ap()
    dbg_out = {}
    if debug:
        for name, shp in debug.items():
            dbg_out[name] = nc.dram_tensor("dbg_" + name, shp, F32, kind="ExternalOutput").ap()

    es = ExitStack()
    ARENA = 206 * 1024
    arena = es.enter_context(nc.sbuf_tensor("arena", [128, ARENA], U8))
    pbank = [es.enter_context(nc.psum_tensor(f"pb{i}", [128, 512], F32)) for i in range(7)]
    PB = es.enter_context(nc.psum_tensor("pbt", [128, 1024], BF16))
    P = [b[:] for b in pbank]
    PBa = PB[:]

    state = {'off': 0}

    def carve(shape, dt):
        esz = 4 if dt == F32 else 2
        n = int(np.prod(shape[1:]))
        off = state['off']
        nb = (n * esz + 63) // 64 * 64
        assert off + nb <= ARENA, ("SBUF arena overflow", off, nb)
        state['off'] = off + nb
        ap = arena[0:shape[0], off:off + n * esz].bitcast(dt)
        if len(shape) == 3:
            ap = ap.rearrange("p (a b) -> p a b", a=shape[1])
        elif len(shape) == 4:
            ap = ap.rearrange("p (a b c) -> p a b c", a=shape[1], b=shape[2])
        return ap

    S = Sched(nc)

    def mm(o, lhsT, rhs, start, stop, r, w, **kw):
        S.add('pe', lambda e: e.matmul(o, lhsT=lhsT, rhs=rhs, start=start, stop=stop, **kw), r=r, w=w)

    def tr(o, in_, r, w):
        S.add('pe', lambda e: e.transpose(out=o, in_=in_, identity=ident), r=list(r) + ['ident'], w=w)

    def act(o, in_, func, r, w, **kw):
        S.add('act', lambda e: e.activation(out=o, in_=in_, func=func, **kw), r=r, w=w)

    def tt(eng, o, a, b, op, r, w):
        S.add(eng, lambda e: e.tensor_tensor(out=o, in0=a, in1=b, op=op), r=r, w=w)

    def stt(eng, o, a, sc, b, op0, op1, r, w):
        S.add(eng, lambda e: e.scalar_tensor_tensor(out=o, in0=a, scalar=sc, in1=b, op0=op0, op1=op1), r=r, w=w)

    def ts(eng, o, a, s1, s2, op0, op1, r, w):
        if op1 is None:
            S.add(eng, lambda e: e.tensor_scalar(out=o, in0=a, scalar1=s1, scalar2=None, op0=op0), r=r, w=w)
        else:
            S.add(eng, lambda e: e.tensor_scalar(out=o, in0=a, scalar1=s1, scalar2=s2, op0=op0, op1=op1), r=r, w=w)

    def cp(eng, o, a, r, w):
        S.add(eng, lambda e: e.tensor_copy(out=o, in_=a), r=r, w=w)

    def recip(o, a, r, w):
        S.add('dve', lambda e: e.reciprocal(out=o, in_=a), r=r, w=w)

    def memset(eng, o, v, w):
        S.add(eng, lambda e: e.memset(o, v), r=(), w=w)

    def dma(eng, o, in_, r, w, **kw):
        S.add(eng, lambda e: e.dma_start(out=o, in_=in_, **kw), r=r, w=w, dma=True)

    def dump(name, ap, r):
        if name in dbg_out:
            dma('pool', dbg_out[name], ap, r=r, w=[])

    ident = carve([128, 128], BF16)
    ones = carve([128, 128], BF16)
    mixedT = carve([128, 8, S_LEN], BF16)
    g_mix = carve([128, 8], F32); g_ffn = carve([128, 8], F32); g_ple = carve([128, 8], F32)
    g_gc = carve([128, 4], F32); g_ga = carve([128, 4], F32)
    xs = [carve([128, D], BF16) for _ in range(2)]
    nTb = [carve([128, 8, 512], BF16) for _ in range(2)]
    nT = nTb[0]
    ss = [carve([128, 1], F32) for _ in range(2)]
    rr = [carve([128, 1], F32) for _ in range(2)]
    junk = carve([128, D], BF16)
    dma('sp', ident, dr['ident'], r=[], w=['ident'])
    dma('sp', ones, dr['ones'], r=[], w=['ones'])
    for nm, tl, n in (('g_mix', g_mix, 8), ('g_ffn', g_ffn, 8), ('g_ple', g_ple, 8), ('g_gn_conv', g_gc, 4), ('g_gn_attn', g_ga, 4)):
        dma('sp', tl, dr[nm].rearrange("(c p) -> p c", p=128), r=[], w=[nm], allow_slow_non_contiguous=True)
    base_off = state['off']
    cnt = {'x': 0, 'n': 0}
    def cast_w(k, inner, rsplit=1, gate=()):
        R = WSHAPES[k][0]
        step = R // rsplit
        for i in range(rsplit):
            rs = slice(i * step, (i + 1) * step)
            dma('pool', wbf[k][rs, :].rearrange("r (a b) -> r a b", b=inner), dr[k][rs, :].rearrange("r (a b) -> r a b", b=inner),
                r=list(gate), w=[k + '_bf'])
    for blk in (4, 0, 1, 2, 3):
        dma('pool', wbf['w_in'][:, blk * 568:(blk + 1) * 568], dr['w_in'][:, blk * 568:(blk + 1) * 568], r=[], w=[('w_in_bf', blk)])

    def win_res(c0, n):
        return [('w_in_bf', b) for b in range(c0 // 568, (c0 + n - 1) // 568 + 1)]

    def rmsnorm_T(src, src_res, gt, g_res, tsl, nb=0):
        k = cnt['n'] % 2; cnt['n'] += 1
        act(junk, src, AF.Square, r=[src_res], w=['junk', ('ss', k)], scale=1.0 / 32, accum_out=ss[k])
        act(rr[k], ss[k], AF.Sqrt, r=[('ss', k)], w=[('rr', k)], bias=EPS, scale=1.0)
        recip(rr[k], rr[k], r=[('rr', k)], w=[('rr', k)])
        act(xs[k], src, AF.Copy, r=[src_res, ('rr', k)], w=[('xs', k)], scale=rr[k])
        for c in range(8):
            tr(PBa[:, c * 128:(c + 1) * 128], xs[k][:, c * 128:(c + 1) * 128], r=[('xs', k)], w=['PB'])
        tt('dve', nTb[nb][:, :, tsl], PBa.rearrange("p (c t) -> p c t", c=8), gt.unsqueeze(2).to_broadcast([128, 8, 128]),
           ALU.mult, r=['PB', g_res], w=[('nT', nb)])

    def phase1(s):
        state['off'] = base_off
        qT = carve([128, 4, S_LEN], BF16)
        ksz = [carve([128, S_LEN], BF16) for _ in range(2)]; kwz = [carve([128, S_LEN], BF16) for _ in range(2)]
        kcmpT = carve([128, S_LEN], BF16); vcmpT = carve([128, S_LEN], BF16)
        vs_ext = carve([128, NT, 2, 65], BF16); vw_ext = carve([128, NT, 2, 65], BF16)
        gates = carve([128, NT, 24], F32)
        w1blk = carve([128, 32, 128], BF16)
        w1d = carve([128, 16, 128], BF16)
        pecol = carve([128, 16], BF16)
        w2blk = carve([128, 128], BF16)
        c1 = carve([128, 1], F32)
        hidT = carve([128, 128], BF16)
        kcraw = carve([128, 128], BF16)
        kcz = [carve([128, 128], BF16) for _ in range(2)]
        vc_ext = carve([128, 2, 97], BF16)
        cosT = carve([128, S_LEN], BF16); sinT = carve([128, S_LEN], BF16)
        coscT = carve([128, 127], BF16); sincT = carve([128, 127], BF16)
        maskc = carve([128, 16, 128], BF16)
        selbias = carve([128, 16, 32], F32)
        eall = carve([128, 16, 128], BF16)
        rblk = carve([128, 128], BF16); causal = carve([128, 128], BF16); lower = carve([128, 128], BF16)
        ovt = carve([128, 32], BF16)
        cw = carve([128, 3, 4], F32)
        xt = [carve([128, D], F32) for _ in range(2)]
        wfm = [carve([128, 8, 128], BF16) for _ in range(4)]
        wtm = carve([128, 8, 280], BF16)
        xin_sb = [carve([128, 512], F32) for _ in range(2)]
        zb = [carve([128, 514], F32) for _ in range(2)]
        zh = carve([128, 4, 2], F32)
        accb = [carve([128, 512], F32) for _ in range(2)]
        ycv = carve([128, 4, 512], F32)
        ysq = carve([128, 4, 512], BF16)
        rc = carve([128, 512], F32)
        qraw = [carve([128, 512], BF16) for _ in range(2)]
        t1 = [carve([128, 512], F32) for _ in range(2)]
        t2 = [carve([128, 512], F32) for _ in range(2)]
        pT = [carve([128, 512], BF16) for _ in range(4)]
        yat = carve([128, 8, 64], F32)
        yab = carve([128, 512], BF16)
        tmpa = carve([128, 4, 64], F32)
        tmpw = carve([128, 4, 64], F32)
        sm = carve([128, 96], F32)
        score = carve([128, 32], F32)
        negsel = carve([128, 32], BF16)
        negselT = carve([128, 128], BF16)
        top8 = carve([128, 8], F32)

        for k3 in range(3):
            dma('sp', cw[:, k3, :], dr['w_conv_mix'][k3].rearrange("(j p) -> p j", p=128), r=[], w=['cw'], allow_slow_non_contiguous=True)
        memset('pool', vs_ext[:, :, :, 64:65], 1.0, w=['vs_ext'])
        memset('pool', vw_ext[:, :, :, 64:65], 1.0, w=['vw_ext'])
        memset('pool', vc_ext, 0.0, w=['vc_ext'])
        memset('pool', vc_ext[:, :, 64:65], 1.0, w=['vc_ext'])
        memset('pool', zh, 0.0, w=['zh'])
        memset('pool', ksz[0][64:128, :], 0.0, w=['ksT']); memset('pool', ksz[1][0:64, :], 0.0, w=['ksT'])
        memset('pool', kwz[0][64:128, :], 0.0, w=['kwT']); memset('pool', kwz[1][0:64, :], 0.0, w=['kwT'])
        memset('pool', kcz[0], 0.0, w=['kcT']); memset('pool', kcz[1], 0.0, w=['kcT'])
        memset('pool', negselT, 0.0, w=['negselT'])

        w_in_v = wbf['w_in'].rearrange("(c p) n -> p c n", p=128)
        xsrc = dr['x']
        fmk = {'k': 0, 'pk': 0, 'nb': 0, 'gi': 0}
        FMB = [0, 1, 2, 5, 6]

        def fm_chunk(col_pieces):
            k = fmk['k'] % 4; fmk['k'] += 1
            pk = FMB[fmk['pk'] % 5]; fmk['pk'] += 1
            o = 0
            for (c0, n) in col_pieces:
                dma('sp', wfm[k][:, :, o:o + n], w_in_v[:, :, c0:c0 + n], r=win_res(c0, n), w=[('wfm', k)])
                o += n
            for kc in range(8):
                mm(P[pk], lhsT=wfm[k][:, kc, :], rhs=nTb[fmk['nb']][:, kc, :], start=(kc == 0), stop=(kc == 7),
                   r=[('wfm', k), ('nT', fmk['nb'])], w=[('P', pk)])
            if pend:
                fn_, g_, t_ = pend.pop(0)
                fn_(g_, t_)
            return pk

        def rope(pk, dst, dst_res, tsl, cT, sT, cres, n=512):
            kq = cnt['x'] % 2; cnt['x'] += 1
            act(qraw[kq][:, 0:n], P[pk][:, 0:n], AF.Copy, r=[('P', pk)], w=[('qraw', kq)])
            mm(P[3][:, 0:n], lhsT=rblk, rhs=qraw[kq][:, 0:n], start=True, stop=True, r=['rblk', ('qraw', kq)], w=[('P', 3)])
            tt('dve', t1[kq][:, 0:n], P[pk][:, 0:n], cT, ALU.mult, r=[('P', pk), cres[0]], w=[('t1', kq)])
            tt('dve', t2[kq][:, 0:n], P[3][:, 0:n], sT, ALU.mult, r=[('P', 3), cres[1]], w=[('t2', kq)])
            if isinstance(dst, list):
                for (d_ap, ps_) in dst:
                    tt('dve', d_ap, t1[kq][ps_, 0:n], t2[kq][ps_, 0:n], ALU.add, r=[('t1', kq), ('t2', kq)], w=[dst_res])
            else:
                tt('dve', dst, t1[kq][:, 0:n], t2[kq][:, 0:n], ALU.add, r=[('t1', kq), ('t2', kq)], w=[dst_res])

        dma('sp', wtm[:, :, 0:128], w_in_v[:, :, 2432:2560], r=win_res(2432, 128), w=['wtm'])
        dma('sp', wtm[:, :, 128:256], w_in_v[:, :, 2688:2816], r=win_res(2688, 128), w=['wtm'])
        dma('sp', wtm[:, :, 256:280], w_in_v[:, :, 2816:2840], r=win_res(2816, 24), w=['wtm'])

        def tf_a(gi, t4):
            ti = gi * 4 + t4
            kx = ti % 2
            k = ti % 2
            dma('sp', xt[kx], xsrc[s, ti * 128:(ti + 1) * 128, :], r=[], w=[('xt', kx)])
            act(junk, xt[kx], AF.Square, r=[('xt', kx)], w=['junk', ('ss', k)], scale=1.0 / 32, accum_out=ss[k])
            act(rr[k], ss[k], AF.Sqrt, r=[('ss', k)], w=[('rr', k)], bias=EPS, scale=1.0)
            recip(rr[k], rr[k], r=[('rr', k)], w=[('rr', k)])
            act(xs[k], xt[kx], AF.Copy, r=[('xt', kx), ('rr', k)], w=[('xs', k)], scale=rr[k])

        def tf_b(gi, t4):
            nb = gi % 2
            k = (gi * 4 + t4) % 2
            for c in range(8):
                tr(PBa[:, c * 128:(c + 1) * 128], xs[k][:, c * 128:(c + 1) * 128], r=[('xs', k)], w=['PB'])
            tt('dve', nTb[nb][:, :, t4 * 128:(t4 + 1) * 128], PBa.rearrange("p (c t) -> p c t", c=8),
               g_mix.unsqueeze(2).to_broadcast([128, 8, 128]), ALU.mult, r=['PB', 'g_mix'], w=[('nT', nb)])

        def tf_c(gi, t4):
            nb = gi % 2
            ti = gi * 4 + t4
            for kc in range(8):
                mm(P[4][:, 0:280], lhsT=nTb[nb][:, kc, t4 * 128:(t4 + 1) * 128], rhs=wtm[:, kc, :], start=(kc == 0),
                   stop=(kc == 7), r=[('nT', nb), 'wtm'], w=[('P', 4)])
            act(vs_ext[:, ti, :, 0:64], P[4][:, 0:128].rearrange("p (h d) -> p h d", h=2), AF.Copy, r=[('P', 4)], w=['vs_ext'])
            act(vw_ext[:, ti, :, 0:64], P[4][:, 128:256].rearrange("p (h d) -> p h d", h=2), AF.Copy, r=[('P', 4)], w=['vw_ext'])
            act(gates[:, ti, :], P[4][:, 256:280], AF.Sigmoid, r=[('P', 4)], w=['gates'])

        def tile_front(gi, t4):
            tf_a(gi, t4); tf_b(gi, t4); tf_c(gi, t4)

        pend = []

        def front_stages(gi):
            st = []
            order = [('a', 0), ('a', 1), ('b', 0), ('a', 2), ('c', 0), ('b', 1), ('a', 3), ('c', 1), ('b', 2), ('c', 2), ('b', 3), ('c', 3)]
            fn = {'a': tf_a, 'b': tf_b, 'c': tf_c}
            for (kind, t4) in order:
                st.append((fn[kind], gi, t4))
            return st

        for t4 in range(4):
            tile_front(0, t4)
        for nm, tl in (('cosT', cosT), ('sinT', sinT), ('coscT', coscT), ('sincT', sincT), ('maskc', maskc),
                       ('selbias', selbias), ('eall', eall), ('rblk', rblk), ('causal', causal), ('lower', lower),
                       ('ov', ovt)):
            dma('sp', tl, dr[nm], r=[], w=[nm])
        for kvh in range(2):
            cp('pool', vc_ext[0:127, kvh, 65:97], ovt[0:127, :], r=['ov'], w=['vc_ext'])
        for gi in range(CFG['nga']):
            g0 = gi * 512
            fmk['nb'] = gi % 2
            fmk['gi'] = gi
            while pend:
                fn_, g_, t_ = pend.pop(0)
                fn_(g_, t_)
            if gi + 1 < CFG['nga']:
                pend.extend(front_stages(gi + 1))
            tsl = slice(g0, g0 + 512)
            if CFG['a_stage'] < 1:
                continue
            for j in range(4):
                kb = j % 2
                pk = fm_chunk([(128 * j, 128)])
                act(xin_sb[kb], P[pk], AF.Copy, r=[('P', pk)], w=[('xin', kb)])
                pk = fm_chunk([(1024 + 128 * j, 128)])
                tt('dve', zb[kb][:, 2:514], P[pk], xin_sb[kb], ALU.mult, r=[('P', pk), ('xin', kb)], w=[('zb', kb)])
                cp('dve', zb[kb][:, 0:2], zh[:, j, :], r=['zh'], w=[('zb', kb)])
                pk = fm_chunk([(512 + 128 * j, 128)])
                act(accb[kb], zb[kb][:, 0:512], AF.Copy, r=[('zb', kb), 'cw'], w=[('acc', kb)], scale=cw[:, 0, j:j + 1])
                stt('dve', accb[kb], zb[kb][:, 1:513], cw[:, 1, j:j + 1], accb[kb], ALU.mult, ALU.add, r=[('zb', kb), 'cw', ('acc', kb)], w=[('acc', kb)])
                stt('dve', accb[kb], zb[kb][:, 2:514], cw[:, 2, j:j + 1], accb[kb], ALU.mult, ALU.add, r=[('zb', kb), 'cw', ('acc', kb)], w=[('acc', kb)])
                cp('dve', zh[:, j, :], zb[kb][:, 512:514], r=[('zb', kb)], w=['zh'])
                tt('dve', ycv[:, j, :], P[pk], accb[kb], ALU.mult, r=[('P', pk), ('acc', kb)], w=[('ycv', j)])
            for j in range(4):
                act(ysq[:, j, :], ycv[:, j, :], AF.Square, r=[('ycv', j)], w=[('ysq', j)])
            for j in range(4):
                mm(P[3], lhsT=ones, rhs=ysq[:, j, :], start=(j == 0), stop=(j == 3), r=['ones', ('ysq', j)], w=[('P', 3)])
            act(rc, P[3], AF.Sqrt, r=[('P', 3)], w=['rc'], bias=EPS, scale=1.0 / 512)
            recip(rc, rc, r=['rc'], w=['rc'])
            for j in range(4):
                stt('dve', mixedT[:, j, tsl], ycv[:, j, :], g_gc[:, j:j + 1], rc, ALU.mult, ALU.mult,
                    r=[('ycv', j), 'g_gn_conv', 'rc'], w=['mixedT'])
            if CFG['a_stage'] < 2:
                continue
            for c in range(4):
                pk = fm_chunk([(1536 + 64 * c, 64), (1536 + 64 * (4 + c), 64)])
                rope(pk, qT[:, c, tsl], 'qT', tsl, cosT[:, tsl], sinT[:, tsl], ('cosT', 'sinT'))
            if CFG['a_stage'] < 3:
                continue
            pk = fm_chunk([(2304, 128)])
            rope(pk, [(ksz[0][0:64, tsl], slice(0, 64)), (ksz[1][64:128, tsl], slice(64, 128))], 'ksT', tsl, cosT[:, tsl], sinT[:, tsl], ('cosT', 'sinT'))
            pk = fm_chunk([(2560, 128)])
            rope(pk, [(kwz[0][0:64, tsl], slice(0, 64)), (kwz[1][64:128, tsl], slice(64, 128))], 'kwT', tsl, cosT[:, tsl], sinT[:, tsl], ('cosT', 'sinT'))
            pk = fm_chunk([(2048, 128)])
            act(kcmpT[:, tsl], P[pk], AF.Copy, r=[('P', pk)], w=['kcmpT'])
            pk = fm_chunk([(2176, 128)])
            act(vcmpT[:, tsl], P[pk], AF.Copy, r=[('P', pk)], w=['vcmpT'])

        for which, srcT, sres in ((('k', kcmpT, 'kcmpT'), ('v', vcmpT, 'vcmpT')) if CFG['do_b'] else ()):
            w1 = dr['cmp_w1_' + which]; w2 = dr['cmp_w2_' + which]; pe = dr['cmp_pe_' + which]
            memset('pool', w1blk, 0.0, w=['w1blk'])
            memset('pool', w2blk, 0.0, w=['w2blk'])
            w1v = w1.rearrange("(j d) h -> d j h", d=64)
            dma('pool', w1blk[0:64, :, 0:64], w1v, r=[], w=['w1blk'])
            dma('pool', w1blk[64:128, :, 64:128], w1v, r=[], w=['w1blk'])
            dma('pool', w2blk[0:64, 0:64], w2, r=[], w=['w2blk'])
            dma('pool', w2blk[64:128, 64:128], w2, r=[], w=['w2blk'])
            w1c = w1.rearrange("(i p) h -> p i h", p=128)
            dma('pool', w1d[:, :, 0:64], w1c, r=[], w=['w1d'])
            dma('pool', w1d[:, :, 64:128], w1c, r=[], w=['w1d'])
            dma('pool', pecol, pe.rearrange("(i a) d -> (a d) i", a=2), r=[], w=['pecol'],
                allow_slow_non_contiguous=True)
            for i in range(16):
                mm(P[6][:, 0:1], lhsT=w1d[:, i, :], rhs=pecol[:, i:i + 1], start=(i == 0), stop=(i == 15),
                   r=['w1d', 'pecol'], w=[('P', 6)])
            act(c1, P[6][:, 0:1], AF.Copy, r=[('P', 6)], w=['c1'])
            for j in range(32):
                mm(P[5][:, 0:127], lhsT=w1blk[:, j, :], rhs=srcT[:, j:j + 2017:16], start=(j == 0), stop=(j == 31),
                   r=['w1blk', sres], w=[('P', 5)])
            act(hidT[:, 0:127], P[5][:, 0:127], AF.Silu, r=[('P', 5), 'c1'], w=['hidT'], bias=c1)
            if which == 'k':
                mm(P[6][:, 0:127], lhsT=w2blk, rhs=hidT[:, 0:127], start=True, stop=True, r=['w2blk', 'hidT'], w=[('P', 6)])
                act(kcraw[:, 0:127], P[6][:, 0:127], AF.Copy, r=[('P', 6)], w=['kcraw'])
                mm(P[3][:, 0:127], lhsT=rblk, rhs=kcraw[:, 0:127], start=True, stop=True, r=['rblk', 'kcraw'], w=[('P', 3)])
                tt('dve', t1[0][:, 0:127], P[6][:, 0:127], coscT, ALU.mult, r=[('P', 6), 'coscT'], w=[('t1', 0)])
                tt('dve', t2[0][:, 0:127], P[3][:, 0:127], sincT, ALU.mult, r=[('P', 3), 'sincT'], w=[('t2', 0)])
                tt('dve', kcz[0][0:64, 0:127], t1[0][0:64, 0:127], t2[0][0:64, 0:127], ALU.add, r=[('t1', 0), ('t2', 0)], w=['kcT'])
                tt('dve', kcz[1][64:128, 0:127], t1[0][64:128, 0:127], t2[0][64:128, 0:127], ALU.add, r=[('t1', 0), ('t2', 0)], w=['kcT'])
            else:
                mm(P[6][0:127, 0:128], lhsT=hidT[:, 0:127], rhs=w2blk, start=True, stop=True, r=['w2blk', 'hidT'], w=[('P', 6)])
                act(vc_ext[0:127, :, 0:64], P[6][0:127, 0:128].rearrange("p (h d) -> p h d", h=2), AF.Copy,
                    r=[('P', 6)], w=['vc_ext'])
        if s == 0:
            cast_w('w_out', 1024, gate=['kcT'])
            cast_w('w_up', 512, rsplit=2, gate=['kcT'])
            cast_w('w_down', 1024, gate=['kcT'])
            cast_w('w_ple_gate', 1024, gate=['kcT'])
            cast_w('w_ple_proj', 1024, gate=['kcT'])
        dump('qT', qT[:, :, 0:512], r=['qT'])
        dump('mixc', mixedT[:, 0:4, 0:512], r=['mixedT'])

        sk = {'s': 0, 'p': 0, 'u': 0}
        for i in range(CFG['nqt']):
            qsl = slice(i * 128, (i + 1) * 128)
            for kvh in range(2):
                hp = slice(0, 64) if kvh == 0 else slice(64, 128)
                q_rhs = qT[:, :, qsl]

                def score_tile(kT_l, extra, kparts=128):
                    b = sk['s'] % 3; sk['s'] += 1
                    kp = sk['p'] % 4; sk['p'] += 1
                    n_ex = len(extra)
                    o3 = P[b][0:kparts, :].rearrange("p (h q) -> p h q", h=4)
                    mm(o3, lhsT=kT_l[0], rhs=q_rhs, start=True, stop=(n_ex == 0), r=[kT_l[1], 'qT'], w=[('P', b)])
                    for xi, (l, rhs_, rres) in enumerate(extra):
                        mm(o3, lhsT=l, rhs=rhs_, start=False, stop=(xi == n_ex - 1), r=rres, w=[('P', b)])
                    act(pT[kp][0:kparts, :], P[b][0:kparts, :], AF.Exp, r=[('P', b)], w=[('pT', kp)], scale=0.125)
                    return kp

                gv = gates[:, i, :].rearrange("p (h b) -> p h b", b=3)
                yv = yat[:, kvh * 4:(kvh + 1) * 4, :]
                ccc = sm[:, 8:12]; ccs = sm[:, 12:16]; ccw = sm[:, 16:20]
                owv = P[5][:, 0:260].rearrange("p (h c) -> p h c", h=4)
                ocb = 3 if (sk['u'] % 2 == 0) else 6
                sk['u'] += 1
                oc = P[ocb][:, 0:388].rearrange("p (h c) -> p h c", h=4)
                rdc = sm[:, 0:4]
                nsb = negselT.unsqueeze(1).to_broadcast([128, 4, 128])
                cb = causal.unsqueeze(1).to_broadcast([128, 4, 128])
                lb = lower.unsqueeze(1).to_broadcast([128, 4, 128])
                mk = maskc[:, i, :].unsqueeze(1).to_broadcast([128, 4, 128])

                def pv_c(kp):
                    for hb in range(4):
                        mm(P[ocb][:, hb * 97:(hb + 1) * 97], lhsT=pT[kp][:, hb * 128:(hb + 1) * 128], rhs=vc_ext[:, kvh, :],
                           start=True, stop=True, r=[('pT', kp), 'vc_ext'], w=[('P', ocb)])
                    ts('dve', rdc, oc[:, :, 64], 1e-30, None, ALU.max, None, r=[('P', ocb)], w=['rdc'])
                    recip(rdc, rdc, r=['rdc'], w=['rdc'])
                    ts('dve', score, oc[:, 0, 65:97], rdc[:, 0:1], None, ALU.mult, None, r=[('P', ocb), 'rdc'], w=['score'])
                    for hb in range(1, 4):
                        stt('dve', score, oc[:, hb, 65:97], rdc[:, hb:hb + 1], score, ALU.mult, ALU.add,
                            r=[('P', ocb), 'rdc', 'score'], w=['score'])
                    tt('dve', score, score, selbias[:, i, :], ALU.add, r=['score', 'selbias'], w=['score'])
                    S.add('dve', lambda e: e.max(out=top8, in_=score), r=['score'], w=['top8'])
                    ts('dve', score, score, top8[:, 7:8], None, ALU.is_ge, None, r=['score', 'top8'], w=['score'])
                    ts('dve', negsel, score, 1.0, -NEGB, ALU.subtract, ALU.mult, r=['score'], w=['negsel'])
                    tr(PBa[0:32, 0:128], negsel, r=['negsel'], w=['PB'])
                    cp('dve', negselT[0:32, :], PBa[0:32, 0:128], r=['PB'], w=['negselT'])
                    tt('dve', ccc, rdc, gv[:, kvh * 4:(kvh + 1) * 4, 0], ALU.mult, r=['rdc', 'gates'], w=['ccc'])
                    tt('dve', yv, oc[:, :, 0:64], ccc.unsqueeze(2).to_broadcast([128, 4, 64]), ALU.mult, r=[('P', ocb), 'ccc'], w=['yat'])

                def win_done():
                    ts('dve', ccw, owv[:, :, 64], 1e-30, None, ALU.max, None, r=[('P', 5)], w=['ccw'])
                    recip(ccw, ccw, r=['ccw'], w=['ccw'])
                    tt('dve', ccw, ccw, gv[:, kvh * 4:(kvh + 1) * 4, 2], ALU.mult, r=['ccw', 'gates'], w=['ccw'])
                    tt('dve', tmpw, owv[:, :, 0:64], ccw.unsqueeze(2).to_broadcast([128, 4, 64]), ALU.mult, r=[('P', 5), 'ccw'], w=['tmpw'])
                    tt('dve', yv, yv, tmpw, ALU.add, r=['yat', 'tmpw'], w=['yat'])

                def mk_pv(bank, vext, vres, j, first, last):
                    def pv(kp):
                        for hb in range(4):
                            mm(P[bank][:, hb * 65:(hb + 1) * 65], lhsT=pT[kp][:, hb * 128:(hb + 1) * 128], rhs=vext[:, j, kvh, :],
                               start=(first and hb == 0), stop=(hb == 3), r=[('pT', kp), vres], w=[('P', bank)],
                               skip_group_check=True)
                        if last and bank == 5:
                            win_done()
                    return pv

                tasks = [(((kcz[kvh], 'kcT'), [(ident, mk, ['ident', 'maskc'])], 128), pv_c)]
                j0 = max(0, i - 4)
                for j in range(j0, i + 1):
                    ex = []
                    if j == i:
                        ex.append((ident, cb, ['ident', 'causal']))
                    if j == i - 4:
                        ex.append((ident, lb, ['ident', 'lower']))
                    tasks.append((((kwz[kvh][:, j * 128:(j + 1) * 128], 'kwT'), ex, 128), mk_pv(5, vw_ext, 'vw_ext', j, j == j0, j == i)))
                for j in range(i + 1):
                    ex = [(eall[:, j, :], nsb, ['eall', 'negselT'])]
                    if j == i:
                        ex.append((ident, cb, ['ident', 'causal']))
                    tasks.append((((ksz[kvh][:, j * 128:(j + 1) * 128], 'ksT'), ex, 128), mk_pv(4, vs_ext, 'vs_ext', j, j == 0, j == i)))
                prev = None
                for (sargs, pvf) in tasks:
                    kp = score_tile(sargs[0], sargs[1], kparts=sargs[2])
                    if prev is not None:
                        prev[0](prev[1])
                    prev = (pvf, kp)
                prev[0](prev[1])
                osv = P[4][:, 0:260].rearrange("p (h c) -> p h c", h=4)
                ts('dve', ccs, osv[:, :, 64], 1e-30, None, ALU.max, None, r=[('P', 4)], w=['ccs'])
                recip(ccs, ccs, r=['ccs'], w=['ccs'])
                tt('dve', ccs, ccs, gv[:, kvh * 4:(kvh + 1) * 4, 1], ALU.mult, r=['ccs', 'gates'], w=['ccs'])
                tt('dve', tmpa, osv[:, :, 0:64], ccs.unsqueeze(2).to_broadcast([128, 4, 64]), ALU.mult, r=[('P', 4), 'ccs'], w=['tmpa'])
                tt('dve', yv, yv, tmpa, ALU.add, r=['yat', 'tmpa'], w=['yat'])
            k = cnt['n'] % 2; cnt['n'] += 1
            yflat = yat.rearrange("p h d -> p (h d)")
            if i == 0:
                dump('yat0', yflat, r=['yat'])
            act(junk[:, 0:512], yflat, AF.Square, r=['yat'], w=['junk', ('ss', k)], scale=float(512 ** -0.5), accum_out=ss[k])
            act(rr[k], ss[k], AF.Sqrt, r=[('ss', k)], w=[('rr', k)], bias=EPS, scale=1.0)
            recip(rr[k], rr[k], r=[('rr', k)], w=[('rr', k)])
            act(yab, yflat, AF.Copy, r=['yat', ('rr', k)], w=['yab'], scale=rr[k])
            for c in range(4):
                tr(PBa[:, c * 128:(c + 1) * 128], yab[:, c * 128:(c + 1) * 128], r=['yab'], w=['PB'])
            tt('dve', mixedT[:, 4:8, qsl], PBa[:, 0:512].rearrange("p (c t) -> p c t", c=4),
               g_ga.unsqueeze(2).to_broadcast([128, 4, 128]), ALU.mult, r=['PB', 'g_gn_attn'], w=['mixedT'])
        dump('mixa', mixedT[:, 4:8, 0:512], r=['mixedT'])

    def phase2(s):
        state['off'] = base_off
        h = [carve([128, D], F32) for _ in range(4)]
        actT = carve([128, NF, 512], BF16)
        abuf = [carve([128, 514], F32) for _ in range(4)]
        ub = [carve([128, 512], F32) for _ in range(4)]
        sg = [carve([128, 512], F32) for _ in range(2)]
        wub = [carve([128, 8, 256], BF16) for _ in range(4)]
        wdb = [carve([128, 512], BF16) for _ in range(8)]
        w_out = carve([128, 8, D], BF16)
        wpg = carve([128, 8, D], BF16)
        wpp = carve([128, 2, D], BF16)
        gfin = carve([128, D], F32)
        outt = [carve([128, D], F32) for _ in range(2)]
        pt = [carve([128, 256], F32) for _ in range(2)]
        pbf = [carve([128, 256], BF16) for _ in range(2)]
        ppT = carve([128, 2, 512], BF16)
        sgm = [carve([128, 512], F32) for _ in range(2)]
        tmp = [carve([128, 512], F32) for _ in range(2)]
        hal = carve([128, 2 * NF, 2], F32)
        cwf = carve([128, 3, 2 * NF], F32)

        dma('sp', w_out, wbf['w_out'].rearrange("(c p) n -> p c n", p=128), r=['w_out_bf'], w=['w_out'])
        dma('sp', wpg, wbf['w_ple_gate'].rearrange("(c p) n -> p c n", p=128), r=['w_ple_gate_bf'], w=['wpg'])
        dma('sp', wpp, wbf['w_ple_proj'].rearrange("(c p) n -> p c n", p=128), r=['w_ple_proj_bf'], w=['wpp'])
        dma('sp', gfin, dr['g_final'].partition_broadcast(128), r=[], w=['gfin'])
        for k3 in range(3):
            dma('sp', cwf[:, k3, :], dr['w_ffn_conv'][k3].rearrange("(f p) -> p f", p=128), r=[], w=['cwf'], allow_slow_non_contiguous=True)
        memset('pool', hal, 0.0, w=['hal'])
        w_up_v = wbf['w_up'].rearrange("(c p) n -> p c n", p=128)
        w_dn_v = wbf['w_down'].rearrange("(f p) n -> p f n", p=128)
        ck = {'u': 0, 'd': 0, 'pp': 0, 'ab': 0, 'o': 0, 'sg': 0}

        for gi in range(CFG['ng2']):
            g0 = gi * 512
            for t4 in range(4):
                tok = slice(g0 + t4 * 128, g0 + (t4 + 1) * 128)
                dma('sp', h[t4], dr['x'][s, tok, :], r=[], w=[('h', t4)])
                for half in range(2):
                    pk = ck['pp'] % 4; ck['pp'] += 1
                    for kc in range(8):
                        mm(P[pk], lhsT=mixedT[:, kc, tok], rhs=w_out[:, kc, half * 512:(half + 1) * 512], start=(kc == 0),
                           stop=(kc == 7), r=['mixedT', 'w_out'], w=[('P', pk)])
                    hs = h[t4][:, half * 512:(half + 1) * 512]
                    tt('dve', hs, P[pk], hs, ALU.add, r=[('P', pk), ('h', t4)], w=[('h', t4)])
                if t4 >= 1:
                    rmsnorm_T(h[t4 - 1], ('h', t4 - 1), g_ffn, 'g_ffn', slice((t4 - 1) * 128, t4 * 128))
            rmsnorm_T(h[3], ('h', 3), g_ffn, 'g_ffn', slice(3 * 128, 4 * 128))
            def up_head(f):
                k = ck['u'] % 4; ck['u'] += 1
                dma('sp', wub[k][:, :, 0:128], w_up_v[:, :, f * 128:(f + 1) * 128], r=['w_up_bf'], w=[('wub', k)])
                dma('sp', wub[k][:, :, 128:256], w_up_v[:, :, DFF + f * 128:DFF + (f + 1) * 128], r=['w_up_bf'], w=[('wub', k)])
                pg = (ck['pp'] % 3) * 2; ck['pp'] += 1
                us = []
                for hv in range(2):
                    pk = pg + hv
                    for kc in range(8):
                        mm(P[pk], lhsT=wub[k][:, kc, hv * 128:(hv + 1) * 128], rhs=nT[:, kc, :], start=(kc == 0), stop=(kc == 7),
                           r=[('wub', k), ('nT', 0)], w=[('P', pk)])
                    a = ck['ab'] % 4; ck['ab'] += 1
                    us.append((a, hv * NF + f, pk))
                for (a, fi, pk) in us:
                    act(abuf[a][:, 2:514], P[pk], AF.Copy, r=[('P', pk)], w=[('abuf', a)])
                    cp('dve', abuf[a][:, 0:2], hal[:, fi, :], r=['hal'], w=[('abuf', a)])
                return us

            def up_mid(us):
                for (a, fi, pk) in us:
                    act(ub[a], abuf[a][:, 0:512], AF.Copy, r=[('abuf', a), 'cwf'], w=[('ub', a)], scale=cwf[:, 0, fi:fi + 1])
                    cp('dve', hal[:, fi, :], abuf[a][:, 512:514], r=[('abuf', a)], w=['hal'])
                for tap in (1, 2):
                    for (a, fi, pk) in us:
                        stt('dve', ub[a], abuf[a][:, tap:tap + 512], cwf[:, tap, fi:fi + 1], ub[a], ALU.mult, ALU.add,
                            r=[('abuf', a), 'cwf', ('ub', a)], w=[('ub', a)])

            def up_tail(f, us):
                q = ck['sg'] % 2; ck['sg'] += 1
                act(sg[q], ub[us[0][0]], AF.Silu, r=[('ub', us[0][0])], w=[('sg', q)])
                tt('dve', actT[:, f, :], sg[q], ub[us[1][0]], ALU.mult, r=[('sg', q), ('ub', us[1][0])], w=['actT'])

            hist = {}
            for f in range(NF + 2):
                if f < NF:
                    hist[f] = up_head(f)
                if 0 <= f - 1 < NF:
                    up_mid(hist[f - 1])
                if 0 <= f - 2 < NF:
                    up_tail(f - 2, hist[f - 2])
            for half in range(2):
                banks = [0, 1, 2, 3] if half == 0 else [4, 5, 6, 0]
                for f in range(NF):
                    k = ck['d'] % 8; ck['d'] += 1
                    dma('sp', wdb[k], w_dn_v[:, f, half * 512:(half + 1) * 512], r=['w_down_bf'], w=[('wdb', k)])
                    for t4 in range(4):
                        mm(P[banks[t4]], lhsT=actT[:, f, t4 * 128:(t4 + 1) * 128], rhs=wdb[k], start=(f == 0), stop=True,
                           r=['actT', ('wdb', k)], w=[('P', banks[t4])], skip_group_check=True)
                for t4 in range(4):
                    hs = h[t4][:, half * 512:(half + 1) * 512]
                    tt('dve', hs, P[banks[t4]], hs, ALU.add, r=[('P', banks[t4]), ('h', t4)], w=[('h', t4)])
            for t4 in range(4):
                tok = slice(g0 + t4 * 128, g0 + (t4 + 1) * 128)
                rmsnorm_T(h[t4], ('h', t4), g_ple, 'g_ple', slice(t4 * 128, (t4 + 1) * 128))
                kp = t4 % 2
                dma('sp', pt[kp], dr['p'][s, tok, :], r=[], w=[('pt', kp)])
                act(pbf[kp], pt[kp], AF.Copy, r=[('pt', kp)], w=[('pbf', kp)])
                for c in range(2):
                    tr(PBa[:, c * 128:(c + 1) * 128], pbf[kp][:, c * 128:(c + 1) * 128], r=[('pbf', kp)], w=['PB'])
                cp('dve', ppT[:, :, t4 * 128:(t4 + 1) * 128], PBa[:, 0:256].rearrange("p (c t) -> p c t", c=2), r=['PB'], w=['ppT'])
            for t4 in range(4):
                for half in range(2):
                    pk = (ck['pp'] % 3) * 2; ck['pp'] += 1
                    cs = slice(half * 512, (half + 1) * 512)
                    for kc in range(8):
                        mm(P[pk], lhsT=nT[:, kc, t4 * 128:(t4 + 1) * 128], rhs=wpg[:, kc, cs], start=(kc == 0), stop=(kc == 7),
                           r=[('nT', 0), 'wpg'], w=[('P', pk)])
                    for kc in range(2):
                        mm(P[pk + 1], lhsT=ppT[:, kc, t4 * 128:(t4 + 1) * 128], rhs=wpp[:, kc, cs], start=(kc == 0), stop=(kc == 1),
                           r=['ppT', 'wpp'], w=[('P', pk + 1)])
                    q = ck['sg'] % 2; ck['sg'] += 1
                    act(sgm[q], P[pk], AF.Sigmoid, r=[('P', pk)], w=[('sgm', q)])
                    tt('dve', tmp[q], P[pk + 1], sgm[q], ALU.mult, r=[('P', pk + 1), ('sgm', q)], w=[('tmp', q)])
                    tt('dve', h[t4][:, cs], h[t4][:, cs], tmp[q], ALU.add, r=[('h', t4), ('tmp', q)], w=[('h', t4)])
            for t4 in range(4):
                tok = slice(g0 + t4 * 128, g0 + (t4 + 1) * 128)
                k = cnt['n'] % 2; cnt['n'] += 1
                o = ck['o'] % 2; ck['o'] += 1
                act(junk, h[t4], AF.Square, r=[('h', t4)], w=['junk', ('ss', k)], scale=1.0 / 32, accum_out=ss[k])
                act(rr[k], ss[k], AF.Sqrt, r=[('ss', k)], w=[('rr', k)], bias=EPS, scale=1.0)
                recip(rr[k], rr[k], r=[('rr', k)], w=[('rr', k)])
                stt('dve', outt[o], h[t4], rr[k], gfin, ALU.mult, ALU.mult, r=[('h', t4), ('rr', k), 'gfin'], w=[('outt', o)])
                dma('sp', out[s, tok, :], outt[o], r=[('outt', o)], w=[])

    for s in range(CFG['nseq']):
        phase1(s)
        S.barrier()
        if CFG['do_p2']:
            phase2(s)
            S.barrier()
    S.run()
    es.close()
    return nc


_CACHE = {}


def kernel(**inputs):
    ncores = 8
    consts = host_consts()
    if 'nc' not in _CACHE:
        _CACHE['nc'] = build_program()
    nc = _CACHE['nc']
    x = np.ascontiguousarray(np.asarray(inputs['x'], dtype=np.float32))
    p = np.ascontiguousarray(np.asarray(inputs['p'], dtype=np.float32))
    in_maps = []
    for c in range(ncores):
        m = {'x': x[NSEQ * c:NSEQ * (c + 1)], 'p': p[0, NSEQ * c:NSEQ * (c + 1)]}
        for k in WSHAPES:
            a = np.asarray(inputs[k], dtype=np.float32)
            if k != 'g_final':
                a = a[0]
            m[k] = np.ascontiguousarray(a)
        for k, v in consts.items():
            m['c_' + k] = v
        in_maps.append(m)
    res = run_bass_kernel_spmd(nc, in_maps, core_ids=list(range(ncores)))
    outs = [np.asarray(r['out'], dtype=np.float32) for r in res.results]
    return np.concatenate(outs, axis=0)
```

```python
import numpy as np
import ml_dtypes
from contextlib import ExitStack
import concourse.bass as bass
import concourse.mybir as mybir
from concourse.bass_utils import run_bass_kernel_spmd

F32 = mybir.dt.float32
BF16 = mybir.dt.bfloat16
U8 = mybir.dt.uint8
ALU = mybir.AluOpType
AF = mybir.ActivationFunctionType

ENGS = ['pe', 'act', 'dve', 'pool', 'sp']
EPOCH = 12000
NDSEM = 16

D = 1024
S_LEN = 2048
NSEQ = 2
NT = 16
DFF = 2816
NF = 22
NEGB = -30000.0
EPS = 1e-6
CFG = dict(tilebar=False, ntile=4, tm=True, a_stage=9, nseq=2, nga=4, do_b=True, nqt=16, do_p2=True, ng2=4)


class Op:
    __slots__ = ('eng', 'emit', 'deps', 'id', 'sig', 'dma', 'comp', 'dk')


class Sched:
    def __init__(self, nc):
        self.nc = nc
        self.ops = []
        self.last_w = {}
        self.readers = {}
        self.eng_list = {e: [] for e in ENGS}
        self.ndma = {e: 0 for e in ENGS}

    def add(self, eng, emit, r=(), w=(), dma=False):
        op = Op()
        op.eng = eng; op.emit = emit; op.id = len(self.ops); op.dma = dma
        op.sig = False; op.comp = None; op.dk = None
        deps = set()
        rw = set()
        for res in r:
            if res in self.last_w:
                deps.add(self.last_w[res]); rw.add(self.last_w[res])
            if res == 'PB' or (isinstance(res, tuple) and res[0] == 'P'):
                for x in self.readers.get(res, ()):
                    if self.ops[x].eng != eng:
                        deps.add(x)
        for res in w:
            if res in self.last_w:
                deps.add(self.last_w[res])
            deps.update(self.readers.get(res, ()))
        fd = set()
        for d in deps:
            dop = self.ops[d]
            if (not dma) and (not dop.dma) and dop.eng == eng:
                if eng == 'pe':
                    continue
            fd.add(d)
        op.deps = fd
        for res in r:
            lst = self.readers.setdefault(res, [])
            if not dma:
                lst[:] = [x for x in lst if self.ops[x].dma or self.ops[x].eng != eng]
            lst.append(op.id)
        for res in w:
            self.last_w[res] = op.id
            self.readers[res] = []
        if dma:
            op.dk = self.ndma[eng]
            self.ndma[eng] += 1
        self.ops.append(op)
        self.eng_list[eng].append(op)
        return op

    def barrier(self):
        n = len(self.ops)
        for e in ENGS:
            self.add(e, None, r=(), w=[('bar', n, e)])
        lastd = {}
        for op in self.ops:
            if op.dma:
                lastd[(op.eng, op.dk % NDSEM)] = op.id
        for e in ENGS:
            w = self.add(e, None, r=[('bar', n, e2) for e2 in ENGS], w=())
            w.deps.update(lastd.values())
        self.last_w.clear()
        self.readers.clear()

    def finalize(self):
        for op in self.ops:
            for d in op.deps:
                self.ops[d].sig = True
        self.nsem_eng = {}
        for e in ENGS:
            c = 0
            for op in self.eng_list[e]:
                if op.dma:
                    continue
                if op.sig:
                    c += 1
                    op.comp = ('c', e, (c - 1) // EPOCH, (c - 1) % EPOCH + 1)
            self.nsem_eng[e] = (c + EPOCH - 1) // EPOCH if c else 0
        for op in self.ops:
            if op.dma:
                op.comp = ('d', op.eng, op.dk % NDSEM, 16 * (op.dk // NDSEM + 1))

    def run(self):
        nc = self.nc
        self.finalize()
        es = ExitStack()
        sems = {}
        for e in ENGS:
            for i in range(self.nsem_eng[e]):
                sems[('c', e, i)] = es.enter_context(nc.semaphore(f"s_{e}_{i}"))
            for i in range(min(NDSEM, self.ndma[e])):
                sems[('d', e, i)] = es.enter_context(nc.semaphore(f"d_{e}_{i}"))
        block = es.enter_context(nc.Block())
        sched = self

        def body(ename):
            def f(eng):
                waited = {}
                cnt = {}
                for op in sched.eng_list[ename]:
                    need = {}
                    for d in op.deps:
                        k = sched.ops[d].comp
                        need[k[:3]] = max(need.get(k[:3], 0), k[3])
                    if op.dma and op.dk >= NDSEM:
                        key = ('d', ename, op.dk % NDSEM)
                        need[key] = max(need.get(key, 0), 16 * (op.dk // NDSEM))
                    for key, val in need.items():
                        if waited.get(key, 0) >= val:
                            continue
                        waited[key] = val
                        eng.wait_ge(sems[key], val)
                    if op.emit is None:
                        if op.sig:
                            eng.drain().then_inc(sems[op.comp[:3]], 1)
                        continue
                    ins = op.emit(eng)
                    if op.dma:
                        ins.then_inc(sems[op.comp[:3]], 16)
                        cnt[op.dk % NDSEM] = cnt.get(op.dk % NDSEM, 0) + 1
                    elif op.sig:
                        ins.then_inc(sems[op.comp[:3]], 1)
                for i, c in cnt.items():
                    key = ('d', ename, i)
                    if waited.get(key, 0) < 16 * c:
                        eng.wait_ge(sems[key], 16 * c)
            return f

        block.tensor(body('pe'))
        block.scalar(body('act'))
        block.vector(body('dve'))
        block.gpsimd(body('pool'))
        block.sync(body('sp'))
        es.close()


def host_consts():
    bf = ml_dtypes.bfloat16
    c = {}
    c['ident'] = np.eye(128, dtype=np.float32).astype(bf)
    k = np.arange(128)[:, None]; m = np.arange(128)[None, :]
    c['rblk'] = ((k // 64 == m // 64) & (k % 64 == (m % 64 + 32) % 64)).astype(np.float32).astype(bf)
    inv = (10000.0 ** (-np.arange(0, 64, 2, dtype=np.float32) / 64)).astype(np.float32)
    pr = np.arange(128) % 64
    fr = inv[pr % 32][:, None]
    sgn = np.where(pr < 32, -1.0, 1.0)[:, None].astype(np.float32)
    pos = np.arange(S_LEN, dtype=np.float32)[None, :]
    ang = (pos * fr).astype(np.float32)
    c['cosT'] = np.cos(ang).astype(np.float32).astype(bf)
    c['sinT'] = (np.sin(ang) * sgn).astype(np.float32).astype(bf)
    posc = (np.arange(127, dtype=np.float32) * 16 + 31)[None, :]
    angc = (posc * fr).astype(np.float32)
    c['coscT'] = np.cos(angc).astype(np.float32).astype(bf)
    c['sincT'] = (np.sin(angc) * sgn).astype(np.float32).astype(bf)
    cc = np.arange(128)[:, None, None]; ii = np.arange(16)[None, :, None]; qq = np.arange(128)[None, None, :]
    c['maskc'] = np.where(16 * cc + 31 <= 128 * ii + qq, 0.0, NEGB).astype(np.float32).astype(bf)
    qq2 = np.arange(128)[:, None, None]; ii2 = np.arange(16)[None, :, None]; nn = np.arange(32)[None, None, :]
    t = 128 * ii2 + qq2
    cur = t // 64
    valid = nn * 64 <= t
    forced = (nn == 0) | (nn == cur) | (nn == cur - 1)
    c['selbias'] = np.where(valid, np.where(forced, 1e4, 0.0), -1e30).astype(np.float32)
    b = np.arange(128)[:, None, None]; jj = np.arange(16)[None, :, None]; p = np.arange(128)[None, None, :]
    c['eall'] = (b == 2 * jj + p // 64).astype(np.float32).astype(bf)
    kk = np.arange(128)[:, None]; q = np.arange(128)[None, :]
    c['causal'] = np.where(kk > q, NEGB, 0.0).astype(np.float32).astype(bf)
    c['lower'] = np.where(kk <= q, NEGB, 0.0).astype(np.float32).astype(bf)
    cs = np.arange(127)[:, None] * 16; bs = np.arange(32)[None, :] * 64
    ov = np.clip(np.minimum(cs + 32, bs + 64) - np.maximum(cs, bs), 0, None).astype(np.float32) / 32
    c['ov'] = np.concatenate([ov, np.zeros((1, 32), np.float32)], 0).astype(bf)
    c['ones'] = np.ones((128, 128), np.float32).astype(bf)
    return c


CONST_SHAPES = {
    'ident': ([128, 128], BF16), 'rblk': ([128, 128], BF16), 'cosT': ([128, S_LEN], BF16),
    'sinT': ([128, S_LEN], BF16), 'coscT': ([128, 127], BF16), 'sincT': ([128, 127], BF16),
    'maskc': ([128, 16, 128], BF16), 'selbias': ([128, 16, 32], F32), 'eall': ([128, 16, 128], BF16),
    'causal': ([128, 128], BF16), 'lower': ([128, 128], BF16), 'ov': ([128, 32], BF16),
    'ones': ([128, 128], BF16),
}

WSHAPES = {
    'g_mix': [D], 'w_in': [D, 2840], 'w_conv_mix': [3, 512], 'cmp_pe_k': [32, 64], 'cmp_w1_k': [2048, 64],
    'cmp_w2_k': [64, 64], 'cmp_pe_v': [32, 64], 'cmp_w1_v': [2048, 64], 'cmp_w2_v': [64, 64],
    'g_gn_conv': [512], 'g_gn_attn': [512], 'w_out': [D, D], 'g_ffn': [D], 'w_up': [D, 2 * DFF],
    'w_ffn_conv': [3, 2 * DFF], 'w_down': [DFF, D], 'g_ple': [D], 'w_ple_gate': [D, D],
    'w_ple_proj': [256, D], 'g_final': [D],
}


def build_program(debug=None):
    nc = bass.Bass("TRN2", target_bir_lowering=False)
    dr = {}
    dr['x'] = nc.dram_tensor("x", [NSEQ, S_LEN, D], F32, kind="ExternalInput").ap()
    dr['p'] = nc.dram_tensor("p", [NSEQ, S_LEN, 256], F32, kind="ExternalInput").ap()
    for k, shp in WSHAPES.items():
        dr[k] = nc.dram_tensor(k, shp, F32, kind="ExternalInput").ap()
    for k, (shp, dt) in CONST_SHAPES.items():
        dr[k] = nc.dram_tensor("c_" + k, shp, dt, kind="ExternalInput").ap()
    out = nc.dram_tensor("out", [NSEQ, S_LEN, D], F32, kind="ExternalOutput").ap()
    wbf = {}
    for k in ('w_in', 'w_up', 'w_down', 'w_out', 'w_ple_gate', 'w_ple_proj'):
        wbf[k] = nc.dram_tensor(k + "_bf", WSHAPES[k], BF16, kind="Internal").ap()
    dbg_out = {}
    if debug:
        for name, shp in debug.items():
            dbg_out[name] = nc.dram_tensor("dbg_" + name, shp, F32, kind="ExternalOutput").ap()

    es = ExitStack()
    ARENA = 206 * 1024
    arena = es.enter_context(nc.sbuf_tensor("arena", [128, ARENA], U8))
    pbank = [es.enter_context(nc.psum_tensor(f"pb{i}", [128, 512], F32)) for i in range(7)]
    PB = es.enter_context(nc.psum_tensor("pbt", [128, 1024], BF16))
    P = [b[:] for b in pbank]
    PBa = PB[:]

    state = {'off': 0}

    def carve(shape, dt):
        esz = 4 if dt == F32 else 2
        n = int(np.prod(shape[1:]))
        off = state['off']
        nb = (n * esz + 63) // 64 * 64
        assert off + nb <= ARENA, ("SBUF arena overflow", off, nb)
        state['off'] = off + nb
        ap = arena[0:shape[0], off:off + n * esz].bitcast(dt)
        if len(shape) == 3:
            ap = ap.rearrange("p (a b) -> p a b", a=shape[1])
        elif len(shape) == 4:
            ap = ap.rearrange("p (a b c) -> p a b c", a=shape[1], b=shape[2])
        return ap

    S = Sched(nc)

    def mm(o, lhsT, rhs, start, stop, r, w, **kw):
        S.add('pe', lambda e: e.matmul(o, lhsT=lhsT, rhs=rhs, start=start, stop=stop, **kw), r=r, w=w)

    def tr(o, in_, r, w):
        S.add('pe', lambda e: e.transpose(out=o, in_=in_, identity=ident), r=list(r) + ['ident'], w=w)

    def act(o, in_, func, r, w, **kw):
        S.add('act', lambda e: e.activation(out=o, in_=in_, func=func, **kw), r=r, w=w)

    def tt(eng, o, a, b, op, r, w):
        S.add(eng, lambda e: e.tensor_tensor(out=o, in0=a, in1=b, op=op), r=r, w=w)

    def stt(eng, o, a, sc, b, op0, op1, r, w):
        S.add(eng, lambda e: e.scalar_tensor_tensor(out=o, in0=a, scalar=sc, in1=b, op0=op0, op1=op1), r=r, w=w)

    def ts(eng, o, a, s1, s2, op0, op1, r, w):
        if op1 is None:
            S.add(eng, lambda e: e.tensor_scalar(out=o, in0=a, scalar1=s1, scalar2=None, op0=op0), r=r, w=w)
        else:
            S.add(eng, lambda e: e.tensor_scalar(out=o, in0=a, scalar1=s1, scalar2=s2, op0=op0, op1=op1), r=r, w=w)

    def cp(eng, o, a, r, w):
        S.add(eng, lambda e: e.tensor_copy(out=o, in_=a), r=r, w=w)

    def recip(o, a, r, w):
        S.add('dve', lambda e: e.reciprocal(out=o, in_=a), r=r, w=w)

    def memset(eng, o, v, w):
        S.add(eng, lambda e: e.memset(o, v), r=(), w=w)

    def dma(eng, o, in_, r, w, **kw):
        S.add(eng, lambda e: e.dma_start(out=o, in_=in_, **kw), r=r, w=w, dma=True)

    def dump(name, ap, r):
        if name in dbg_out:
            dma('pool', dbg_out[name], ap, r=r, w=[])

    ident = carve([128, 128], BF16)
    ones = carve([128, 128], BF16)
    mixedT = carve([128, 8, S_LEN], BF16)
    g_mix = carve([128, 8], F32); g_ffn = carve([128, 8], F32); g_ple = carve([128, 8], F32)
    g_gc = carve([128, 4], F32); g_ga = carve([128, 4], F32)
    xs = [carve([128, D], BF16) for _ in range(2)]
    nTb = [carve([128, 8, 512], BF16) for _ in range(2)]
    nT = nTb[0]
    ss = [carve([128, 1], F32) for _ in range(2)]
    rr = [carve([128, 1], F32) for _ in range(2)]
    junk = carve([128, D], BF16)
    dma('sp', ident, dr['ident'], r=[], w=['ident'])
    dma('sp', ones, dr['ones'], r=[], w=['ones'])
    for nm, tl, n in (('g_mix', g_mix, 8), ('g_ffn', g_ffn, 8), ('g_ple', g_ple, 8), ('g_gn_conv', g_gc, 4), ('g_gn_attn', g_ga, 4)):
        dma('sp', tl, dr[nm].rearrange("(c p) -> p c", p=128), r=[], w=[nm], allow_slow_non_contiguous=True)
    base_off = state['off']
    cnt = {'x': 0, 'n': 0}
    def cast_w(k, inner, rsplit=1, gate=()):
        R = WSHAPES[k][0]
        step = R // rsplit
        for i in range(rsplit):
            rs = slice(i * step, (i + 1) * step)
            dma('pool', wbf[k][rs, :].rearrange("r (a b) -> r a b", b=inner), dr[k][rs, :].rearrange("r (a b) -> r a b", b=inner),
                r=list(gate), w=[k + '_bf'])
    for blk in (4, 0, 1, 2, 3):
        dma('pool', wbf['w_in'][:, blk * 568:(blk + 1) * 568], dr['w_in'][:, blk * 568:(blk + 1) * 568], r=[], w=[('w_in_bf', blk)])

    def win_res(c0, n):
        return [('w_in_bf', b) for b in range(c0 // 568, (c0 + n - 1) // 568 + 1)]

    def rmsnorm_T(src, src_res, gt, g_res, tsl, nb=0):
        k = cnt['n'] % 2; cnt['n'] += 1
        act(junk, src, AF.Square, r=[src_res], w=['junk', ('ss', k)], scale=1.0 / 32, accum_out=ss[k])
        act(rr[k], ss[k], AF.Sqrt, r=[('ss', k)], w=[('rr', k)], bias=EPS, scale=1.0)
        recip(rr[k], rr[k], r=[('rr', k)], w=[('rr', k)])
        act(xs[k], src, AF.Copy, r=[src_res, ('rr', k)], w=[('xs', k)], scale=rr[k])
        for c in range(8):
            tr(PBa[:, c * 128:(c + 1) * 128], xs[k][:, c * 128:(c + 1) * 128], r=[('xs', k)], w=['PB'])
        tt('dve', nTb[nb][:, :, tsl], PBa.rearrange("p (c t) -> p c t", c=8), gt.unsqueeze(2).to_broadcast([128, 8, 128]),
           ALU.mult, r=['PB', g_res], w=[('nT', nb)])

    def phase1(s):
        state['off'] = base_off
        qT = carve([128, 4, S_LEN], BF16)
        ksz = [carve([128, S_LEN], BF16) for _ in range(2)]; kwz = [carve([128, S_LEN], BF16) for _ in range(2)]
        kcmpT = carve([128, S_LEN], BF16); vcmpT = carve([128, S_LEN], BF16)
        vs_ext = carve([128, NT, 2, 65], BF16); vw_ext = carve([128, NT, 2, 65], BF16)
        gates = carve([128, NT, 24], F32)
        w1blk = carve([128, 32, 128], BF16)
        w1d = carve([128, 16, 128], BF16)
        pecol = carve([128, 16], BF16)
        w2blk = carve([128, 128], BF16)
        c1 = carve([128, 1], F32)
        hidT = carve([128, 128], BF16)
        kcraw = carve([128, 128], BF16)
        kcz = [carve([128, 128], BF16) for _ in range(2)]
        vc_ext = carve([128, 2, 97], BF16)
        cosT = carve([128, S_LEN], BF16); sinT = carve([128, S_LEN], BF16)
        coscT = carve([128, 127], BF16); sincT = carve([128, 127], BF16)
        maskc = carve([128, 16, 128], BF16)
        selbias = carve([128, 16, 32], F32)
        eall = carve([128, 16, 128], BF16)
        rblk = carve([128, 128], BF16); causal = carve([128, 128], BF16); lower = carve([128, 128], BF16)
        ovt = carve([128, 32], BF16)
        cw = carve([128, 3, 4], F32)
        xt = [carve([128, D], F32) for _ in range(2)]
        wfm = [carve([128, 8, 128], BF16) for _ in range(3)]
        wtm = carve([128, 8, 280], BF16)
        xin_sb = [carve([128, 512], F32) for _ in range(2)]
        zb = [carve([128, 514], F32) for _ in range(2)]
        zh = carve([128, 4, 2], F32)
        accb = [carve([128, 512], F32) for _ in range(2)]
        ycv = carve([128, 4, 512], F32)
        ysq = carve([128, 4, 512], BF16)
        rc = carve([128, 512], F32)
        qraw = [carve([128, 512], BF16) for _ in range(2)]
        t1 = [carve([128, 512], F32) for _ in range(2)]
        t2 = [carve([128, 512], F32) for _ in range(2)]
        pT = [carve([128, 512], BF16) for _ in range(4)]
        yatb = [carve([128, 8, 64], F32) for _ in range(2)]
        yab = carve([128, 512], BF16)
        tmpa = carve([128, 4, 64], F32)
        tmpw = carve([128, 4, 64], F32)
        sm = carve([128, 96], F32)
        score = carve([128, 32], F32)
        negsel = carve([128, 32], BF16)
        negselT = carve([128, 128], BF16)
        top8 = carve([128, 8], F32)

        for k3 in range(3):
            dma('sp', cw[:, k3, :], dr['w_conv_mix'][k3].rearrange("(j p) -> p j", p=128), r=[], w=['cw'], allow_slow_non_contiguous=True)
        memset('pool', vs_ext[:, :, :, 64:65], 1.0, w=['vs_ext'])
        memset('pool', vw_ext[:, :, :, 64:65], 1.0, w=['vw_ext'])
        memset('pool', vc_ext, 0.0, w=['vc_ext'])
        memset('pool', vc_ext[:, :, 64:65], 1.0, w=['vc_ext'])
        memset('pool', zh, 0.0, w=['zh'])
        memset('pool', ksz[0][64:128, :], 0.0, w=['ksT']); memset('pool', ksz[1][0:64, :], 0.0, w=['ksT'])
        memset('pool', kwz[0][64:128, :], 0.0, w=['kwT']); memset('pool', kwz[1][0:64, :], 0.0, w=['kwT'])
        memset('pool', kcz[0], 0.0, w=['kcT']); memset('pool', kcz[1], 0.0, w=['kcT'])
        memset('pool', negselT, 0.0, w=['negselT'])

        w_in_v = wbf['w_in'].rearrange("(c p) n -> p c n", p=128)
        xsrc = dr['x']
        fmk = {'k': 0, 'pk': 0, 'nb': 0, 'gi': 0}
        FMB = [0, 1, 2, 5, 6]

        def fm_chunk(col_pieces):
            k = fmk['k'] % 3; fmk['k'] += 1
            pk = FMB[fmk['pk'] % 5]; fmk['pk'] += 1
            o = 0
            for (c0, n) in col_pieces:
                dma('sp', wfm[k][:, :, o:o + n], w_in_v[:, :, c0:c0 + n], r=win_res(c0, n), w=[('wfm', k)])
                o += n
            for kc in range(8):
                mm(P[pk], lhsT=wfm[k][:, kc, :], rhs=nTb[fmk['nb']][:, kc, :], start=(kc == 0), stop=(kc == 7),
                   r=[('wfm', k), ('nT', fmk['nb'])], w=[('P', pk)])
            if pend:
                fn_, g_, t_ = pend.pop(0)
                fn_(g_, t_)
            return pk

        def rope(pk, dst, dst_res, tsl, cT, sT, cres, n=512):
            kq = cnt['x'] % 2; cnt['x'] += 1
            act(qraw[kq][:, 0:n], P[pk][:, 0:n], AF.Copy, r=[('P', pk)], w=[('qraw', kq)])
            mm(P[3][:, 0:n], lhsT=rblk, rhs=qraw[kq][:, 0:n], start=True, stop=True, r=['rblk', ('qraw', kq)], w=[('P', 3)])
            tt('dve', t1[kq][:, 0:n], P[pk][:, 0:n], cT, ALU.mult, r=[('P', pk), cres[0]], w=[('t1', kq)])
            tt('dve', t2[kq][:, 0:n], P[3][:, 0:n], sT, ALU.mult, r=[('P', 3), cres[1]], w=[('t2', kq)])
            if isinstance(dst, list):
                for (d_ap, ps_) in dst:
                    tt('dve', d_ap, t1[kq][ps_, 0:n], t2[kq][ps_, 0:n], ALU.add, r=[('t1', kq), ('t2', kq)], w=[dst_res])
            else:
                tt('dve', dst, t1[kq][:, 0:n], t2[kq][:, 0:n], ALU.add, r=[('t1', kq), ('t2', kq)], w=[dst_res])

        dma('sp', wtm[:, :, 0:128], w_in_v[:, :, 2432:2560], r=win_res(2432, 128), w=['wtm'])
        dma('sp', wtm[:, :, 128:256], w_in_v[:, :, 2688:2816], r=win_res(2688, 128), w=['wtm'])
        dma('sp', wtm[:, :, 256:280], w_in_v[:, :, 2816:2840], r=win_res(2816, 24), w=['wtm'])

        def tf_a(gi, t4):
            ti = gi * 4 + t4
            kx = ti % 2
            k = ti % 2
            dma('sp', xt[kx], xsrc[s, ti * 128:(ti + 1) * 128, :], r=[], w=[('xt', kx)])
            act(junk, xt[kx], AF.Square, r=[('xt', kx)], w=['junk', ('ss', k)], scale=1.0 / 32, accum_out=ss[k])
            act(rr[k], ss[k], AF.Sqrt, r=[('ss', k)], w=[('rr', k)], bias=EPS, scale=1.0)
            recip(rr[k], rr[k], r=[('rr', k)], w=[('rr', k)])
            act(xs[k], xt[kx], AF.Copy, r=[('xt', kx), ('rr', k)], w=[('xs', k)], scale=rr[k])

        def tf_b(gi, t4):
            nb = gi % 2
            k = (gi * 4 + t4) % 2
            for c in range(8):
                tr(PBa[:, c * 128:(c + 1) * 128], xs[k][:, c * 128:(c + 1) * 128], r=[('xs', k)], w=['PB'])
            tt('dve', nTb[nb][:, :, t4 * 128:(t4 + 1) * 128], PBa.rearrange("p (c t) -> p c t", c=8),
               g_mix.unsqueeze(2).to_broadcast([128, 8, 128]), ALU.mult, r=['PB', 'g_mix'], w=[('nT', nb)])

        def tf_c(gi, t4):
            nb = gi % 2
            ti = gi * 4 + t4
            for kc in range(8):
                mm(P[4][:, 0:280], lhsT=nTb[nb][:, kc, t4 * 128:(t4 + 1) * 128], rhs=wtm[:, kc, :], start=(kc == 0),
                   stop=(kc == 7), r=[('nT', nb), 'wtm'], w=[('P', 4)])
            act(vs_ext[:, ti, :, 0:64], P[4][:, 0:128].rearrange("p (h d) -> p h d", h=2), AF.Copy, r=[('P', 4)], w=['vs_ext'])
            act(vw_ext[:, ti, :, 0:64], P[4][:, 128:256].rearrange("p (h d) -> p h d", h=2), AF.Copy, r=[('P', 4)], w=['vw_ext'])
            act(gates[:, ti, :], P[4][:, 256:280], AF.Sigmoid, r=[('P', 4)], w=['gates'])

        def tile_front(gi, t4):
            tf_a(gi, t4); tf_b(gi, t4); tf_c(gi, t4)

        pend = []

        def front_stages(gi):
            st = []
            order = [('a', 0), ('a', 1), ('b', 0), ('a', 2), ('c', 0), ('b', 1), ('a', 3), ('c', 1), ('b', 2), ('c', 2), ('b', 3), ('c', 3)]
            fn = {'a': tf_a, 'b': tf_b, 'c': tf_c}
            for (kind, t4) in order:
                st.append((fn[kind], gi, t4))
            return st

        for t4 in range(4):
            tile_front(0, t4)
        for nm, tl in (('cosT', cosT), ('sinT', sinT), ('coscT', coscT), ('sincT', sincT), ('maskc', maskc),
                       ('selbias', selbias), ('eall', eall), ('rblk', rblk), ('causal', causal), ('lower', lower),
                       ('ov', ovt)):
            dma('sp', tl, dr[nm], r=[], w=[nm])
        for kvh in range(2):
            cp('pool', vc_ext[0:127, kvh, 65:97], ovt[0:127, :], r=['ov'], w=['vc_ext'])
        for gi in range(CFG['nga']):
            g0 = gi * 512
            fmk['nb'] = gi % 2
            fmk['gi'] = gi
            while pend:
                fn_, g_, t_ = pend.pop(0)
                fn_(g_, t_)
            if gi + 1 < CFG['nga']:
                pend.extend(front_stages(gi + 1))
            tsl = slice(g0, g0 + 512)
            if CFG['a_stage'] < 1:
                continue
            for j in range(4):
                kb = j % 2
                pk = fm_chunk([(128 * j, 128)])
                act(xin_sb[kb], P[pk], AF.Copy, r=[('P', pk)], w=[('xin', kb)])
                pk = fm_chunk([(1024 + 128 * j, 128)])
                tt('dve', zb[kb][:, 2:514], P[pk], xin_sb[kb], ALU.mult, r=[('P', pk), ('xin', kb)], w=[('zb', kb)])
                cp('dve', zb[kb][:, 0:2], zh[:, j, :], r=['zh'], w=[('zb', kb)])
                pk = fm_chunk([(512 + 128 * j, 128)])
                act(accb[kb], zb[kb][:, 0:512], AF.Copy, r=[('zb', kb), 'cw'], w=[('acc', kb)], scale=cw[:, 0, j:j + 1])
                stt('dve', accb[kb], zb[kb][:, 1:513], cw[:, 1, j:j + 1], accb[kb], ALU.mult, ALU.add, r=[('zb', kb), 'cw', ('acc', kb)], w=[('acc', kb)])
                stt('dve', accb[kb], zb[kb][:, 2:514], cw[:, 2, j:j + 1], accb[kb], ALU.mult, ALU.add, r=[('zb', kb), 'cw', ('acc', kb)], w=[('acc', kb)])
                cp('dve', zh[:, j, :], zb[kb][:, 512:514], r=[('zb', kb)], w=['zh'])
                tt('dve', ycv[:, j, :], P[pk], accb[kb], ALU.mult, r=[('P', pk), ('acc', kb)], w=[('ycv', j)])
            for j in range(4):
                act(ysq[:, j, :], ycv[:, j, :], AF.Square, r=[('ycv', j)], w=[('ysq', j)])
            for j in range(4):
                mm(P[3], lhsT=ones, rhs=ysq[:, j, :], start=(j == 0), stop=(j == 3), r=['ones', ('ysq', j)], w=[('P', 3)])
            act(rc, P[3], AF.Sqrt, r=[('P', 3)], w=['rc'], bias=EPS, scale=1.0 / 512)
            recip(rc, rc, r=['rc'], w=['rc'])
            for j in range(4):
                stt('dve', mixedT[:, j, tsl], ycv[:, j, :], g_gc[:, j:j + 1], rc, ALU.mult, ALU.mult,
                    r=[('ycv', j), 'g_gn_conv', 'rc'], w=['mixedT'])
            if CFG['a_stage'] < 2:
                continue
            for c in range(4):
                pk = fm_chunk([(1536 + 64 * c, 64), (1536 + 64 * (4 + c), 64)])
                rope(pk, qT[:, c, tsl], 'qT', tsl, cosT[:, tsl], sinT[:, tsl], ('cosT', 'sinT'))
            if CFG['a_stage'] < 3:
                continue
            pk = fm_chunk([(2304, 128)])
            rope(pk, [(ksz[0][0:64, tsl], slice(0, 64)), (ksz[1][64:128, tsl], slice(64, 128))], 'ksT', tsl, cosT[:, tsl], sinT[:, tsl], ('cosT', 'sinT'))
            pk = fm_chunk([(2560, 128)])
            rope(pk, [(kwz[0][0:64, tsl], slice(0, 64)), (kwz[1][64:128, tsl], slice(64, 128))], 'kwT', tsl, cosT[:, tsl], sinT[:, tsl], ('cosT', 'sinT'))
            pk = fm_chunk([(2048, 128)])
            act(kcmpT[:, tsl], P[pk], AF.Copy, r=[('P', pk)], w=['kcmpT'])
            pk = fm_chunk([(2176, 128)])
            act(vcmpT[:, tsl], P[pk], AF.Copy, r=[('P', pk)], w=['vcmpT'])

        for which, srcT, sres in ((('k', kcmpT, 'kcmpT'), ('v', vcmpT, 'vcmpT')) if CFG['do_b'] else ()):
            w1 = dr['cmp_w1_' + which]; w2 = dr['cmp_w2_' + which]; pe = dr['cmp_pe_' + which]
            memset('pool', w1blk, 0.0, w=['w1blk'])
            memset('pool', w2blk, 0.0, w=['w2blk'])
            w1v = w1.rearrange("(j d) h -> d j h", d=64)
            dma('pool', w1blk[0:64, :, 0:64], w1v, r=[], w=['w1blk'])
            dma('pool', w1blk[64:128, :, 64:128], w1v, r=[], w=['w1blk'])
            dma('pool', w2blk[0:64, 0:64], w2, r=[], w=['w2blk'])
            dma('pool', w2blk[64:128, 64:128], w2, r=[], w=['w2blk'])
            w1c = w1.rearrange("(i p) h -> p i h", p=128)
            dma('pool', w1d[:, :, 0:64], w1c, r=[], w=['w1d'])
            dma('pool', w1d[:, :, 64:128], w1c, r=[], w=['w1d'])
            dma('pool', pecol, pe.rearrange("(i a) d -> (a d) i", a=2), r=[], w=['pecol'],
                allow_slow_non_contiguous=True)
            for i in range(16):
                mm(P[6][:, 0:1], lhsT=w1d[:, i, :], rhs=pecol[:, i:i + 1], start=(i == 0), stop=(i == 15),
                   r=['w1d', 'pecol'], w=[('P', 6)])
            act(c1, P[6][:, 0:1], AF.Copy, r=[('P', 6)], w=['c1'])
            for j in range(32):
                mm(P[5][:, 0:127], lhsT=w1blk[:, j, :], rhs=srcT[:, j:j + 2017:16], start=(j == 0), stop=(j == 31),
                   r=['w1blk', sres], w=[('P', 5)])
            act(hidT[:, 0:127], P[5][:, 0:127], AF.Silu, r=[('P', 5), 'c1'], w=['hidT'], bias=c1)
            if which == 'k':
                mm(P[6][:, 0:127], lhsT=w2blk, rhs=hidT[:, 0:127], start=True, stop=True, r=['w2blk', 'hidT'], w=[('P', 6)])
                act(kcraw[:, 0:127], P[6][:, 0:127], AF.Copy, r=[('P', 6)], w=['kcraw'])
                mm(P[3][:, 0:127], lhsT=rblk, rhs=kcraw[:, 0:127], start=True, stop=True, r=['rblk', 'kcraw'], w=[('P', 3)])
                tt('dve', t1[0][:, 0:127], P[6][:, 0:127], coscT, ALU.mult, r=[('P', 6), 'coscT'], w=[('t1', 0)])
                tt('dve', t2[0][:, 0:127], P[3][:, 0:127], sincT, ALU.mult, r=[('P', 3), 'sincT'], w=[('t2', 0)])
                tt('dve', kcz[0][0:64, 0:127], t1[0][0:64, 0:127], t2[0][0:64, 0:127], ALU.add, r=[('t1', 0), ('t2', 0)], w=['kcT'])
                tt('dve', kcz[1][64:128, 0:127], t1[0][64:128, 0:127], t2[0][64:128, 0:127], ALU.add, r=[('t1', 0), ('t2', 0)], w=['kcT'])
            else:
                mm(P[6][0:127, 0:128], lhsT=hidT[:, 0:127], rhs=w2blk, start=True, stop=True, r=['w2blk', 'hidT'], w=[('P', 6)])
                act(vc_ext[0:127, :, 0:64], P[6][0:127, 0:128].rearrange("p (h d) -> p h d", h=2), AF.Copy,
                    r=[('P', 6)], w=['vc_ext'])
        if s == 0:
            cast_w('w_out', 1024, gate=['kcT'])
            cast_w('w_up', 512, rsplit=2, gate=['kcT'])
            cast_w('w_down', 1024, gate=['kcT'])
            cast_w('w_ple_gate', 1024, gate=['kcT'])
            cast_w('w_ple_proj', 1024, gate=['kcT'])
        dump('qT', qT[:, :, 0:512], r=['qT'])
        dump('mixc', mixedT[:, 0:4, 0:512], r=['mixedT'])

        sk = {'s': 0, 'p': 0, 'u': 0}
        fin_pending = []
        for i in range(CFG['nqt']):
            qsl = slice(i * 128, (i + 1) * 128)
            yat = yatb[i % 2]
            yres = ('yat', i % 2)
            for kvh in range(2):
                hp = slice(0, 64) if kvh == 0 else slice(64, 128)
                q_rhs = qT[:, :, qsl]

                def score_tile(kT_l, extra, kparts=128):
                    b = sk['s'] % 3; sk['s'] += 1
                    kp = sk['p'] % 4; sk['p'] += 1
                    n_ex = len(extra)
                    o3 = P[b][0:kparts, :].rearrange("p (h q) -> p h q", h=4)
                    mm(o3, lhsT=kT_l[0], rhs=q_rhs, start=True, stop=(n_ex == 0), r=[kT_l[1], 'qT'], w=[('P', b)])
                    for xi, (l, rhs_, rres) in enumerate(extra):
                        mm(o3, lhsT=l, rhs=rhs_, start=False, stop=(xi == n_ex - 1), r=rres, w=[('P', b)])
                    act(pT[kp][0:kparts, :], P[b][0:kparts, :], AF.Exp, r=[('P', b)], w=[('pT', kp)], scale=0.125)
                    return kp

                gv = gates[:, i, :].rearrange("p (h b) -> p h b", b=3)
                yv = yat[:, kvh * 4:(kvh + 1) * 4, :]
                ccc = sm[:, 8:12]; ccs = sm[:, 12:16]; ccw = sm[:, 16:20]
                owv = P[5][:, 0:260].rearrange("p (h c) -> p h c", h=4)
                ocb = 3 if (sk['u'] % 2 == 0) else 6
                sk['u'] += 1
                oc = P[ocb][:, 0:388].rearrange("p (h c) -> p h c", h=4)
                rdc = sm[:, 0:4]
                nsb = negselT.unsqueeze(1).to_broadcast([128, 4, 128])
                cb = causal.unsqueeze(1).to_broadcast([128, 4, 128])
                lb = lower.unsqueeze(1).to_broadcast([128, 4, 128])
                mk = maskc[:, i, :].unsqueeze(1).to_broadcast([128, 4, 128])

                def pv_c(kp):
                    for hb in range(4):
                        mm(P[ocb][:, hb * 97:(hb + 1) * 97], lhsT=pT[kp][:, hb * 128:(hb + 1) * 128], rhs=vc_ext[:, kvh, :],
                           start=True, stop=True, r=[('pT', kp), 'vc_ext'], w=[('P', ocb)])
                    ts('dve', rdc, oc[:, :, 64], 1e-30, None, ALU.max, None, r=[('P', ocb)], w=['rdc'])
                    recip(rdc, rdc, r=['rdc'], w=['rdc'])
                    ts('dve', score, oc[:, 0, 65:97], rdc[:, 0:1], None, ALU.mult, None, r=[('P', ocb), 'rdc'], w=['score'])
                    for hb in range(1, 4):
                        stt('dve', score, oc[:, hb, 65:97], rdc[:, hb:hb + 1], score, ALU.mult, ALU.add,
                            r=[('P', ocb), 'rdc', 'score'], w=['score'])
                    tt('dve', score, score, selbias[:, i, :], ALU.add, r=['score', 'selbias'], w=['score'])
                    S.add('dve', lambda e: e.max(out=top8, in_=score), r=['score'], w=['top8'])
                    ts('dve', score, score, top8[:, 7:8], None, ALU.is_ge, None, r=['score', 'top8'], w=['score'])
                    ts('dve', negsel, score, 1.0, -NEGB, ALU.subtract, ALU.mult, r=['score'], w=['negsel'])
                    tr(PBa[0:32, 0:128], negsel, r=['negsel'], w=['PB'])
                    cp('dve', negselT[0:32, :], PBa[0:32, 0:128], r=['PB'], w=['negselT'])
                    tt('dve', ccc, rdc, gv[:, kvh * 4:(kvh + 1) * 4, 0], ALU.mult, r=['rdc', 'gates'], w=['ccc'])
                    tt('dve', yv, oc[:, :, 0:64], ccc.unsqueeze(2).to_broadcast([128, 4, 64]), ALU.mult, r=[('P', ocb), 'ccc'], w=[yres])

                def win_done():
                    ts('dve', ccw, owv[:, :, 64], 1e-30, None, ALU.max, None, r=[('P', 5)], w=['ccw'])
                    recip(ccw, ccw, r=['ccw'], w=['ccw'])
                    tt('dve', ccw, ccw, gv[:, kvh * 4:(kvh + 1) * 4, 2], ALU.mult, r=['ccw', 'gates'], w=['ccw'])
                    tt('dve', tmpw, owv[:, :, 0:64], ccw.unsqueeze(2).to_broadcast([128, 4, 64]), ALU.mult, r=[('P', 5), 'ccw'], w=['tmpw'])
                    tt('dve', yv, yv, tmpw, ALU.add, r=[yres, 'tmpw'], w=[yres])

                def mk_pv(bank, vext, vres, j, first, last):
                    def pv(kp):
                        for hb in range(4):
                            mm(P[bank][:, hb * 65:(hb + 1) * 65], lhsT=pT[kp][:, hb * 128:(hb + 1) * 128], rhs=vext[:, j, kvh, :],
                               start=(first and hb == 0), stop=(hb == 3), r=[('pT', kp), vres], w=[('P', bank)],
                               skip_group_check=True)
                        if last and bank == 5:
                            win_done()
                    return pv

                tasks = [(((kcz[kvh], 'kcT'), [(ident, mk, ['ident', 'maskc'])], 128), pv_c)]
                j0 = max(0, i - 4)
                for j in range(j0, i + 1):
                    ex = []
                    if j == i:
                        ex.append((ident, cb, ['ident', 'causal']))
                    if j == i - 4:
                        ex.append((ident, lb, ['ident', 'lower']))
                    tasks.append((((kwz[kvh][:, j * 128:(j + 1) * 128], 'kwT'), ex, 128), mk_pv(5, vw_ext, 'vw_ext', j, j == j0, j == i)))
                for j in range(i + 1):
                    ex = [(eall[:, j, :], nsb, ['eall', 'negselT'])]
                    if j == i:
                        ex.append((ident, cb, ['ident', 'causal']))
                    tasks.append((((ksz[kvh][:, j * 128:(j + 1) * 128], 'ksT'), ex, 128), mk_pv(4, vs_ext, 'vs_ext', j, j == 0, j == i)))
                prev = None
                for ti_, (sargs, pvf) in enumerate(tasks):
                    kp = score_tile(sargs[0], sargs[1], kparts=sargs[2])
                    if prev is not None:
                        prev[0](prev[1])
                    prev = (pvf, kp)
                    if kvh == 0 and ti_ == min(3, len(tasks) - 1) and fin_pending:
                        fin_pending.pop(0)()
                prev[0](prev[1])
                osv = P[4][:, 0:260].rearrange("p (h c) -> p h c", h=4)
                ts('dve', ccs, osv[:, :, 64], 1e-30, None, ALU.max, None, r=[('P', 4)], w=['ccs'])
                recip(ccs, ccs, r=['ccs'], w=['ccs'])
                tt('dve', ccs, ccs, gv[:, kvh * 4:(kvh + 1) * 4, 1], ALU.mult, r=['ccs', 'gates'], w=['ccs'])
                tt('dve', tmpa, osv[:, :, 0:64], ccs.unsqueeze(2).to_broadcast([128, 4, 64]), ALU.mult, r=[('P', 4), 'ccs'], w=['tmpa'])
                tt('dve', yv, yv, tmpa, ALU.add, r=[yres, 'tmpa'], w=[yres])
            def make_fin(i=i, qsl=qsl, yat=yat, yres=yres):
                def fin():
                    k = cnt['n'] % 2; cnt['n'] += 1
                    yflat = yat.rearrange("p h d -> p (h d)")
                    if i == 0:
                        dump('yat0', yflat, r=[yres])
                    act(junk[:, 0:512], yflat, AF.Square, r=[yres], w=['junk', ('ss', k)], scale=float(512 ** -0.5), accum_out=ss[k])
                    act(rr[k], ss[k], AF.Sqrt, r=[('ss', k)], w=[('rr', k)], bias=EPS, scale=1.0)
                    recip(rr[k], rr[k], r=[('rr', k)], w=[('rr', k)])
                    act(yab, yflat, AF.Copy, r=[yres, ('rr', k)], w=['yab'], scale=rr[k])
                    for c in range(4):
                        tr(PBa[:, c * 128:(c + 1) * 128], yab[:, c * 128:(c + 1) * 128], r=['yab'], w=['PB'])
                    tt('dve', mixedT[:, 4:8, qsl], PBa[:, 0:512].rearrange("p (c t) -> p c t", c=4),
                       g_ga.unsqueeze(2).to_broadcast([128, 4, 128]), ALU.mult, r=['PB', 'g_gn_attn'], w=['mixedT'])
                return fin
            fin_pending.append(make_fin())
            if i == CFG['nqt'] - 1:
                while fin_pending:
                    fin_pending.pop(0)()
        dump('mixa', mixedT[:, 4:8, 0:512], r=['mixedT'])

    def phase2(s):
        state['off'] = base_off
        h = [carve([128, D], F32) for _ in range(4)]
        actT = carve([128, NF, 512], BF16)
        abuf = [carve([128, 514], F32) for _ in range(4)]
        ub = [carve([128, 512], F32) for _ in range(4)]
        sg = [carve([128, 512], F32) for _ in range(2)]
        wub = [carve([128, 8, 256], BF16) for _ in range(4)]
        wdb = [carve([128, 512], BF16) for _ in range(8)]
        w_out = carve([128, 8, D], BF16)
        wpg = carve([128, 8, D], BF16)
        wpp = carve([128, 2, D], BF16)
        gfin = carve([128, D], F32)
        outt = [carve([128, D], F32) for _ in range(2)]
        pt = [carve([128, 256], F32) for _ in range(2)]
        pbf = [carve([128, 256], BF16) for _ in range(2)]
        ppT = carve([128, 2, 512], BF16)
        sgm = [carve([128, 512], F32) for _ in range(2)]
        tmp = [carve([128, 512], F32) for _ in range(2)]
        hal = carve([128, 2 * NF, 2], F32)
        cwf = carve([128, 3, 2 * NF], F32)

        dma('sp', w_out, wbf['w_out'].rearrange("(c p) n -> p c n", p=128), r=['w_out_bf'], w=['w_out'])
        dma('sp', wpg, wbf['w_ple_gate'].rearrange("(c p) n -> p c n", p=128), r=['w_ple_gate_bf'], w=['wpg'])
        dma('sp', wpp, wbf['w_ple_proj'].rearrange("(c p) n -> p c n", p=128), r=['w_ple_proj_bf'], w=['wpp'])
        dma('sp', gfin, dr['g_final'].partition_broadcast(128), r=[], w=['gfin'])
        for k3 in range(3):
            dma('sp', cwf[:, k3, :], dr['w_ffn_conv'][k3].rearrange("(f p) -> p f", p=128), r=[], w=['cwf'], allow_slow_non_contiguous=True)
        memset('pool', hal, 0.0, w=['hal'])
        w_up_v = wbf['w_up'].rearrange("(c p) n -> p c n", p=128)
        w_dn_v = wbf['w_down'].rearrange("(f p) n -> p f n", p=128)
        ck = {'u': 0, 'd': 0, 'pp': 0, 'ab': 0, 'o': 0, 'sg': 0}

        for gi in range(CFG['ng2']):
            g0 = gi * 512
            for t4 in range(4):
                tok = slice(g0 + t4 * 128, g0 + (t4 + 1) * 128)
                dma('sp', h[t4], dr['x'][s, tok, :], r=[], w=[('h', t4)])
                for half in range(2):
                    pk = ck['pp'] % 4; ck['pp'] += 1
                    for kc in range(8):
                        mm(P[pk], lhsT=mixedT[:, kc, tok], rhs=w_out[:, kc, half * 512:(half + 1) * 512], start=(kc == 0),
                           stop=(kc == 7), r=['mixedT', 'w_out'], w=[('P', pk)])
                    hs = h[t4][:, half * 512:(half + 1) * 512]
                    tt('dve', hs, P[pk], hs, ALU.add, r=[('P', pk), ('h', t4)], w=[('h', t4)])
                if t4 >= 1:
                    rmsnorm_T(h[t4 - 1], ('h', t4 - 1), g_ffn, 'g_ffn', slice((t4 - 1) * 128, t4 * 128))
            rmsnorm_T(h[3], ('h', 3), g_ffn, 'g_ffn', slice(3 * 128, 4 * 128))
            def up_head(f):
                k = ck['u'] % 4; ck['u'] += 1
                dma('sp', wub[k][:, :, 0:128], w_up_v[:, :, f * 128:(f + 1) * 128], r=['w_up_bf'], w=[('wub', k)])
                dma('sp', wub[k][:, :, 128:256], w_up_v[:, :, DFF + f * 128:DFF + (f + 1) * 128], r=['w_up_bf'], w=[('wub', k)])
                pg = (ck['pp'] % 3) * 2; ck['pp'] += 1
                us = []
                for hv in range(2):
                    pk = pg + hv
                    for kc in range(8):
                        mm(P[pk], lhsT=wub[k][:, kc, hv * 128:(hv + 1) * 128], rhs=nT[:, kc, :], start=(kc == 0), stop=(kc == 7),
                           r=[('wub', k), ('nT', 0)], w=[('P', pk)])
                    a = ck['ab'] % 4; ck['ab'] += 1
                    us.append((a, hv * NF + f, pk))
                for (a, fi, pk) in us:
                    act(abuf[a][:, 2:514], P[pk], AF.Copy, r=[('P', pk)], w=[('abuf', a)])
                    cp('dve', abuf[a][:, 0:2], hal[:, fi, :], r=['hal'], w=[('abuf', a)])
                return us

            def up_mid(us):
                for (a, fi, pk) in us:
                    act(ub[a], abuf[a][:, 0:512], AF.Copy, r=[('abuf', a), 'cwf'], w=[('ub', a)], scale=cwf[:, 0, fi:fi + 1])
                    cp('dve', hal[:, fi, :], abuf[a][:, 512:514], r=[('abuf', a)], w=['hal'])
                for tap in (1, 2):
                    for (a, fi, pk) in us:
                        stt('dve', ub[a], abuf[a][:, tap:tap + 512], cwf[:, tap, fi:fi + 1], ub[a], ALU.mult, ALU.add,
                            r=[('abuf', a), 'cwf', ('ub', a)], w=[('ub', a)])

            def up_tail(f, us):
                q = ck['sg'] % 2; ck['sg'] += 1
                act(sg[q], ub[us[0][0]], AF.Silu, r=[('ub', us[0][0])], w=[('sg', q)])
                tt('dve', actT[:, f, :], sg[q], ub[us[1][0]], ALU.mult, r=[('sg', q), ('ub', us[1][0])], w=['actT'])

            hist = {}
            for f in range(NF + 2):
                if f < NF:
                    hist[f] = up_head(f)
                if 0 <= f - 1 < NF:
                    up_mid(hist[f - 1])
                if 0 <= f - 2 < NF:
                    up_tail(f - 2, hist[f - 2])
            for half in range(2):
                banks = [0, 1, 2, 3] if half == 0 else [4, 5, 6, 0]
                for f in range(NF):
                    k = ck['d'] % 8; ck['d'] += 1
                    dma('sp', wdb[k], w_dn_v[:, f, half * 512:(half + 1) * 512], r=['w_down_bf'], w=[('wdb', k)])
                    for t4 in range(4):
                        mm(P[banks[t4]], lhsT=actT[:, f, t4 * 128:(t4 + 1) * 128], rhs=wdb[k], start=(f == 0), stop=True,
                           r=['actT', ('wdb', k)], w=[('P', banks[t4])], skip_group_check=True)
                for t4 in range(4):
                    hs = h[t4][:, half * 512:(half + 1) * 512]
                    tt('dve', hs, P[banks[t4]], hs, ALU.add, r=[('P', banks[t4]), ('h', t4)], w=[('h', t4)])
            for t4 in range(4):
                tok = slice(g0 + t4 * 128, g0 + (t4 + 1) * 128)
                rmsnorm_T(h[t4], ('h', t4), g_ple, 'g_ple', slice(t4 * 128, (t4 + 1) * 128))
                kp = t4 % 2
                dma('sp', pt[kp], dr['p'][s, tok, :], r=[], w=[('pt', kp)])
                act(pbf[kp], pt[kp], AF.Copy, r=[('pt', kp)], w=[('pbf', kp)])
                for c in range(2):
                    tr(PBa[:, c * 128:(c + 1) * 128], pbf[kp][:, c * 128:(c + 1) * 128], r=[('pbf', kp)], w=['PB'])
                cp('dve', ppT[:, :, t4 * 128:(t4 + 1) * 128], PBa[:, 0:256].rearrange("p (c t) -> p c t", c=2), r=['PB'], w=['ppT'])
            for t4 in range(4):
                for half in range(2):
                    pk = (ck['pp'] % 3) * 2; ck['pp'] += 1
                    cs = slice(half * 512, (half + 1) * 512)
                    for kc in range(8):
                        mm(P[pk], lhsT=nT[:, kc, t4 * 128:(t4 + 1) * 128], rhs=wpg[:, kc, cs], start=(kc == 0), stop=(kc == 7),
                           r=[('nT', 0), 'wpg'], w=[('P', pk)])
                    for kc in range(2):
                        mm(P[pk + 1], lhsT=ppT[:, kc, t4 * 128:(t4 + 1) * 128], rhs=wpp[:, kc, cs], start=(kc == 0), stop=(kc == 1),
                           r=['ppT', 'wpp'], w=[('P', pk + 1)])
                    q = ck['sg'] % 2; ck['sg'] += 1
                    act(sgm[q], P[pk], AF.Sigmoid, r=[('P', pk)], w=[('sgm', q)])
                    tt('dve', tmp[q], P[pk + 1], sgm[q], ALU.mult, r=[('P', pk + 1), ('sgm', q)], w=[('tmp', q)])
                    tt('dve', h[t4][:, cs], h[t4][:, cs], tmp[q], ALU.add, r=[('h', t4), ('tmp', q)], w=[('h', t4)])
            for t4 in range(4):
                tok = slice(g0 + t4 * 128, g0 + (t4 + 1) * 128)
                k = cnt['n'] % 2; cnt['n'] += 1
                o = ck['o'] % 2; ck['o'] += 1
                act(junk, h[t4], AF.Square, r=[('h', t4)], w=['junk', ('ss', k)], scale=1.0 / 32, accum_out=ss[k])
                act(rr[k], ss[k], AF.Sqrt, r=[('ss', k)], w=[('rr', k)], bias=EPS, scale=1.0)
                recip(rr[k], rr[k], r=[('rr', k)], w=[('rr', k)])
                stt('dve', outt[o], h[t4], rr[k], gfin, ALU.mult, ALU.mult, r=[('h', t4), ('rr', k), 'gfin'], w=[('outt', o)])
                dma('sp', out[s, tok, :], outt[o], r=[('outt', o)], w=[])

    for s in range(CFG['nseq']):
        phase1(s)
        S.barrier()
        if CFG['do_p2']:
            phase2(s)
            S.barrier()
    S.run()
    es.close()
    return nc


_CACHE = {}


def kernel(**inputs):
    ncores = 8
    consts = host_consts()
    if 'nc' not in _CACHE:
        _CACHE['nc'] = build_program()
    nc = _CACHE['nc']
    x = np.ascontiguousarray(np.asarray(inputs['x'], dtype=np.float32))
    p = np.ascontiguousarray(np.asarray(inputs['p'], dtype=np.float32))
    in_maps = []
    for c in range(ncores):
        m = {'x': x[NSEQ * c:NSEQ * (c + 1)], 'p': p[0, NSEQ * c:NSEQ * (c + 1)]}
        for k in WSHAPES:
            a = np.asarray(inputs[k], dtype=np.float32)
            if k != 'g_final':
                a = a[0]
            m[k] = np.ascontiguousarray(a)
        for k, v in consts.items():
            m['c_' + k] = v
        in_maps.append(m)
    res = run_bass_kernel_spmd(nc, in_maps, core_ids=list(range(ncores)))
    outs = [np.asarray(r['out'], dtype=np.float32) for r in res.results]
    return np.concatenate(outs, axis=0)
```

```python
import numpy as np
import ml_dtypes
from contextlib import ExitStack
import concourse.bass as bass
import concourse.mybir as mybir
from concourse.bass_utils import run_bass_kernel_spmd

F32 = mybir.dt.float32
BF16 = mybir.dt.bfloat16
U8 = mybir.dt.uint8
ALU = mybir.AluOpType
AF = mybir.ActivationFunctionType

ENGS = ['pe', 'act', 'dve', 'pool', 'sp']
EPOCH = 12000
NDSEM = 16

D = 1024
S_LEN = 2048
NSEQ = 2
NT = 16
DFF = 2816
NF = 22
NEGB = -30000.0
EPS = 1e-6
CFG = dict(tilebar=False, ntile=4, tm=True, a_stage=9, nseq=2, nga=4, do_b=True, nqt=16, do_p2=True, ng2=4)


class Op:
    __slots__ = ('eng', 'emit', 'deps', 'id', 'sig', 'dma', 'comp', 'dk')


class Sched:
    def __init__(self, nc):
        self.nc = nc
        self.ops = []
        self.last_w = {}
        self.readers = {}
        self.eng_list = {e: [] for e in ENGS}
        self.ndma = {e: 0 for e in ENGS}

    def add(self, eng, emit, r=(), w=(), dma=False):
        op = Op()
        op.eng = eng; op.emit = emit; op.id = len(self.ops); op.dma = dma
        op.sig = False; op.comp = None; op.dk = None
        deps = set()
        rw = set()
        for res in r:
            if res in self.last_w:
                deps.add(self.last_w[res]); rw.add(self.last_w[res])
            if res == 'PB' or (isinstance(res, tuple) and res[0] == 'P'):
                for x in self.readers.get(res, ()):
                    if self.ops[x].eng != eng:
                        deps.add(x)
        for res in w:
            if res in self.last_w:
                deps.add(self.last_w[res])
            deps.update(self.readers.get(res, ()))
        fd = set()
        for d in deps:
            dop = self.ops[d]
            if (not dma) and (not dop.dma) and dop.eng == eng:
                if eng == 'pe':
                    continue
            fd.add(d)
        op.deps = fd
        for res in r:
            lst = self.readers.setdefault(res, [])
            if not dma:
                lst[:] = [x for x in lst if self.ops[x].dma or self.ops[x].eng != eng]
            lst.append(op.id)
        for res in w:
            self.last_w[res] = op.id
            self.readers[res] = []
        if dma:
            op.dk = self.ndma[eng]
            self.ndma[eng] += 1
        self.ops.append(op)
        self.eng_list[eng].append(op)
        return op

    def barrier(self):
        n = len(self.ops)
        for e in ENGS:
            self.add(e, None, r=(), w=[('bar', n, e)])
        lastd = {}
        for op in self.ops:
            if op.dma:
                lastd[(op.eng, op.dk % NDSEM)] = op.id
        for e in ENGS:
            w = self.add(e, None, r=[('bar', n, e2) for e2 in ENGS], w=())
            w.deps.update(lastd.values())
        self.last_w.clear()
        self.readers.clear()

    def finalize(self):
        for op in self.ops:
            for d in op.deps:
                self.ops[d].sig = True
        self.nsem_eng = {}
        for e in ENGS:
            c = 0
            for op in self.eng_list[e]:
                if op.dma:
                    continue
                if op.sig:
                    c += 1
                    op.comp = ('c', e, (c - 1) // EPOCH, (c - 1) % EPOCH + 1)
            self.nsem_eng[e] = (c + EPOCH - 1) // EPOCH if c else 0
        for op in self.ops:
            if op.dma:
                op.comp = ('d', op.eng, op.dk % NDSEM, 16 * (op.dk // NDSEM + 1))

    def run(self):
        nc = self.nc
        self.finalize()
        es = ExitStack()
        sems = {}
        for e in ENGS:
            for i in range(self.nsem_eng[e]):
                sems[('c', e, i)] = es.enter_context(nc.semaphore(f"s_{e}_{i}"))
            for i in range(min(NDSEM, self.ndma[e])):
                sems[('d', e, i)] = es.enter_context(nc.semaphore(f"d_{e}_{i}"))
        block = es.enter_context(nc.Block())
        sched = self

        def body(ename):
            def f(eng):
                waited = {}
                cnt = {}
                for op in sched.eng_list[ename]:
                    need = {}
                    for d in op.deps:
                        k = sched.ops[d].comp
                        need[k[:3]] = max(need.get(k[:3], 0), k[3])
                    if op.dma and op.dk >= NDSEM:
                        key = ('d', ename, op.dk % NDSEM)
                        need[key] = max(need.get(key, 0), 16 * (op.dk // NDSEM))
                    for key, val in need.items():
                        if waited.get(key, 0) >= val:
                            continue
                        waited[key] = val
                        eng.wait_ge(sems[key], val)
                    if op.emit is None:
                        if op.sig:
                            eng.drain().then_inc(sems[op.comp[:3]], 1)
                        continue
                    ins = op.emit(eng)
                    if op.dma:
                        ins.then_inc(sems[op.comp[:3]], 16)
                        cnt[op.dk % NDSEM] = cnt.get(op.dk % NDSEM, 0) + 1
                    elif op.sig:
                        ins.then_inc(sems[op.comp[:3]], 1)
                for i, c in cnt.items():
                    key = ('d', ename, i)
                    if waited.get(key, 0) < 16 * c:
                        eng.wait_ge(sems[key], 16 * c)
            return f

        block.tensor(body('pe'))
        block.scalar(body('act'))
        block.vector(body('dve'))
        block.gpsimd(body('pool'))
        block.sync(body('sp'))
        es.close()


def host_consts():
    bf = ml_dtypes.bfloat16
    c = {}
    c['ident'] = np.eye(128, dtype=np.float32).astype(bf)
    k = np.arange(128)[:, None]; m = np.arange(128)[None, :]
    c['rblk'] = ((k // 64 == m // 64) & (k % 64 == (m % 64 + 32) % 64)).astype(np.float32).astype(bf)
    inv = (10000.0 ** (-np.arange(0, 64, 2, dtype=np.float32) / 64)).astype(np.float32)
    pr = np.arange(128) % 64
    fr = inv[pr % 32][:, None]
    sgn = np.where(pr < 32, -1.0, 1.0)[:, None].astype(np.float32)
    pos = np.arange(S_LEN, dtype=np.float32)[None, :]
    ang = (pos * fr).astype(np.float32)
    c['cosT'] = np.cos(ang).astype(np.float32).astype(bf)
    c['sinT'] = (np.sin(ang) * sgn).astype(np.float32).astype(bf)
    posc = (np.arange(127, dtype=np.float32) * 16 + 31)[None, :]
    angc = (posc * fr).astype(np.float32)
    c['coscT'] = np.cos(angc).astype(np.float32).astype(bf)
    c['sincT'] = (np.sin(angc) * sgn).astype(np.float32).astype(bf)
    cc = np.arange(128)[:, None, None]; ii = np.arange(16)[None, :, None]; qq = np.arange(128)[None, None, :]
    c['maskc'] = np.where(16 * cc + 31 <= 128 * ii + qq, 0.0, NEGB).astype(np.float32).astype(bf)
    qq2 = np.arange(128)[:, None, None]; ii2 = np.arange(16)[None, :, None]; nn = np.arange(32)[None, None, :]
    t = 128 * ii2 + qq2
    cur = t // 64
    valid = nn * 64 <= t
    forced = (nn == 0) | (nn == cur) | (nn == cur - 1)
    c['selbias'] = np.where(valid, np.where(forced, 1e4, 0.0), -1e30).astype(np.float32)
    b = np.arange(128)[:, None, None]; jj = np.arange(16)[None, :, None]; p = np.arange(128)[None, None, :]
    c['eall'] = (b == 2 * jj + p // 64).astype(np.float32).astype(bf)
    kk = np.arange(128)[:, None]; q = np.arange(128)[None, :]
    c['causal'] = np.where(kk > q, NEGB, 0.0).astype(np.float32).astype(bf)
    c['lower'] = np.where(kk <= q, NEGB, 0.0).astype(np.float32).astype(bf)
    cs = np.arange(127)[:, None] * 16; bs = np.arange(32)[None, :] * 64
    ov = np.clip(np.minimum(cs + 32, bs + 64) - np.maximum(cs, bs), 0, None).astype(np.float32) / 32
    c['ov'] = np.concatenate([ov, np.zeros((1, 32), np.float32)], 0).astype(bf)
    c['ones'] = np.ones((128, 128), np.float32).astype(bf)
    return c


CONST_SHAPES = {
    'ident': ([128, 128], BF16), 'rblk': ([128, 128], BF16), 'cosT': ([128, S_LEN], BF16),
    'sinT': ([128, S_LEN], BF16), 'coscT': ([128, 127], BF16), 'sincT': ([128, 127], BF16),
    'maskc': ([128, 16, 128], BF16), 'selbias': ([128, 16, 32], F32), 'eall': ([128, 16, 128], BF16),
    'causal': ([128, 128], BF16), 'lower': ([128, 128], BF16), 'ov': ([128, 32], BF16),
    'ones': ([128, 128], BF16),
}

WSHAPES = {
    'g_mix': [D], 'w_in': [D, 2840], 'w_conv_mix': [3, 512], 'cmp_pe_k': [32, 64], 'cmp_w1_k': [2048, 64],
    'cmp_w2_k': [64, 64], 'cmp_pe_v': [32, 64], 'cmp_w1_v': [2048, 64], 'cmp_w2_v': [64, 64],
    'g_gn_conv': [512], 'g_gn_attn': [512], 'w_out': [D, D], 'g_ffn': [D], 'w_up': [D, 2 * DFF],
    'w_ffn_conv': [3, 2 * DFF], 'w_down': [DFF, D], 'g_ple': [D], 'w_ple_gate': [D, D],
    'w_ple_proj': [256, D], 'g_final': [D],
}


def build_program(debug=None):
    nc = bass.Bass("TRN2", target_bir_lowering=False)
    dr = {}
    dr['x'] = nc.dram_tensor("x", [NSEQ, S_LEN, D], F32, kind="ExternalInput").ap()
    dr['p'] = nc.dram_tensor("p", [NSEQ, S_LEN, 256], F32, kind="ExternalInput").ap()
    for k, shp in WSHAPES.items():
        dr[k] = nc.dram_tensor(k, shp, F32, kind="ExternalInput").ap()
    for k, (shp, dt) in CONST_SHAPES.items():
        dr[k] = nc.dram_tensor("c_" + k, shp, dt, kind="ExternalInput").ap()
    out = nc.dram_tensor("out", [NSEQ, S_LEN, D], F32, kind="ExternalOutput").ap()
    wbf = {}
    for k in ('w_in', 'w_up', 'w_down', 'w_out', 'w_ple_gate', 'w_ple_proj'):
        wbf[k] = nc.dram_tensor(k + "_bf", WSHAPES[k], BF16, kind="Internal").ap()
    dbg_out = {}
    if debug:
        for name, shp in debug.items():
            dbg_out[name] = nc.dram_tensor("dbg_" + name, shp, F32, kind="ExternalOutput").ap()

    es = ExitStack()
    ARENA = 206 * 1024
    arena = es.enter_context(nc.sbuf_tensor("arena", [128, ARENA], U8))
    pbank = [es.enter_context(nc.psum_tensor(f"pb{i}", [128, 512], F32)) for i in range(7)]
    PB = es.enter_context(nc.psum_tensor("pbt", [128, 1024], BF16))
    P = [b[:] for b in pbank]
    PBa = PB[:]

    state = {'off': 0}

    def carve(shape, dt):
        esz = 4 if dt == F32 else 2
        n = int(np.prod(shape[1:]))
        off = state['off']
        nb = (n * esz + 63) // 64 * 64
        assert off + nb <= ARENA, ("SBUF arena overflow", off, nb)
        state['off'] = off + nb
        ap = arena[0:shape[0], off:off + n * esz].bitcast(dt)
        if len(shape) == 3:
            ap = ap.rearrange("p (a b) -> p a b", a=shape[1])
        elif len(shape) == 4:
            ap = ap.rearrange("p (a b c) -> p a b c", a=shape[1], b=shape[2])
        return ap

    S = Sched(nc)

    def mm(o, lhsT, rhs, start, stop, r, w, **kw):
        S.add('pe', lambda e: e.matmul(o, lhsT=lhsT, rhs=rhs, start=start, stop=stop, **kw), r=r, w=w)

    def tr(o, in_, r, w):
        S.add('pe', lambda e: e.transpose(out=o, in_=in_, identity=ident), r=list(r) + ['ident'], w=w)

    def act(o, in_, func, r, w, **kw):
        S.add('act', lambda e: e.activation(out=o, in_=in_, func=func, **kw), r=r, w=w)

    def tt(eng, o, a, b, op, r, w):
        S.add(eng, lambda e: e.tensor_tensor(out=o, in0=a, in1=b, op=op), r=r, w=w)

    def stt(eng, o, a, sc, b, op0, op1, r, w):
        S.add(eng, lambda e: e.scalar_tensor_tensor(out=o, in0=a, scalar=sc, in1=b, op0=op0, op1=op1), r=r, w=w)

    def ts(eng, o, a, s1, s2, op0, op1, r, w):
        if op1 is None:
            S.add(eng, lambda e: e.tensor_scalar(out=o, in0=a, scalar1=s1, scalar2=None, op0=op0), r=r, w=w)
        else:
            S.add(eng, lambda e: e.tensor_scalar(out=o, in0=a, scalar1=s1, scalar2=s2, op0=op0, op1=op1), r=r, w=w)

    def cp(eng, o, a, r, w):
        S.add(eng, lambda e: e.tensor_copy(out=o, in_=a), r=r, w=w)

    def recip(o, a, r, w):
        S.add('dve', lambda e: e.reciprocal(out=o, in_=a), r=r, w=w)

    def memset(eng, o, v, w):
        S.add(eng, lambda e: e.memset(o, v), r=(), w=w)

    def dma(eng, o, in_, r, w, **kw):
        S.add(eng, lambda e: e.dma_start(out=o, in_=in_, **kw), r=r, w=w, dma=True)

    def dump(name, ap, r):
        if name in dbg_out:
            dma('pool', dbg_out[name], ap, r=r, w=[])

    ident = carve([128, 128], BF16)
    ones = carve([128, 128], BF16)
    mixedT = carve([128, 8, S_LEN], BF16)
    g_mix = carve([128, 8], F32); g_ffn = carve([128, 8], F32); g_ple = carve([128, 8], F32)
    g_gc = carve([128, 4], F32); g_ga = carve([128, 4], F32)
    xs = [carve([128, D], BF16) for _ in range(2)]
    nTb = [carve([128, 8, 512], BF16) for _ in range(2)]
    nT = nTb[0]
    ss = [carve([128, 1], F32) for _ in range(2)]
    rr = [carve([128, 1], F32) for _ in range(2)]
    junk = carve([128, D], BF16)
    dma('sp', ident, dr['ident'], r=[], w=['ident'])
    dma('sp', ones, dr['ones'], r=[], w=['ones'])
    for nm, tl, n in (('g_mix', g_mix, 8), ('g_ffn', g_ffn, 8), ('g_ple', g_ple, 8), ('g_gn_conv', g_gc, 4), ('g_gn_attn', g_ga, 4)):
        dma('sp', tl, dr[nm].rearrange("(c p) -> p c", p=128), r=[], w=[nm], allow_slow_non_contiguous=True)
    base_off = state['off']
    cnt = {'x': 0, 'n': 0}
    def cast_w(k, inner, rsplit=1, gate=()):
        R = WSHAPES[k][0]
        step = R // rsplit
        for i in range(rsplit):
            rs = slice(i * step, (i + 1) * step)
            dma('pool', wbf[k][rs, :].rearrange("r (a b) -> r a b", b=inner), dr[k][rs, :].rearrange("r (a b) -> r a b", b=inner),
                r=list(gate), w=[k + '_bf'])
    for blk in (4, 0, 1, 2, 3):
        dma('pool', wbf['w_in'][:, blk * 568:(blk + 1) * 568], dr['w_in'][:, blk * 568:(blk + 1) * 568], r=[], w=[('w_in_bf', blk)])

    def win_res(c0, n):
        return [('w_in_bf', b) for b in range(c0 // 568, (c0 + n - 1) // 568 + 1)]

    def rmsnorm_T(src, src_res, gt, g_res, tsl, nb=0):
        k = cnt['n'] % 2; cnt['n'] += 1
        act(junk, src, AF.Square, r=[src_res], w=['junk', ('ss', k)], scale=1.0 / 32, accum_out=ss[k])
        act(rr[k], ss[k], AF.Sqrt, r=[('ss', k)], w=[('rr', k)], bias=EPS, scale=1.0)
        recip(rr[k], rr[k], r=[('rr', k)], w=[('rr', k)])
        act(xs[k], src, AF.Copy, r=[src_res, ('rr', k)], w=[('xs', k)], scale=rr[k])
        for c in range(8):
            tr(PBa[:, c * 128:(c + 1) * 128], xs[k][:, c * 128:(c + 1) * 128], r=[('xs', k)], w=['PB'])
        tt('dve', nTb[nb][:, :, tsl], PBa.rearrange("p (c t) -> p c t", c=8), gt.unsqueeze(2).to_broadcast([128, 8, 128]),
           ALU.mult, r=['PB', g_res], w=[('nT', nb)])

    def phase1(s):
        state['off'] = base_off
        qT = carve([128, 4, S_LEN], BF16)
        ksz = [carve([128, S_LEN], BF16) for _ in range(2)]; kwz = [carve([128, S_LEN], BF16) for _ in range(2)]
        kcmpT = carve([128, S_LEN], BF16); vcmpT = carve([128, S_LEN], BF16)
        vs_ext = carve([128, NT, 2, 65], BF16); vw_ext = carve([128, NT, 2, 65], BF16)
        gates = carve([128, NT, 24], F32)
        w1blk = carve([128, 32, 128], BF16)
        w1d = carve([128, 16, 128], BF16)
        pecol = carve([128, 16], BF16)
        w2blk = carve([128, 128], BF16)
        c1 = carve([128, 1], F32)
        hidT = carve([128, 128], BF16)
        kcraw = carve([128, 128], BF16)
        kcz = [carve([128, 128], BF16) for _ in range(2)]
        vc_ext = carve([128, 2, 97], BF16)
        cosT = carve([128, S_LEN], BF16); sinT = carve([128, S_LEN], BF16)
        coscT = carve([128, 127], BF16); sincT = carve([128, 127], BF16)
        maskc = carve([128, 16, 128], BF16)
        selbias = carve([128, 16, 32], F32)
        eall = carve([128, 16, 128], BF16)
        rblk = carve([128, 128], BF16); causal = carve([128, 128], BF16); lower = carve([128, 128], BF16)
        ovt = carve([128, 32], BF16)
        cw = carve([128, 3, 4], F32)
        xt = [carve([128, D], F32) for _ in range(2)]
        wfm = [carve([128, 8, 128], BF16) for _ in range(3)]
        wtm = carve([128, 8, 280], BF16)
        xin_sb = [carve([128, 512], F32) for _ in range(2)]
        zb = [carve([128, 514], F32) for _ in range(2)]
        zh = carve([128, 4, 2], F32)
        accb = [carve([128, 512], F32) for _ in range(2)]
        ycv = carve([128, 4, 512], F32)
        ysq = carve([128, 4, 512], BF16)
        rc = carve([128, 512], F32)
        qraw = [carve([128, 512], BF16) for _ in range(2)]
        t1 = [carve([128, 512], F32) for _ in range(2)]
        t2 = [carve([128, 512], F32) for _ in range(2)]
        pT = [carve([128, 512], BF16) for _ in range(4)]
        yatb = [carve([128, 8, 64], F32) for _ in range(2)]
        yab = carve([128, 512], BF16)
        tmpa = carve([128, 4, 64], F32)
        tmpw = carve([128, 4, 64], F32)
        sm = carve([128, 96], F32)
        score = carve([128, 32], F32)
        negsel = carve([128, 32], BF16)
        negselT = carve([128, 128], BF16)
        top8 = carve([128, 8], F32)

        for k3 in range(3):
            dma('sp', cw[:, k3, :], dr['w_conv_mix'][k3].rearrange("(j p) -> p j", p=128), r=[], w=['cw'], allow_slow_non_contiguous=True)
        memset('pool', vs_ext[:, :, :, 64:65], 1.0, w=['vs_ext'])
        memset('pool', vw_ext[:, :, :, 64:65], 1.0, w=['vw_ext'])
        memset('pool', vc_ext, 0.0, w=['vc_ext'])
        memset('pool', vc_ext[:, :, 64:65], 1.0, w=['vc_ext'])
        memset('pool', zh, 0.0, w=['zh'])
        memset('pool', ksz[0][64:128, :], 0.0, w=['ksT']); memset('pool', ksz[1][0:64, :], 0.0, w=['ksT'])
        memset('pool', kwz[0][64:128, :], 0.0, w=['kwT']); memset('pool', kwz[1][0:64, :], 0.0, w=['kwT'])
        memset('pool', kcz[0], 0.0, w=['kcT']); memset('pool', kcz[1], 0.0, w=['kcT'])
        memset('pool', negselT, 0.0, w=['negselT'])

        w_in_v = wbf['w_in'].rearrange("(c p) n -> p c n", p=128)
        xsrc = dr['x']
        fmk = {'k': 0, 'pk': 0, 'nb': 0, 'gi': 0}
        FMB = [0, 1, 2, 5, 6]

        def fm_chunk(col_pieces):
            k = fmk['k'] % 3; fmk['k'] += 1
            pk = FMB[fmk['pk'] % 5]; fmk['pk'] += 1
            o = 0
            for (c0, n) in col_pieces:
                dma('sp', wfm[k][:, :, o:o + n], w_in_v[:, :, c0:c0 + n], r=win_res(c0, n), w=[('wfm', k)])
                o += n
            for kc in range(8):
                mm(P[pk], lhsT=wfm[k][:, kc, :], rhs=nTb[fmk['nb']][:, kc, :], start=(kc == 0), stop=(kc == 7),
                   r=[('wfm', k), ('nT', fmk['nb'])], w=[('P', pk)])
            if pend:
                fn_, g_, t_ = pend.pop(0)
                fn_(g_, t_)
            return pk

        def rope(pk, dst, dst_res, tsl, cT, sT, cres, n=512):
            kq = cnt['x'] % 2; cnt['x'] += 1
            act(qraw[kq][:, 0:n], P[pk][:, 0:n], AF.Copy, r=[('P', pk)], w=[('qraw', kq)])
            mm(P[3][:, 0:n], lhsT=rblk, rhs=qraw[kq][:, 0:n], start=True, stop=True, r=['rblk', ('qraw', kq)], w=[('P', 3)])
            tt('dve', t1[kq][:, 0:n], P[pk][:, 0:n], cT, ALU.mult, r=[('P', pk), cres[0]], w=[('t1', kq)])
            tt('dve', t2[kq][:, 0:n], P[3][:, 0:n], sT, ALU.mult, r=[('P', 3), cres[1]], w=[('t2', kq)])
            if isinstance(dst, list):
                for (d_ap, ps_) in dst:
                    tt('dve', d_ap, t1[kq][ps_, 0:n], t2[kq][ps_, 0:n], ALU.add, r=[('t1', kq), ('t2', kq)], w=[dst_res])
            else:
                tt('dve', dst, t1[kq][:, 0:n], t2[kq][:, 0:n], ALU.add, r=[('t1', kq), ('t2', kq)], w=[dst_res])

        dma('sp', wtm[:, :, 0:128], w_in_v[:, :, 2432:2560], r=win_res(2432, 128), w=['wtm'])
        dma('sp', wtm[:, :, 128:256], w_in_v[:, :, 2688:2816], r=win_res(2688, 128), w=['wtm'])
        dma('sp', wtm[:, :, 256:280], w_in_v[:, :, 2816:2840], r=win_res(2816, 24), w=['wtm'])

        def tf_a(gi, t4):
            ti = gi * 4 + t4
            kx = ti % 2
            k = ti % 2
            dma('sp', xt[kx], xsrc[s, ti * 128:(ti + 1) * 128, :], r=[], w=[('xt', kx)])
            act(junk, xt[kx], AF.Square, r=[('xt', kx)], w=['junk', ('ss', k)], scale=1.0 / 32, accum_out=ss[k])
            act(rr[k], ss[k], AF.Sqrt, r=[('ss', k)], w=[('rr', k)], bias=EPS, scale=1.0)
            recip(rr[k], rr[k], r=[('rr', k)], w=[('rr', k)])
            act(xs[k], xt[kx], AF.Copy, r=[('xt', kx), ('rr', k)], w=[('xs', k)], scale=rr[k])

        def tf_b(gi, t4):
            nb = gi % 2
            k = (gi * 4 + t4) % 2
            for c in range(8):
                tr(PBa[:, c * 128:(c + 1) * 128], xs[k][:, c * 128:(c + 1) * 128], r=[('xs', k)], w=['PB'])
            tt('dve', nTb[nb][:, :, t4 * 128:(t4 + 1) * 128], PBa.rearrange("p (c t) -> p c t", c=8),
               g_mix.unsqueeze(2).to_broadcast([128, 8, 128]), ALU.mult, r=['PB', 'g_mix'], w=[('nT', nb)])

        def tf_c(gi, t4):
            nb = gi % 2
            ti = gi * 4 + t4
            for kc in range(8):
                mm(P[4][:, 0:280], lhsT=nTb[nb][:, kc, t4 * 128:(t4 + 1) * 128], rhs=wtm[:, kc, :], start=(kc == 0),
                   stop=(kc == 7), r=[('nT', nb), 'wtm'], w=[('P', 4)])
            act(vs_ext[:, ti, :, 0:64], P[4][:, 0:128].rearrange("p (h d) -> p h d", h=2), AF.Copy, r=[('P', 4)], w=['vs_ext'])
            act(vw_ext[:, ti, :, 0:64], P[4][:, 128:256].rearrange("p (h d) -> p h d", h=2), AF.Copy, r=[('P', 4)], w=['vw_ext'])
            act(gates[:, ti, :], P[4][:, 256:280], AF.Sigmoid, r=[('P', 4)], w=['gates'])

        def tile_front(gi, t4):
            tf_a(gi, t4); tf_b(gi, t4); tf_c(gi, t4)

        pend = []

        def front_stages(gi):
            st = []
            order = [('a', 0), ('a', 1), ('b', 0), ('a', 2), ('c', 0), ('b', 1), ('a', 3), ('c', 1), ('b', 2), ('c', 2), ('b', 3), ('c', 3)]
            fn = {'a': tf_a, 'b': tf_b, 'c': tf_c}
            for (kind, t4) in order:
                st.append((fn[kind], gi, t4))
            return st

        for t4 in range(4):
            tile_front(0, t4)
        for nm, tl in (('cosT', cosT), ('sinT', sinT), ('coscT', coscT), ('sincT', sincT), ('maskc', maskc),
                       ('selbias', selbias), ('eall', eall), ('rblk', rblk), ('causal', causal), ('lower', lower),
                       ('ov', ovt)):
            dma('sp', tl, dr[nm], r=[], w=[nm])
        for kvh in range(2):
            cp('pool', vc_ext[0:127, kvh, 65:97], ovt[0:127, :], r=['ov'], w=['vc_ext'])
        for gi in range(CFG['nga']):
            g0 = gi * 512
            fmk['nb'] = gi % 2
            fmk['gi'] = gi
            while pend:
                fn_, g_, t_ = pend.pop(0)
                fn_(g_, t_)
            if gi + 1 < CFG['nga']:
                pend.extend(front_stages(gi + 1))
            tsl = slice(g0, g0 + 512)
            if CFG['a_stage'] < 1:
                continue
            for j in range(4):
                kb = j % 2
                pk = fm_chunk([(128 * j, 128)])
                act(xin_sb[kb], P[pk], AF.Copy, r=[('P', pk)], w=[('xin', kb)])
                pk = fm_chunk([(1024 + 128 * j, 128)])
                tt('dve', zb[kb][:, 2:514], P[pk], xin_sb[kb], ALU.mult, r=[('P', pk), ('xin', kb)], w=[('zb', kb)])
                cp('dve', zb[kb][:, 0:2], zh[:, j, :], r=['zh'], w=[('zb', kb)])
                pk = fm_chunk([(512 + 128 * j, 128)])
                act(accb[kb], zb[kb][:, 0:512], AF.Copy, r=[('zb', kb), 'cw'], w=[('acc', kb)], scale=cw[:, 0, j:j + 1])
                stt('dve', accb[kb], zb[kb][:, 1:513], cw[:, 1, j:j + 1], accb[kb], ALU.mult, ALU.add, r=[('zb', kb), 'cw', ('acc', kb)], w=[('acc', kb)])
                stt('dve', accb[kb], zb[kb][:, 2:514], cw[:, 2, j:j + 1], accb[kb], ALU.mult, ALU.add, r=[('zb', kb), 'cw', ('acc', kb)], w=[('acc', kb)])
                cp('dve', zh[:, j, :], zb[kb][:, 512:514], r=[('zb', kb)], w=['zh'])
                tt('dve', ycv[:, j, :], P[pk], accb[kb], ALU.mult, r=[('P', pk), ('acc', kb)], w=[('ycv', j)])
            for j in range(4):
                act(ysq[:, j, :], ycv[:, j, :], AF.Square, r=[('ycv', j)], w=[('ysq', j)])
            for j in range(4):
                mm(P[3], lhsT=ones, rhs=ysq[:, j, :], start=(j == 0), stop=(j == 3), r=['ones', ('ysq', j)], w=[('P', 3)])
            act(rc, P[3], AF.Sqrt, r=[('P', 3)], w=['rc'], bias=EPS, scale=1.0 / 512)
            recip(rc, rc, r=['rc'], w=['rc'])
            for j in range(4):
                stt('dve', mixedT[:, j, tsl], ycv[:, j, :], g_gc[:, j:j + 1], rc, ALU.mult, ALU.mult,
                    r=[('ycv', j), 'g_gn_conv', 'rc'], w=['mixedT'])
            if CFG['a_stage'] < 2:
                continue
            for c in range(4):
                pk = fm_chunk([(1536 + 64 * c, 64), (1536 + 64 * (4 + c), 64)])
                rope(pk, qT[:, c, tsl], 'qT', tsl, cosT[:, tsl], sinT[:, tsl], ('cosT', 'sinT'))
            if CFG['a_stage'] < 3:
                continue
            pk = fm_chunk([(2304, 128)])
            rope(pk, [(ksz[0][0:64, tsl], slice(0, 64)), (ksz[1][64:128, tsl], slice(64, 128))], 'ksT', tsl, cosT[:, tsl], sinT[:, tsl], ('cosT', 'sinT'))
            pk = fm_chunk([(2560, 128)])
            rope(pk, [(kwz[0][0:64, tsl], slice(0, 64)), (kwz[1][64:128, tsl], slice(64, 128))], 'kwT', tsl, cosT[:, tsl], sinT[:, tsl], ('cosT', 'sinT'))
            pk = fm_chunk([(2048, 128)])
            act(kcmpT[:, tsl], P[pk], AF.Copy, r=[('P', pk)], w=['kcmpT'])
            pk = fm_chunk([(2176, 128)])
            act(vcmpT[:, tsl], P[pk], AF.Copy, r=[('P', pk)], w=['vcmpT'])

        for which, srcT, sres in ((('k', kcmpT, 'kcmpT'), ('v', vcmpT, 'vcmpT')) if CFG['do_b'] else ()):
            w1 = dr['cmp_w1_' + which]; w2 = dr['cmp_w2_' + which]; pe = dr['cmp_pe_' + which]
            memset('pool', w1blk, 0.0, w=['w1blk'])
            memset('pool', w2blk, 0.0, w=['w2blk'])
            w1v = w1.rearrange("(j d) h -> d j h", d=64)
            dma('pool', w1blk[0:64, :, 0:64], w1v, r=[], w=['w1blk'])
            dma('pool', w1blk[64:128, :, 64:128], w1v, r=[], w=['w1blk'])
            dma('pool', w2blk[0:64, 0:64], w2, r=[], w=['w2blk'])
            dma('pool', w2blk[64:128, 64:128], w2, r=[], w=['w2blk'])
            w1c = w1.rearrange("(i p) h -> p i h", p=128)
            dma('pool', w1d[:, :, 0:64], w1c, r=[], w=['w1d'])
            dma('pool', w1d[:, :, 64:128], w1c, r=[], w=['w1d'])
            dma('pool', pecol, pe.rearrange("(i a) d -> (a d) i", a=2), r=[], w=['pecol'],
                allow_slow_non_contiguous=True)
            for i in range(16):
                mm(P[6][:, 0:1], lhsT=w1d[:, i, :], rhs=pecol[:, i:i + 1], start=(i == 0), stop=(i == 15),
                   r=['w1d', 'pecol'], w=[('P', 6)])
            act(c1, P[6][:, 0:1], AF.Copy, r=[('P', 6)], w=['c1'])
            for j in range(32):
                mm(P[5][:, 0:127], lhsT=w1blk[:, j, :], rhs=srcT[:, j:j + 2017:16], start=(j == 0), stop=(j == 31),
                   r=['w1blk', sres], w=[('P', 5)])
            act(hidT[:, 0:127], P[5][:, 0:127], AF.Silu, r=[('P', 5), 'c1'], w=['hidT'], bias=c1)
            if which == 'k':
                mm(P[6][:, 0:127], lhsT=w2blk, rhs=hidT[:, 0:127], start=True, stop=True, r=['w2blk', 'hidT'], w=[('P', 6)])
                act(kcraw[:, 0:127], P[6][:, 0:127], AF.Copy, r=[('P', 6)], w=['kcraw'])
                mm(P[3][:, 0:127], lhsT=rblk, rhs=kcraw[:, 0:127], start=True, stop=True, r=['rblk', 'kcraw'], w=[('P', 3)])
                tt('dve', t1[0][:, 0:127], P[6][:, 0:127], coscT, ALU.mult, r=[('P', 6), 'coscT'], w=[('t1', 0)])
                tt('dve', t2[0][:, 0:127], P[3][:, 0:127], sincT, ALU.mult, r=[('P', 3), 'sincT'], w=[('t2', 0)])
                tt('dve', kcz[0][0:64, 0:127], t1[0][0:64, 0:127], t2[0][0:64, 0:127], ALU.add, r=[('t1', 0), ('t2', 0)], w=['kcT'])
                tt('dve', kcz[1][64:128, 0:127], t1[0][64:128, 0:127], t2[0][64:128, 0:127], ALU.add, r=[('t1', 0), ('t2', 0)], w=['kcT'])
            else:
                mm(P[6][0:127, 0:128], lhsT=hidT[:, 0:127], rhs=w2blk, start=True, stop=True, r=['w2blk', 'hidT'], w=[('P', 6)])
                act(vc_ext[0:127, :, 0:64], P[6][0:127, 0:128].rearrange("p (h d) -> p h d", h=2), AF.Copy,
                    r=[('P', 6)], w=['vc_ext'])
        if s == 0:
            cast_w('w_out', 1024, gate=['kcT'])
            cast_w('w_up', 512, rsplit=2, gate=['kcT'])
            cast_w('w_down', 1024, gate=['kcT'])
            cast_w('w_ple_gate', 1024, gate=['kcT'])
            cast_w('w_ple_proj', 1024, gate=['kcT'])
        dump('qT', qT[:, :, 0:512], r=['qT'])
        dump('mixc', mixedT[:, 0:4, 0:512], r=['mixedT'])

        sk = {'s': 0, 'p': 0, 'u': 0}
        fin_pending = []
        for i in range(CFG['nqt']):
            qsl = slice(i * 128, (i + 1) * 128)
            yat = yatb[i % 2]
            yres = ('yat', i % 2)
            for kvh in range(2):
                hp = slice(0, 64) if kvh == 0 else slice(64, 128)
                q_rhs = qT[:, :, qsl]

                def score_tile(kT_l, extra, kparts=128):
                    b = sk['s'] % 3; sk['s'] += 1
                    kp = sk['p'] % 4; sk['p'] += 1
                    n_ex = len(extra)
                    o3 = P[b][0:kparts, :].rearrange("p (h q) -> p h q", h=4)
                    mm(o3, lhsT=kT_l[0], rhs=q_rhs, start=True, stop=(n_ex == 0), r=[kT_l[1], 'qT'], w=[('P', b)])
                    for xi, (l, rhs_, rres) in enumerate(extra):
                        mm(o3, lhsT=l, rhs=rhs_, start=False, stop=(xi == n_ex - 1), r=rres, w=[('P', b)])
                    act(pT[kp][0:kparts, :], P[b][0:kparts, :], AF.Exp, r=[('P', b)], w=[('pT', kp)], scale=0.125)
                    return kp

                gv = gates[:, i, :].rearrange("p (h b) -> p h b", b=3)
                yv = yat[:, kvh * 4:(kvh + 1) * 4, :]
                ccc = sm[:, 8:12]; ccs = sm[:, 12:16]; ccw = sm[:, 16:20]
                owv = P[5][:, 0:260].rearrange("p (h c) -> p h c", h=4)
                ocb = 3 if (sk['u'] % 2 == 0) else 6
                sk['u'] += 1
                oc = P[ocb][:, 0:388].rearrange("p (h c) -> p h c", h=4)
                rdc = sm[:, 0:4]
                nsb = negselT.unsqueeze(1).to_broadcast([128, 4, 128])
                cb = causal.unsqueeze(1).to_broadcast([128, 4, 128])
                lb = lower.unsqueeze(1).to_broadcast([128, 4, 128])
                mk = maskc[:, i, :].unsqueeze(1).to_broadcast([128, 4, 128])

                def pv_c(kp):
                    for hb in range(4):
                        mm(P[ocb][:, hb * 97:(hb + 1) * 97], lhsT=pT[kp][:, hb * 128:(hb + 1) * 128], rhs=vc_ext[:, kvh, :],
                           start=True, stop=True, r=[('pT', kp), 'vc_ext'], w=[('P', ocb)])
                    ts('dve', rdc, oc[:, :, 64], 1e-30, None, ALU.max, None, r=[('P', ocb)], w=['rdc'])
                    recip(rdc, rdc, r=['rdc'], w=['rdc'])
                    ts('dve', score, oc[:, 0, 65:97], rdc[:, 0:1], None, ALU.mult, None, r=[('P', ocb), 'rdc'], w=['score'])
                    for hb in range(1, 4):
                        stt('dve', score, oc[:, hb, 65:97], rdc[:, hb:hb + 1], score, ALU.mult, ALU.add,
                            r=[('P', ocb), 'rdc', 'score'], w=['score'])
                    tt('dve', score, score, selbias[:, i, :], ALU.add, r=['score', 'selbias'], w=['score'])
                    S.add('dve', lambda e: e.max(out=top8, in_=score), r=['score'], w=['top8'])
                    ts('dve', score, score, top8[:, 7:8], None, ALU.is_ge, None, r=['score', 'top8'], w=['score'])
                    ts('dve', negsel, score, 1.0, -NEGB, ALU.subtract, ALU.mult, r=['score'], w=['negsel'])
                    tr(PBa[0:32, 0:128], negsel, r=['negsel'], w=['PB'])
                    cp('dve', negselT[0:32, :], PBa[0:32, 0:128], r=['PB'], w=['negselT'])
                    tt('dve', ccc, rdc, gv[:, kvh * 4:(kvh + 1) * 4, 0], ALU.mult, r=['rdc', 'gates'], w=['ccc'])
                    tt('dve', yv, oc[:, :, 0:64], ccc.unsqueeze(2).to_broadcast([128, 4, 64]), ALU.mult, r=[('P', ocb), 'ccc'], w=[yres])

                def win_done():
                    ts('dve', ccw, owv[:, :, 64], 1e-30, None, ALU.max, None, r=[('P', 5)], w=['ccw'])
                    recip(ccw, ccw, r=['ccw'], w=['ccw'])
                    tt('dve', ccw, ccw, gv[:, kvh * 4:(kvh + 1) * 4, 2], ALU.mult, r=['ccw', 'gates'], w=['ccw'])
                    tt('dve', tmpw, owv[:, :, 0:64], ccw.unsqueeze(2).to_broadcast([128, 4, 64]), ALU.mult, r=[('P', 5), 'ccw'], w=['tmpw'])
                    tt('dve', yv, yv, tmpw, ALU.add, r=[yres, 'tmpw'], w=[yres])

                def mk_pv(bank, vext, vres, j, first, last):
                    def pv(kp):
                        for hb in range(4):
                            mm(P[bank][:, hb * 65:(hb + 1) * 65], lhsT=pT[kp][:, hb * 128:(hb + 1) * 128], rhs=vext[:, j, kvh, :],
                               start=(first and hb == 0), stop=(hb == 3), r=[('pT', kp), vres], w=[('P', bank)],
                               skip_group_check=True)
                        if last and bank == 5:
                            win_done()
                    return pv

                tasks = [(((kcz[kvh], 'kcT'), [(ident, mk, ['ident', 'maskc'])], 128), pv_c)]
                j0 = max(0, i - 4)
                for j in range(j0, i + 1):
                    ex = []
                    if j == i:
                        ex.append((ident, cb, ['ident', 'causal']))
                    if j == i - 4:
                        ex.append((ident, lb, ['ident', 'lower']))
                    tasks.append((((kwz[kvh][:, j * 128:(j + 1) * 128], 'kwT'), ex, 128), mk_pv(5, vw_ext, 'vw_ext', j, j == j0, j == i)))
                n_presel = len(tasks)
                for j in range(i + 1):
                    ex = [(eall[:, j, :], nsb, ['eall', 'negselT'])]
                    if j == i:
                        ex.append((ident, cb, ['ident', 'causal']))
                    tasks.append((((ksz[kvh][:, j * 128:(j + 1) * 128], 'ksT'), ex, 128), mk_pv(4, vs_ext, 'vs_ext', j, j == 0, j == i)))
                pvq = []
                for ti_, (sargs, pvf) in enumerate(tasks):
                    if ti_ == n_presel and any(f_ is pv_c for (f_, _k) in pvq):
                        while pvq:
                            f_, k_ = pvq.pop(0)
                            f_(k_)
                    kp = score_tile(sargs[0], sargs[1], kparts=sargs[2])
                    pvq.append((pvf, kp))
                    if len(pvq) > 2:
                        f_, k_ = pvq.pop(0)
                        f_(k_)
                    if kvh == 0 and ti_ == min(3, len(tasks) - 1) and fin_pending:
                        fin_pending.pop(0)()
                while pvq:
                    f_, k_ = pvq.pop(0)
                    f_(k_)
                osv = P[4][:, 0:260].rearrange("p (h c) -> p h c", h=4)
                ts('dve', ccs, osv[:, :, 64], 1e-30, None, ALU.max, None, r=[('P', 4)], w=['ccs'])
                recip(ccs, ccs, r=['ccs'], w=['ccs'])
                tt('dve', ccs, ccs, gv[:, kvh * 4:(kvh + 1) * 4, 1], ALU.mult, r=['ccs', 'gates'], w=['ccs'])
                tt('dve', tmpa, osv[:, :, 0:64], ccs.unsqueeze(2).to_broadcast([128, 4, 64]), ALU.mult, r=[('P', 4), 'ccs'], w=['tmpa'])
                tt('dve', yv, yv, tmpa, ALU.add, r=[yres, 'tmpa'], w=[yres])
            def make_fin(i=i, qsl=qsl, yat=yat, yres=yres):
                def fin():
                    k = cnt['n'] % 2; cnt['n'] += 1
                    yflat = yat.rearrange("p h d -> p (h d)")
                    if i == 0:
                        dump('yat0', yflat, r=[yres])
                    act(junk[:, 0:512], yflat, AF.Square, r=[yres], w=['junk', ('ss', k)], scale=float(512 ** -0.5), accum_out=ss[k])
                    act(rr[k], ss[k], AF.Sqrt, r=[('ss', k)], w=[('rr', k)], bias=EPS, scale=1.0)
                    recip(rr[k], rr[k], r=[('rr', k)], w=[('rr', k)])
                    act(yab, yflat, AF.Copy, r=[yres, ('rr', k)], w=['yab'], scale=rr[k])
                    for c in range(4):
                        tr(PBa[:, c * 128:(c + 1) * 128], yab[:, c * 128:(c + 1) * 128], r=['yab'], w=['PB'])
                    tt('dve', mixedT[:, 4:8, qsl], PBa[:, 0:512].rearrange("p (c t) -> p c t", c=4),
                       g_ga.unsqueeze(2).to_broadcast([128, 4, 128]), ALU.mult, r=['PB', 'g_gn_attn'], w=['mixedT'])
                return fin
            fin_pending.append(make_fin())
            if i == CFG['nqt'] - 1:
                while fin_pending:
                    fin_pending.pop(0)()
        dump('mixa', mixedT[:, 4:8, 0:512], r=['mixedT'])

    def phase2(s):
        state['off'] = base_off
        h = [carve([128, D], F32) for _ in range(4)]
        actT = carve([128, NF, 512], BF16)
        abuf = [carve([128, 514], F32) for _ in range(4)]
        ub = [carve([128, 512], F32) for _ in range(4)]
        sg = [carve([128, 512], F32) for _ in range(2)]
        wub = [carve([128, 8, 256], BF16) for _ in range(4)]
        wdb = [carve([128, 512], BF16) for _ in range(8)]
        w_out = carve([128, 8, D], BF16)
        wpg = carve([128, 8, D], BF16)
        wpp = carve([128, 2, D], BF16)
        gfin = carve([128, D], F32)
        outt = [carve([128, D], F32) for _ in range(2)]
        pt = [carve([128, 256], F32) for _ in range(2)]
        pbf = [carve([128, 256], BF16) for _ in range(2)]
        ppT = carve([128, 2, 512], BF16)
        sgm = [carve([128, 512], F32) for _ in range(2)]
        tmp = [carve([128, 512], F32) for _ in range(2)]
        hal = carve([128, 2 * NF, 2], F32)
        cwf = carve([128, 3, 2 * NF], F32)

        dma('sp', w_out, wbf['w_out'].rearrange("(c p) n -> p c n", p=128), r=['w_out_bf'], w=['w_out'])
        dma('sp', wpg, wbf['w_ple_gate'].rearrange("(c p) n -> p c n", p=128), r=['w_ple_gate_bf'], w=['wpg'])
        dma('sp', wpp, wbf['w_ple_proj'].rearrange("(c p) n -> p c n", p=128), r=['w_ple_proj_bf'], w=['wpp'])
        dma('sp', gfin, dr['g_final'].partition_broadcast(128), r=[], w=['gfin'])
        for k3 in range(3):
            dma('sp', cwf[:, k3, :], dr['w_ffn_conv'][k3].rearrange("(f p) -> p f", p=128), r=[], w=['cwf'], allow_slow_non_contiguous=True)
        memset('pool', hal, 0.0, w=['hal'])
        w_up_v = wbf['w_up'].rearrange("(c p) n -> p c n", p=128)
        w_dn_v = wbf['w_down'].rearrange("(f p) n -> p f n", p=128)
        ck = {'u': 0, 'd': 0, 'pp': 0, 'ab': 0, 'o': 0, 'sg': 0}

        for gi in range(CFG['ng2']):
            g0 = gi * 512
            for t4 in range(4):
                tok = slice(g0 + t4 * 128, g0 + (t4 + 1) * 128)
                dma('sp', h[t4], dr['x'][s, tok, :], r=[], w=[('h', t4)])
                for half in range(2):
                    pk = ck['pp'] % 4; ck['pp'] += 1
                    for kc in range(8):
                        mm(P[pk], lhsT=mixedT[:, kc, tok], rhs=w_out[:, kc, half * 512:(half + 1) * 512], start=(kc == 0),
                           stop=(kc == 7), r=['mixedT', 'w_out'], w=[('P', pk)])
                    hs = h[t4][:, half * 512:(half + 1) * 512]
                    tt('dve', hs, P[pk], hs, ALU.add, r=[('P', pk), ('h', t4)], w=[('h', t4)])
                if t4 >= 1:
                    rmsnorm_T(h[t4 - 1], ('h', t4 - 1), g_ffn, 'g_ffn', slice((t4 - 1) * 128, t4 * 128))
            rmsnorm_T(h[3], ('h', 3), g_ffn, 'g_ffn', slice(3 * 128, 4 * 128))
            def up_head(f):
                k = ck['u'] % 4; ck['u'] += 1
                dma('sp', wub[k][:, :, 0:128], w_up_v[:, :, f * 128:(f + 1) * 128], r=['w_up_bf'], w=[('wub', k)])
                dma('sp', wub[k][:, :, 128:256], w_up_v[:, :, DFF + f * 128:DFF + (f + 1) * 128], r=['w_up_bf'], w=[('wub', k)])
                pg = (ck['pp'] % 3) * 2; ck['pp'] += 1
                us = []
                for hv in range(2):
                    pk = pg + hv
                    for kc in range(8):
                        mm(P[pk], lhsT=wub[k][:, kc, hv * 128:(hv + 1) * 128], rhs=nT[:, kc, :], start=(kc == 0), stop=(kc == 7),
                           r=[('wub', k), ('nT', 0)], w=[('P', pk)])
                    a = ck['ab'] % 4; ck['ab'] += 1
                    us.append((a, hv * NF + f, pk))
                for (a, fi, pk) in us:
                    act(abuf[a][:, 2:514], P[pk], AF.Copy, r=[('P', pk)], w=[('abuf', a)])
                    cp('dve', abuf[a][:, 0:2], hal[:, fi, :], r=['hal'], w=[('abuf', a)])
                return us

            def up_mid(us):
                for (a, fi, pk) in us:
                    act(ub[a], abuf[a][:, 0:512], AF.Copy, r=[('abuf', a), 'cwf'], w=[('ub', a)], scale=cwf[:, 0, fi:fi + 1])
                    cp('dve', hal[:, fi, :], abuf[a][:, 512:514], r=[('abuf', a)], w=['hal'])
                for tap in (1, 2):
                    for (a, fi, pk) in us:
                        stt('dve', ub[a], abuf[a][:, tap:tap + 512], cwf[:, tap, fi:fi + 1], ub[a], ALU.mult, ALU.add,
                            r=[('abuf', a), 'cwf', ('ub', a)], w=[('ub', a)])

            def up_tail(f, us):
                q = ck['sg'] % 2; ck['sg'] += 1
                act(sg[q], ub[us[0][0]], AF.Silu, r=[('ub', us[0][0])], w=[('sg', q)])
                tt('dve', actT[:, f, :], sg[q], ub[us[1][0]], ALU.mult, r=[('sg', q), ('ub', us[1][0])], w=['actT'])

            hist = {}
            for f in range(NF + 2):
                if f < NF:
                    hist[f] = up_head(f)
                if 0 <= f - 1 < NF:
                    up_mid(hist[f - 1])
                if 0 <= f - 2 < NF:
                    up_tail(f - 2, hist[f - 2])
            for half in range(2):
                banks = [0, 1, 2, 3] if half == 0 else [4, 5, 6, 0]
                for f in range(NF):
                    k = ck['d'] % 8; ck['d'] += 1
                    dma('sp', wdb[k], w_dn_v[:, f, half * 512:(half + 1) * 512], r=['w_down_bf'], w=[('wdb', k)])
                    for t4 in range(4):
                        mm(P[banks[t4]], lhsT=actT[:, f, t4 * 128:(t4 + 1) * 128], rhs=wdb[k], start=(f == 0), stop=True,
                           r=['actT', ('wdb', k)], w=[('P', banks[t4])], skip_group_check=True)
                for t4 in range(4):
                    hs = h[t4][:, half * 512:(half + 1) * 512]
                    tt('dve', hs, P[banks[t4]], hs, ALU.add, r=[('P', banks[t4]), ('h', t4)], w=[('h', t4)])
            for t4 in range(4):
                tok = slice(g0 + t4 * 128, g0 + (t4 + 1) * 128)
                rmsnorm_T(h[t4], ('h', t4), g_ple, 'g_ple', slice(t4 * 128, (t4 + 1) * 128))
                kp = t4 % 2
                dma('sp', pt[kp], dr['p'][s, tok, :], r=[], w=[('pt', kp)])
                act(pbf[kp], pt[kp], AF.Copy, r=[('pt', kp)], w=[('pbf', kp)])
                for c in range(2):
                    tr(PBa[:, c * 128:(c + 1) * 128], pbf[kp][:, c * 128:(c + 1) * 128], r=[('pbf', kp)], w=['PB'])
                cp('dve', ppT[:, :, t4 * 128:(t4 + 1) * 128], PBa[:, 0:256].rearrange("p (c t) -> p c t", c=2), r=['PB'], w=['ppT'])
            for t4 in range(4):
                for half in range(2):
                    pk = (ck['pp'] % 3) * 2; ck['pp'] += 1
                    cs = slice(half * 512, (half + 1) * 512)
                    for kc in range(8):
                        mm(P[pk], lhsT=nT[:, kc, t4 * 128:(t4 + 1) * 128], rhs=wpg[:, kc, cs], start=(kc == 0), stop=(kc == 7),
                           r=[('nT', 0), 'wpg'], w=[('P', pk)])
                    for kc in range(2):
                        mm(P[pk + 1], lhsT=ppT[:, kc, t4 * 128:(t4 + 1) * 128], rhs=wpp[:, kc, cs], start=(kc == 0), stop=(kc == 1),
                           r=['ppT', 'wpp'], w=[('P', pk + 1)])
                    q = ck['sg'] % 2; ck['sg'] += 1
                    act(sgm[q], P[pk], AF.Sigmoid, r=[('P', pk)], w=[('sgm', q)])
                    tt('dve', tmp[q], P[pk + 1], sgm[q], ALU.mult, r=[('P', pk + 1), ('sgm', q)], w=[('tmp', q)])
                    tt('dve', h[t4][:, cs], h[t4][:, cs], tmp[q], ALU.add, r=[('h', t4), ('tmp', q)], w=[('h', t4)])
            for t4 in range(4):
                tok = slice(g0 + t4 * 128, g0 + (t4 + 1) * 128)
                k = cnt['n'] % 2; cnt['n'] += 1
                o = ck['o'] % 2; ck['o'] += 1
                act(junk, h[t4], AF.Square, r=[('h', t4)], w=['junk', ('ss', k)], scale=1.0 / 32, accum_out=ss[k])
                act(rr[k], ss[k], AF.Sqrt, r=[('ss', k)], w=[('rr', k)], bias=EPS, scale=1.0)
                recip(rr[k], rr[k], r=[('rr', k)], w=[('rr', k)])
                stt('dve', outt[o], h[t4], rr[k], gfin, ALU.mult, ALU.mult, r=[('h', t4), ('rr', k), 'gfin'], w=[('outt', o)])
                dma('sp', out[s, tok, :], outt[o], r=[('outt', o)], w=[])

    for s in range(CFG['nseq']):
        phase1(s)
        S.barrier()
        if CFG['do_p2']:
            phase2(s)
            S.barrier()
    S.run()
    es.close()
    return nc


_CACHE = {}


def kernel(**inputs):
    ncores = 8
    consts = host_consts()
    if 'nc' not in _CACHE:
        _CACHE['nc'] = build_program()
    nc = _CACHE['nc']
    x = np.ascontiguousarray(np.asarray(inputs['x'], dtype=np.float32))
    p = np.ascontiguousarray(np.asarray(inputs['p'], dtype=np.float32))
    in_maps = []
    for c in range(ncores):
        m = {'x': x[NSEQ * c:NSEQ * (c + 1)], 'p': p[0, NSEQ * c:NSEQ * (c + 1)]}
        for k in WSHAPES:
            a = np.asarray(inputs[k], dtype=np.float32)
            if k != 'g_final':
                a = a[0]
            m[k] = np.ascontiguousarray(a)
        for k, v in consts.items():
            m['c_' + k] = v
        in_maps.append(m)
    res = run_bass_kernel_spmd(nc, in_maps, core_ids=list(range(ncores)))
    outs = [np.asarray(r['out'], dtype=np.float32) for r in res.results]
    return np.concatenate(outs, axis=0)
```
